# Optimizing a Trainium2 kernel written in Bass

```python
import math
import jax
import jax.numpy as jnp
from jax import lax
import numpy as np

D_MODEL = 1024
BATCH = 8
SEQ = 4096
DEPTH = 4

CTX_LEN = 256
GRID_W = 64
NORM_EPS = 1e-6

DA_QK_DIM = 64
DA_V_DIM = 2 * DA_QK_DIM
DA_WIDTH = D_MODEL // 2
DA_HEADS = DA_WIDTH // DA_V_DIM
DA_QK_COLS = DA_HEADS * 2 * DA_QK_DIM
DA_COLS = 2 * DA_QK_COLS + DA_WIDTH
DA_QBLOCK = 128
ROPE_THETA = 10000.0

RW_HEAD_DIM = 64
RW_WIDTH = D_MODEL // 4
RW_HEADS = RW_WIDTH // RW_HEAD_DIM
RW_DECAY_RANK = 32
RW_ICL_RANK = 32
RW_GATE_RANK = 64
RW_DECAY_SCALE = 0.606531
RW_GN_EPS = 64e-5
RW_COLS = 3 * RW_WIDTH + 2 * RW_DECAY_RANK + 2 * RW_ICL_RANK + RW_GATE_RANK

ML_HEAD_DIM = 64
ML_WIDTH = D_MODEL - DA_WIDTH - RW_WIDTH
ML_HEADS = ML_WIDTH // ML_HEAD_DIM
ML_CHUNK = 128
ML_COLS = 4 * ML_WIDTH + 4 * ML_HEADS

IN_COLS = DA_COLS + RW_COLS + ML_COLS
MIX_WIDTH = DA_WIDTH + RW_WIDTH + ML_WIDTH

N_GROUPS = 4
EXPERTS_PER_GROUP = 8
N_EXPERTS = N_GROUPS * EXPERTS_PER_GROUP
TOP_K = 2
EXPERT_HIDDEN = D_MODEL // 2
MOE_BLOCK = 256

kernel_name = 'hybrid_diffattn_rwkv7_mlstm_hmoe_dit'


def rms_norm(x, g):
    xf = x.astype(jnp.float32)
    y = xf * lax.rsqrt(jnp.mean(xf * xf, axis=-1, keepdims=True) + NORM_EPS)
    return (y * g.astype(jnp.float32)).astype(x.dtype)


def layer_norm_nogain(y, eps):
    mu = jnp.mean(y, axis=-1, keepdims=True)
    var = jnp.mean(jnp.square(y - mu), axis=-1, keepdims=True)
    return (y - mu) * lax.rsqrt(var + eps)


def split_heads(u, h, d):
    return u.reshape(u.shape[:-1] + (h, d))


def neighbour_mean(z):
    zp = jnp.pad(z, ((0, 0), (1, 1), (0, 0)))
    return 0.5 * (zp[:, :-2] + zp[:, 2:])


def dwconv3(z, w, b):
    zp = jnp.pad(z, ((0, 0), (1, 1), (0, 0)))
    return zp[:, :-2] * w[0] + zp[:, 1:-1] * w[1] + zp[:, 2:] * w[2] + b


def axial_rope(u, row, col):
    half = DA_QK_DIM // 2
    nf = half // 2
    inv = ROPE_THETA ** (-jnp.arange(nf, dtype=jnp.float32) / nf)

    def rot(v, pos):
        ang = pos[:, None] * inv
        cos = jnp.cos(ang)[None, :, None, None, :]
        sin = jnp.sin(ang)[None, :, None, None, :]
        v1, v2 = v[..., :nf], v[..., nf:]
        return jnp.concatenate([v1 * cos - v2 * sin, v1 * sin + v2 * cos], axis=-1)

    uf = u.astype(jnp.float32)
    return jnp.concatenate([rot(uf[..., :half], row), rot(uf[..., half:], col)], axis=-1).astype(u.dtype)


def diff_attention(q, k, v, lam):
    B, Tq = q.shape[:2]
    nb = Tq // DA_QBLOCK
    qb = jnp.moveaxis(q.reshape((B, nb, DA_QBLOCK) + q.shape[2:]), 1, 0)
    scale = DA_QK_DIM ** -0.5

    def block(qi):
        s = jnp.einsum('bqhmd,bkhmd->bhmqk', qi, k).astype(jnp.float32) * scale
        p = jax.nn.softmax(s, axis=-1)
        a = p[:, :, 0] - lam * p[:, :, 1]
        return jnp.einsum('bhqk,bkhe->bqhe', a, v.astype(jnp.float32))

    o = lax.map(block, qb)
    return jnp.moveaxis(o, 0, 1).reshape((B, Tq) + o.shape[3:])


def da_heads(z):
    B, T = z.shape[:2]
    q = z[..., :DA_QK_COLS].reshape(B, T, DA_HEADS, 2, DA_QK_DIM)
    k = z[..., DA_QK_COLS:2 * DA_QK_COLS].reshape(B, T, DA_HEADS, 2, DA_QK_DIM)
    v = z[..., 2 * DA_QK_COLS:DA_COLS].reshape(B, T, DA_HEADS, DA_V_DIM)
    return q, k, v


def da_post(o, g, lam_init):
    B, T = o.shape[:2]
    return (rms_norm(o, g) * (1.0 - lam_init)).reshape(B, T, DA_WIDTH)


def rwkv_step(S, inp):
    r, w, kk, a, k, v = inp
    sa = jnp.einsum('dbhij,dbhj->dbhi', S, kk)
    S = S * w[..., None, :] - sa[..., :, None] * (kk * a)[..., None, :] + v[..., :, None] * k[..., None, :]
    return S, jnp.einsum('dbhij,dbhj->dbhi', S, r)


def rwkv_group(z, state0, mu, w0, w2, a0, a2, g2, k_k, k_a, r_k, ln_g, ln_b):
    z = z.astype(jnp.float32)
    z = z + mu * (neighbour_mean(z) - z)
    B, T = z.shape[:2]
    C, H, N = RW_WIDTH, RW_HEADS, RW_HEAD_DIM
    r, k, v = z[..., :C], z[..., C:2 * C], z[..., 2 * C:3 * C]
    o1 = 3 * C + 2 * RW_DECAY_RANK
    o2 = o1 + 2 * RW_ICL_RANK
    wd = z[..., 3 * C:o1].reshape(B, T, 2, RW_DECAY_RANK)
    ad = z[..., o1:o2].reshape(B, T, 2, RW_ICL_RANK)
    gd = z[..., o2:]
    decay = jnp.exp(-RW_DECAY_SCALE * jax.nn.sigmoid(w0 + jnp.einsum('btdr,drc->btdc', jnp.tanh(wd), w2)))
    a = jax.nn.sigmoid(a0 + jnp.einsum('btdr,drc->btdc', ad, a2))
    g = jax.nn.sigmoid(gd) @ g2
    kk = split_heads(k * k_k, H, N)
    kk = kk / jnp.maximum(jnp.sqrt(jnp.sum(kk * kk, axis=-1, keepdims=True)), 1e-12)
    kmod = split_heads(k[:, :, None, :] * (1.0 + (a - 1.0) * k_a), H, N)
    rh = split_heads(r, H, N)
    vh = split_heads(v, H, N)

    def shared(u):
        return jnp.stack([u, jnp.flip(u, 1)], 0).transpose(2, 0, 1, 3, 4)

    def per_dir(u):
        return jnp.stack([u[:, :, 0], jnp.flip(u[:, :, 1], 1)], 0).transpose(2, 0, 1, 3, 4)

    state, ys = lax.scan(rwkv_step, state0,
                         (shared(rh), per_dir(split_heads(decay, H, N)), shared(kk),
                          per_dir(split_heads(a, H, N)), per_dir(kmod), shared(vh)))
    y = (ys[:, 0] + jnp.flip(ys[:, 1], 0)).transpose(1, 0, 2, 3)
    y = layer_norm_nogain(y, RW_GN_EPS).reshape(B, T, C) * ln_g + ln_b
    bonus = jnp.sum(rh * r_k * (kmod[:, :, 0] + kmod[:, :, 1]), axis=-1, keepdims=True) * vh
    return (y + bonus.reshape(B, T, C)) * g, state


def mlstm_chunk_step(carry, inp):
    C0, n0, m0 = carry
    q, k, v, ig, lf = inp
    L = q.shape[-2]
    mask = jnp.tril(jnp.ones((L, L), dtype=bool))
    b = jnp.cumsum(lf, axis=-1)
    dmat = jnp.where(mask, b[..., :, None] - b[..., None, :] + ig[..., None, :], -jnp.inf)
    inter = b + m0[..., None]
    m = jnp.maximum(jnp.max(dmat, axis=-1), inter)
    s = jnp.einsum('...td,...sd->...ts', q, k) * jnp.exp(dmat - m[..., None])
    ie = jnp.exp(inter - m)
    num = jnp.einsum('...ts,...sd->...td', s, v) + ie[..., None] * jnp.einsum('...td,...ed->...te', q, C0)
    den = jnp.sum(s, axis=-1) + ie * jnp.einsum('...td,...d->...t', q, n0)
    h = num / jnp.maximum(jnp.abs(den), jnp.exp(-m))[..., None]
    b_last = b[..., -1]
    gam = b_last[..., None] - b + ig
    m_new = jnp.maximum(b_last + m0, jnp.max(gam, axis=-1))
    sc = jnp.exp(gam - m_new[..., None])
    carry_decay = jnp.exp(b_last + m0 - m_new)
    C_new = carry_decay[..., None, None] * C0 + jnp.einsum('...s,...se,...sd->...ed', sc, v, k)
    n_new = carry_decay[..., None] * n0 + jnp.einsum('...s,...sd->...d', sc, k)
    return (C_new, n_new, m_new), h


def mlstm_group(z, state0, conv_w, conv_b, gate_b, norm_g):
    z = z.astype(jnp.float32)
    B, T = z.shape[:2]
    W, H, d, L = ML_WIDTH, ML_HEADS, ML_HEAD_DIM, ML_CHUNK
    nc = T // L
    qk = jax.nn.silu(dwconv3(z[..., :2 * W], conv_w, conv_b))
    q = split_heads(qk[..., :W], H, d)
    k = split_heads(qk[..., W:], H, d) * (d ** -0.5)
    v = split_heads(z[..., 2 * W:3 * W], H, d)
    o = z[..., 3 * W:4 * W]
    gates = (z[..., 4 * W:] + gate_b).reshape(B, T, 2, 2, H)
    ig = gates[:, :, :, 0]
    lf = jax.nn.log_sigmoid(gates[:, :, :, 1])

    def shared(u):
        u = jnp.stack([u, jnp.flip(u, 1)], 0)
        return u.reshape(2, B, nc, L, H, d).transpose(2, 0, 1, 4, 3, 5)

    def per_dir(u):
        u = jnp.stack([u[:, :, 0], jnp.flip(u[:, :, 1], 1)], 0)
        return u.reshape(2, B, nc, L, H).transpose(2, 0, 1, 4, 3)

    state, hs = lax.scan(mlstm_chunk_step, state0,
                         (shared(q), shared(k), shared(v), per_dir(ig), per_dir(lf)))
    hs = hs.transpose(1, 2, 0, 4, 3, 5).reshape(2, B, T, H, d)
    h = layer_norm_nogain(hs[0] + jnp.flip(hs[1], 1), NORM_EPS).reshape(B, T, W) * norm_g
    return jax.nn.sigmoid(o) * h, state


def hier_moe(h, wg, bg, we, be, w_gu, w_down):
    f32 = jnp.float32
    N, D = h.shape
    hf = h.astype(f32)
    g_logits = hf @ wg.astype(f32) + bg.astype(f32)
    g_prob = jax.nn.softmax(g_logits, axis=-1)
    g_sel = jnp.argmax(g_logits, axis=-1)
    g_w = jnp.take_along_axis(g_prob, g_sel[:, None], axis=-1)
    e_logits = (hf @ we.astype(f32) + be.astype(f32)).reshape(N, N_GROUPS, EXPERTS_PER_GROUP)
    e_logits = jnp.take_along_axis(e_logits, g_sel[:, None, None], axis=1)[:, 0]
    top_p, top_i = lax.top_k(jax.nn.softmax(e_logits, axis=-1), TOP_K)
    slot_w = (g_w * top_p / jnp.sum(top_p, axis=-1, keepdims=True)).reshape(-1)
    slot_e = (g_sel[:, None] * EXPERTS_PER_GROUP + top_i).reshape(-1)
    M = N * TOP_K
    slot_tok = jnp.repeat(jnp.arange(N), TOP_K)
    order = jnp.argsort(slot_e)
    se, st, sw = slot_e[order], slot_tok[order], slot_w[order]
    counts = jnp.bincount(slot_e, length=N_EXPERTS)
    padded = (counts + MOE_BLOCK - 1) // MOE_BLOCK * MOE_BLOCK
    pad_end = jnp.cumsum(padded)
    dest = (pad_end - padded)[se] + jnp.arange(M) - (jnp.cumsum(counts) - counts)[se]
    n_blocks = -(-M // MOE_BLOCK) + N_EXPERTS
    xs = jnp.zeros((n_blocks * MOE_BLOCK, D), h.dtype).at[dest].set(h[st])
    block_e = jnp.minimum(jnp.searchsorted(pad_end, jnp.arange(n_blocks) * MOE_BLOCK, side='right'), N_EXPERTS - 1)

    def expert_block(args):
        xb, e = args
        gate, up = jnp.split(xb @ w_gu[e], 2, axis=-1)
        return (jax.nn.silu(gate) * up) @ w_down[e]

    ys = lax.map(expert_block, (xs.reshape(n_blocks, MOE_BLOCK, D), block_e)).reshape(-1, D)
    out = jnp.zeros((N, D), f32).at[st].add(ys[dest].astype(f32) * sw[:, None])
    return out.astype(h.dtype)


def setup_inputs(seed: int = 0) -> dict:
    key = jax.random.key(seed)
    ks = jax.random.split(key, 40)
    f32 = jnp.float32
    D = D_MODEL

    def nrm(i, shape, scale):
        return scale * jax.random.normal(ks[i], shape, f32)

    i_bias = nrm(25, (DEPTH, 2, 1, ML_HEADS), 0.1)
    f_bias = jnp.linspace(3.0, 6.0, ML_HEADS, dtype=f32) + nrm(26, (DEPTH, 2, 1, ML_HEADS), 0.1)
    return {
        'x': nrm(0, (BATCH, SEQ, D), 1.0),
        'c': nrm(1, (BATCH, D), 1.0),
        'ctx': nrm(2, (BATCH, CTX_LEN, D), 1.0),
        'c_ctx': nrm(3, (D,), 1.0),
        'ada_w': nrm(4, (DEPTH, D, 6 * D), 0.3 * D ** -0.5),
        'ada_b': nrm(5, (DEPTH, 6 * D), 0.01),
        'norm1_g': 1.0 + nrm(6, (DEPTH, D), 0.02),
        'norm2_g': 1.0 + nrm(7, (DEPTH, D), 0.02),
        'w_in': nrm(8, (DEPTH, D, IN_COLS), D ** -0.5),
        'w_out': nrm(9, (DEPTH, MIX_WIDTH, D), MIX_WIDTH ** -0.5),
        'da_lambda': nrm(10, (DEPTH, 4, DA_QK_DIM), 0.1),
        'da_subln_g': 1.0 + nrm(11, (DEPTH, DA_V_DIM), 0.02),
        'rw_shift_mu': jax.random.uniform(ks[12], (DEPTH, RW_COLS), f32),
        'rw_w0': nrm(13, (DEPTH, 2, RW_WIDTH), 0.5),
        'rw_w2': nrm(14, (DEPTH, 2, RW_DECAY_RANK, RW_WIDTH), 0.1 * RW_DECAY_RANK ** -0.5),
        'rw_a0': nrm(15, (DEPTH, 2, RW_WIDTH), 0.5),
        'rw_a2': nrm(16, (DEPTH, 2, RW_ICL_RANK, RW_WIDTH), 0.1 * RW_ICL_RANK ** -0.5),
        'rw_g2': nrm(17, (DEPTH, RW_GATE_RANK, RW_WIDTH), RW_GATE_RANK ** -0.5),
        'rw_k_k': 0.85 + nrm(18, (DEPTH, RW_WIDTH), 0.05),
        'rw_k_a': 1.0 + nrm(19, (DEPTH, RW_WIDTH), 0.05),
        'rw_r_k': nrm(20, (DEPTH, RW_HEADS, RW_HEAD_DIM), 0.1),
        'rw_ln_g': 1.0 + nrm(21, (DEPTH, RW_WIDTH), 0.02),
        'rw_ln_b': nrm(22, (DEPTH, RW_WIDTH), 0.01),
        'ml_conv_w': nrm(23, (DEPTH, 3, 2 * ML_WIDTH), 3 ** -0.5),
        'ml_conv_b': nrm(24, (DEPTH, 2 * ML_WIDTH), 0.01),
        'ml_gate_b': jnp.concatenate([i_bias, f_bias], axis=2).reshape(DEPTH, 4 * ML_HEADS),
        'ml_norm_g': 1.0 + nrm(27, (DEPTH, ML_WIDTH), 0.02),
        'router_wg': nrm(28, (DEPTH, D, N_GROUPS), D ** -0.5),
        'router_bg': nrm(29, (DEPTH, N_GROUPS), 0.01),
        'router_we': nrm(30, (DEPTH, D, N_EXPERTS), D ** -0.5),
        'router_be': nrm(31, (DEPTH, N_EXPERTS), 0.01),
        'exp_w_gu': nrm(32, (DEPTH, N_EXPERTS, D, 2 * EXPERT_HIDDEN), D ** -0.5),
        'exp_w_down': nrm(33, (DEPTH, N_EXPERTS, EXPERT_HIDDEN, D), EXPERT_HIDDEN ** -0.5),
        'final_g': 1.0 + nrm(34, (D,), 0.02),
    }


def reference(x, c, ctx, c_ctx, ada_w, ada_b, norm1_g, norm2_g, w_in, w_out, da_lambda, da_subln_g,
              rw_shift_mu, rw_w0, rw_w2, rw_a0, rw_a2, rw_g2, rw_k_k, rw_k_a, rw_r_k, rw_ln_g, rw_ln_b,
              ml_conv_w, ml_conv_b, ml_gate_b, ml_norm_g, router_wg, router_bg, router_we, router_be,
              exp_w_gu, exp_w_down, final_g):
    f32 = jnp.float32
    B, T, D = x.shape
    Tc = ctx.shape[1]
    ROWS = T // GRID_W
    row = jnp.repeat(jnp.arange(ROWS, dtype=f32), GRID_W)
    col = jnp.tile(jnp.arange(GRID_W, dtype=f32), ROWS)
    rw_state0 = jnp.zeros((2, B, RW_HEADS, RW_HEAD_DIM, RW_HEAD_DIM), f32)
    ml_state0 = (jnp.zeros((2, B, ML_HEADS, ML_HEAD_DIM, ML_HEAD_DIM), f32),
                 jnp.zeros((2, B, ML_HEADS, ML_HEAD_DIM), f32),
                 jnp.zeros((2, B, ML_HEADS), f32))
    cs = ctx
    for l in range(DEPTH):
        last = l == DEPTH - 1
        lam_init = 0.8 - 0.6 * math.exp(-0.3 * l)
        mod_x = jax.nn.silu(c) @ ada_w[l] + ada_b[l]
        mod_c = jax.nn.silu(c_ctx) @ ada_w[l] + ada_b[l]
        sh1x, sc1x, g1x, sh2x, sc2x, g2x = [m[:, None, :] for m in jnp.split(mod_x, 6, axis=-1)]
        sh1c, sc1c, g1c, sh2c, sc2c, g2c = jnp.split(mod_c, 6, axis=-1)
        zx = (rms_norm(x, norm1_g[l]) * (1.0 + sc1x) + sh1x) @ w_in[l]
        zc = (rms_norm(cs, norm1_g[l]) * (1.0 + sc1c) + sh1c) @ w_in[l]

        lq1, lk1, lq2, lk2 = da_lambda[l].astype(f32)
        lam = jnp.exp(jnp.sum(lq1 * lk1)) - jnp.exp(jnp.sum(lq2 * lk2)) + lam_init
        qc, kc, vc = da_heads(zc[..., :DA_COLS])
        qx, kx, vx = da_heads(zx[..., :DA_COLS])
        k_all = jnp.concatenate([kc, axial_rope(kx, row, col)], axis=1)
        v_all = jnp.concatenate([vc, vx], axis=1)
        att_x = da_post(diff_attention(axial_rope(qx, row, col), k_all, v_all, lam), da_subln_g[l], lam_init)

        rw_args = (rw_shift_mu[l], rw_w0[l], rw_w2[l], rw_a0[l], rw_a2[l], rw_g2[l],
                   rw_k_k[l], rw_k_a[l], rw_r_k[l], rw_ln_g[l], rw_ln_b[l])
        rw_c, rw_state = rwkv_group(zc[..., DA_COLS:DA_COLS + RW_COLS], rw_state0, *rw_args)
        rw_x, _ = rwkv_group(zx[..., DA_COLS:DA_COLS + RW_COLS], rw_state, *rw_args)

        ml_args = (ml_conv_w[l], ml_conv_b[l], ml_gate_b[l], ml_norm_g[l])
        ml_c, ml_state = mlstm_group(zc[..., DA_COLS + RW_COLS:], ml_state0, *ml_args)
        ml_x, _ = mlstm_group(zx[..., DA_COLS + RW_COLS:], ml_state, *ml_args)

        x = x + g1x * (jnp.concatenate([att_x, rw_x, ml_x], axis=-1).astype(x.dtype) @ w_out[l])
        moe_args = (router_wg[l], router_bg[l], router_we[l], router_be[l], exp_w_gu[l], exp_w_down[l])
        h2x = rms_norm(x, norm2_g[l]) * (1.0 + sc2x) + sh2x
        if last:
            x = x + g2x * hier_moe(h2x.reshape(B * T, D), *moe_args).reshape(B, T, D)
        else:
            att_c = da_post(diff_attention(qc, kc, vc, lam), da_subln_g[l], lam_init)
            cs = cs + g1c * (jnp.concatenate([att_c, rw_c, ml_c], axis=-1).astype(cs.dtype) @ w_out[l])
            h2c = rms_norm(cs, norm2_g[l]) * (1.0 + sc2c) + sh2c
            y = hier_moe(jnp.concatenate([h2c.reshape(B * Tc, D), h2x.reshape(B * T, D)], axis=0), *moe_args)
            cs = cs + g2c * y[:B * Tc].reshape(B, Tc, D)
            x = x + g2x * y[B * Tc:].reshape(B, T, D)
    return rms_norm(x, final_g)
```

```python
import math
import os
DBG_STOP = int(os.environ.get('DBG_STOP', '99'))
DBG_SKIP = os.environ.get('DBG_SKIP', '')
DSTOP = int(os.environ.get('DSTOP', '99'))
SSTOP = int(os.environ.get('SSTOP', '99'))
NSTEP = int(os.environ.get('NSTEP', '68'))
import numpy as np
from contextlib import ExitStack
import concourse.bass as bass
import concourse.mybir as mybir
from concourse.alu_op_type import AluOpType as ALU
from concourse.bass_utils import run_bass_kernel_spmd

AF = mybir.ActivationFunctionType
AX = mybir.AxisListType
F32 = mybir.dt.float32
BF16 = mybir.dt.bfloat16
I32 = mybir.dt.int32
U32 = mybir.dt.uint32

D = 1024
T = 4096
TC = 256
NT = T + TC
NTILE = NT // 128
DEPTH = 4
IN_COLS = 3536
DA0, RW0, ML0 = 0, 1536, 2496
EPS = 1e-6
BLOCKS = [(0, 256)] + [(256 + 512 * i, 512) for i in range(8)]
NPAD = 4356


def padcol(t):
    return t + 1 if t < 256 else t + 3


class Buf:
    def __init__(self, t, name=""):
        self.t = t
        self.name = name
        self.w = {}
        self.r = {}

    def __getitem__(self, idx):
        return self.t[idx]


class Ctx:
    NPOOL = 12

    def __init__(self, nc, stack, same_engine_sync=True):
        self.nc = nc
        self.st = stack
        self.same = same_engine_sync
        self.eng = dict(pe=nc.tensor, dve=nc.vector, act=nc.scalar, pool=nc.gpsimd, sp=nc.sync)
        self.semh = {}
        self.cnt = {}
        for e in self.eng:
            self.semh[e] = stack.enter_context(nc.semaphore("s_" + e))
            self.cnt[e] = 0
        self.seen = {e: {} for e in self.eng}
        self.dq = {}
        for q in ("sp", "pool", "act"):
            lst = []
            for i in range(self.NPOOL):
                key = "d_%s_%d" % (q, i)
                self.semh[key] = stack.enter_context(nc.semaphore(key))
                self.cnt[key] = 0
                lst.append(key)
            self.dq[q] = [lst, 0]
        self.ninstr = 0
        self.rr = {}

    def sbuf(self, name, shape, dt):
        self.uid = getattr(self, "uid", 0) + 1
        return Buf(self.st.enter_context(self.nc.sbuf_tensor("sb%d_%s" % (self.uid, name), list(shape), dt)), name)

    def psum(self, name, shape, dt=F32):
        b = Buf(self.st.enter_context(self.nc.psum_tensor("pp_" + name, list(shape), dt)), name)
        b.psum = True
        return b

    def dram(self, name, shape, dt, kind="Internal"):
        return Buf(self.nc.dram_tensor(name, list(shape), dt, kind=kind), name)

    def _wait(self, e, deps):
        eng = self.eng[e]
        seen = self.seen[e]
        for key, val in deps.items():
            if key == e and (e == "pe" or not self.same):
                continue
            if seen.get(key, 0) >= val:
                continue
            eng.wait_ge(self.semh[key], val)
            seen[key] = val

    @staticmethod
    def _merge(d, s):
        for k, v in s.items():
            if d.get(k, 0) < v:
                d[k] = v

    def _deps(self, reads, writes):
        deps = {}
        for b in reads:
            self._merge(deps, b.w)
            if getattr(b, "psum", False):
                self._merge(deps, b.r)
        for b in writes:
            self._merge(deps, b.w)
            self._merge(deps, b.r)
        return deps

    def _mark(self, key, val, reads, writes):
        for b in reads:
            if b.r.get(key, 0) < val:
                b.r[key] = val
        for b in writes:
            if b.w.get(key, 0) < val:
                b.w[key] = val

    def op(self, e, fn, reads=(), writes=()):
        self._wait(e, self._deps(reads, writes))
        ins = fn(self.eng[e])
        self.cnt[e] += 1
        ins.then_inc(self.semh[e], 1)
        self._mark(e, self.cnt[e], reads, writes)
        self.ninstr += 1
        return ins

    def dma(self, q, out, in_, reads=(), writes=(), **kw):
        lst, rr = self.dq[q]
        key = lst[rr % len(lst)]
        self.dq[q][1] = rr + 1
        deps = self._deps(reads, writes)
        deps[key] = max(deps.get(key, 0), self.cnt[key])
        self._wait(q, deps)
        ins = self.eng[q].dma_start(out=out, in_=in_, **kw)
        self.cnt[key] += 16
        ins.then_inc(self.semh[key], 16)
        self._mark(key, self.cnt[key], reads, writes)
        self.ninstr += 1
        return ins

    def finish(self, e="sp"):
        deps = {}
        for q in self.dq:
            for key in self.dq[q][0]:
                if self.cnt[key]:
                    deps[key] = self.cnt[key]
        for k in self.eng:
            if self.cnt[k]:
                deps[k] = self.cnt[k]
        self._wait(e, deps)

    def barrier(self):
        deps = {}
        for q in self.dq:
            for key in self.dq[q][0]:
                if self.cnt[key]:
                    deps[key] = self.cnt[key]
        for e in self.eng:
            if self.cnt[e]:
                deps[e] = self.cnt[e]
        for e in self.eng:
            self._wait(e, deps)

    def scope(self):
        ctx = self

        class _S:
            def __enter__(s_):
                s_.old = ctx.st
                s_.sub = ExitStack()
                ctx.st = s_.sub
                return s_

            def __exit__(s_, *a):
                if a[0] is None:
                    ctx.barrier()
                s_.sub.close()
                ctx.st = s_.old
                return False
        return _S()

    def nxt(self, name, lst):
        i = self.rr.get(name, 0)
        self.rr[name] = i + 1
        return lst[i % len(lst)]


def rope_tables():
    nf = 16
    inv = 10000.0 ** (-np.arange(nf, dtype=np.float32) / nf)
    t = np.arange(T)
    row = (t // 64).astype(np.float32)
    col = (t % 64).astype(np.float32)
    cos = np.ones((128, NT), np.float32)
    sin = np.zeros((128, NT), np.float32)
    for p in range(128):
        d = p % 64
        pos = row if d < 32 else col
        j = d % 32
        f = j % 16
        ang = pos * inv[f]
        cos[p, TC:] = np.cos(ang)
        s = np.sin(ang)
        sin[p, TC:] = -s if j < 16 else s
    return cos, sin


def rope_partner_perm():
    perm = np.zeros(64, np.int64)
    for d in range(64):
        j = d % 32
        perm[d] = d + 16 if j < 16 else d - 16
    return perm


def build(n_layers=DEPTH, stage=99, dbg=()):
    nc = bass.Bass("TRN2", target_bir_lowering=False)
    st = ExitStack()
    with st:
        k = Ctx(nc, st)
        build_body(nc, k, n_layers, stage, dbg)
        k.finish()
        print("instructions:", k.ninstr, {e: k.cnt[e] for e in k.eng})
    return nc


def build_body(nc, k, n_layers, stage, dbg):
    L = DEPTH
    ein = lambda name, shape, dt=F32: k.dram(name, shape, dt, kind="ExternalInput")
    x_in = ein("x", [T, D])
    ctx_in = ein("ctx", [TC, D])
    c2T_in = ein("c2T", [128, 8, 2])
    ada_w = ein("ada_w", [L, D, 6 * D])
    ada_bT = ein("ada_bT", [L, 128, 48])
    n1gT = ein("n1gT", [L, 128, 8])
    n2gT = ein("n2gT", [L, 128, 8])
    w_in = ein("w_in", [L, D, IN_COLS])
    w_inp = ein("w_inp", [L, D, 1024])
    w_out = ein("w_out", [L, D, D])
    cos_in = ein("cos_t", [128, NT])
    sin_in = ein("sin_t", [128, NT])
    ident_in = ein("ident", [128, 128])
    fgT = ein("fgT", [128, 8])
    out = k.dram("out", [T, D], F32, kind="ExternalOutput")
    dbg_out = {}
    for name, shape in dbg:
        dbg_out[name] = k.dram("dbg_" + name, shape, F32, kind="ExternalOutput")

    XT = k.dram("XT", [D, NT], F32)
    QT = k.dram("QT", [4, 128, NT], BF16)
    KT = k.dram("KT", [4, 128, NT], BF16)
    VA = k.dram("VA", [NT, 4 * 130], BF16)
    MIXT = k.dram("MIXT", [D, NT], BF16)

    ident = k.sbuf("ident", [128, 128], F32)
    identb = k.sbuf("identb", [128, 128], BF16)
    ones_f = k.sbuf("ones_f", [128, 128], F32)
    k.dma("sp", ident[:], ident_in[:], reads=[ident_in], writes=[ident])
    k.op("dve", lambda e: e.tensor_copy(out=identb[:], in_=ident[:]), reads=[ident], writes=[identb])
    k.op("dve", lambda e: e.memset(ones_f[:], 1.0), writes=[ones_f])

    PS = [k.psum("ps%d" % i, [128, 512], F32) for i in range(8)]

    def ps():
        return k.nxt("ps", PS)

    c2T = k.sbuf("c2T", [128, 8, 2], F32)
    sc2T = k.sbuf("sc2T", [128, 8, 2], F32)
    k.dma("sp", c2T[:], c2T_in[:], reads=[c2T_in], writes=[c2T])
    k.op("act", lambda e: e.activation(out=sc2T[:], in_=c2T[:], func=AF.Silu), reads=[c2T], writes=[sc2T])
    modT = [k.sbuf("modT%d" % l, [128, 48, 2], F32) for l in range(L)]
    adab = [k.sbuf("adab%d" % l, [128, 48], F32) for l in range(L)]
    n1g = [k.sbuf("n1g%d" % l, [128, 8], F32) for l in range(L)]
    n2g = [k.sbuf("n2g%d" % l, [128, 8], F32) for l in range(L)]
    A1 = [k.sbuf("A1_%d" % l, [128, 8, 2], F32) for l in range(L)]
    A2 = [k.sbuf("A2_%d" % l, [128, 8, 2], F32) for l in range(L)]
    fg = k.sbuf("fg", [128, 8], F32)
    k.dma("sp", fg[:], fgT[:], reads=[fgT], writes=[fg])
    with k.scope():
        adaw_sb = [k.sbuf("adaw%d" % i, [128, 8, 768], F32) for i in range(2)]
        for l in range(n_layers):
            k.dma("sp", adab[l][:], ada_bT[l], reads=[ada_bT], writes=[adab[l]])
            k.dma("sp", n1g[l][:], n1gT[l], reads=[n1gT], writes=[n1g[l]])
            k.dma("sp", n2g[l][:], n2gT[l], reads=[n2gT], writes=[n2g[l]])
            for piece in range(8):
                wsb = k.nxt("adaw", adaw_sb)
                for kc in range(8):
                    k.dma("sp" if kc % 2 == 0 else "pool", wsb[:, kc, :],
                          ada_w[l][kc * 128:(kc + 1) * 128, piece * 768:(piece + 1) * 768],
                          reads=[ada_w], writes=[wsb])
                p = ps()
                for cc in range(6):
                    for kc in range(8):
                        k.op("pe", lambda e, cc=cc, kc=kc: e.matmul(
                            p[:, cc * 2:(cc + 1) * 2], wsb[:, kc, cc * 128:(cc + 1) * 128], sc2T[:, kc, :],
                            start=(kc == 0), stop=(kc == 7)), reads=[wsb, sc2T], writes=[p])
                k.op("dve", lambda e, piece=piece: e.tensor_tensor(
                    out=modT[l][:, piece * 6:(piece + 1) * 6, :],
                    in0=p[:, 0:12].rearrange("p (c s) -> p c s", s=2),
                    in1=adab[l][:, piece * 6:(piece + 1) * 6].unsqueeze(2).to_broadcast([128, 6, 2]),
                    op=ALU.add), reads=[p, adab[l]], writes=[modT[l]])
            for (A, g, c0) in ((A1[l], n1g[l], 8), (A2[l], n2g[l], 32)):
                k.op("dve", lambda e, A=A, g=g, c0=c0: e.scalar_tensor_tensor(
                    out=A[:], in0=modT[l][:, c0:c0 + 8, :], scalar=1.0,
                    in1=g[:].unsqueeze(2).to_broadcast([128, 8, 2]), op0=ALU.add, op1=ALU.mult),
                    reads=[modT[l], g], writes=[A])
        if "modT" in dbg_out:
            k.dma("sp", dbg_out["modT"][:], modT[0][:], reads=[modT[0]], writes=[dbg_out["modT"]])

        xin_sb = [k.sbuf("xin%d" % i, [128, D], F32) for i in range(2)]
        xtr_sb = [k.sbuf("xtr%d" % i, [128, 8, 128], F32) for i in range(2)]
        XTv = XT.t.ap().rearrange("(kc p) t -> p kc t", p=128)
        for tt in range(NTILE):
            xs = k.nxt("xin", xin_sb)
            src = ctx_in[tt * 128:(tt + 1) * 128, :] if tt < 2 else x_in[(tt - 2) * 128:(tt - 1) * 128, :]
            k.dma("sp" if tt % 2 == 0 else "pool", xs[:], src, reads=[], writes=[xs])
            xo = k.nxt("xtr", xtr_sb)
            for half in range(2):
                p = ps()
                for j in range(4):
                    kc = half * 4 + j
                    k.op("pe", lambda e, kc=kc, j=j: e.transpose(p[:, j * 128:(j + 1) * 128], xs[:, kc * 128:(kc + 1) * 128], ident[:]),
                         reads=[xs, ident], writes=[p])
                k.op("act" if half == 0 else "dve",
                     (lambda e, half=half: e.copy(out=xo[:, half * 4:(half + 1) * 4, :], in_=p[:, :].rearrange("p (j t) -> p j t", t=128))) if half == 0 else
                     (lambda e, half=half: e.tensor_copy(out=xo[:, half * 4:(half + 1) * 4, :], in_=p[:, :].rearrange("p (j t) -> p j t", t=128))),
                     reads=[p], writes=[xo])
            k.dma("sp", XTv[:, :, tt * 128:(tt + 1) * 128], xo[:], reads=[xo], writes=[XT])

    da_lam_in = ein("da_lambda", [L, 256])
    sublnT_in = ein("sublnT", [128, L])
    RZT = k.dram("RZT", [960, NPAD], F32)
    MQKT = k.dram("MQKT", [512, NPAD], F32)
    MOT = k.dram("MOT", [256, NT], F32)
    MVT = k.dram("MVT", [256, NT], F32)
    MGT = k.dram("MGT", [16, NT], F32)
    HFB = [k.dram("HF", [NT, 256], F32), k.dram("HB", [NT, 256], F32)]
    zero_sb = k.sbuf("zero_sb", [128, 8], F32)
    k.op("dve", lambda e: e.memset(zero_sb[:], 0.0), writes=[zero_sb])
    for (dst, rows) in (() if 'z' in DBG_SKIP else ((RZT, 960), (MQKT, 512))):
        for r0 in range(0, rows, 128):
            nr = min(128, rows - r0)
            for c0, w in ((0, 1), (257, 2), (4355, 1)):
                k.dma("sp", dst[r0:r0 + nr, c0:c0 + w], zero_sb[0:nr, 0:w], reads=[zero_sb], writes=[dst], allow_slow_non_contiguous=True)
    sublnT = k.sbuf("sublnT", [128, L], F32)
    k.dma("sp", sublnT[:], sublnT_in[:], reads=[sublnT_in], writes=[sublnT])

    def phase_A(l):
        WI = k.sbuf("WI", [128, 8, IN_COLS], BF16)
        WIP = k.sbuf("WIP", [128, 8, 1024], BF16)
        pa_x = [k.sbuf("pa_x%d" % i, [128, 8, 512], F32) for i in range(2)]
        pa_sq = k.sbuf("pa_sq", [128, 8, 512], F32)
        pa_r = k.sbuf("pa_r", [128, 512], F32)
        pa_tmp = [k.sbuf("pa_tmp%d" % i, [128, 512], F32) for i in range(2)]
        pa_h = k.sbuf("pa_h", [128, 8, 512], BF16)
        pa_cos = k.sbuf("pa_cos", [128, 512], F32)
        pa_sin = k.sbuf("pa_sin", [128, 512], F32)
        pa_t1 = [k.sbuf("pa_t1_%d" % i, [128, 512], F32) for i in range(2)]
        pa_t2 = [k.sbuf("pa_t2_%d" % i, [128, 512], F32) for i in range(2)]
        pa_ob = [k.sbuf("pa_ob%d" % i, [128, 512], BF16) for i in range(3)]
        pa_of = [k.sbuf("pa_of%d" % i, [128, 512], F32) for i in range(3)]
        pa_va = [k.sbuf("pa_va%d" % i, [128, 4, 130], BF16) for i in range(2)]
        for b_ in ([] if 'm' in DBG_SKIP else pa_va):
            k.op("pool", lambda e, b_=b_: e.memset(b_[:], 1.0), writes=[b_])

        evac_rr = [0]

        def evac_copy(dst_ap, src_ap, reads, writes):
            evac_rr[0] += 1
            if evac_rr[0] % 2 == 0:
                k.op("act", lambda e: e.copy(out=dst_ap, in_=src_ap), reads=reads, writes=writes)
            else:
                k.op("dve", lambda e: e.tensor_copy(out=dst_ap, in_=src_ap), reads=reads, writes=writes)

        for kc in range(0 if 'w' in DBG_SKIP else 8):
            for c0 in range(0, IN_COLS, 1768):
                k.dma("pool", WI[:, kc, c0:c0 + 1768], w_in[l][kc * 128:(kc + 1) * 128, c0:c0 + 1768], reads=[w_in], writes=[WI])
            k.dma("pool", WIP[:, kc, :], w_inp[l][kc * 128:(kc + 1) * 128, :], reads=[w_inp], writes=[WIP])
        if DBG_STOP <= 1:
            return
        for (t0, n) in BLOCKS:
            s = 1 if t0 == 0 else 0
            xb = k.nxt("pa_x", pa_x)
            k.dma("sp", xb[:, :, 0:n], XTv[:, :, t0:t0 + n], reads=[XT], writes=[xb])
            k.dma("sp", pa_cos[:, 0:n], cos_in[:, t0:t0 + n], reads=[cos_in], writes=[pa_cos])
            k.dma("sp", pa_sin[:, 0:n], sin_in[:, t0:t0 + n], reads=[sin_in], writes=[pa_sin])
            k.op("act", lambda e: e.activation(out=pa_sq[:, :, 0:n], in_=xb[:, :, 0:n], func=AF.Square), reads=[xb], writes=[pa_sq])
            p = ps()
            for kc in range(8):
                k.op("pe", lambda e, kc=kc: e.matmul(p[:, 0:n], ones_f[:], pa_sq[:, kc, 0:n], start=(kc == 0), stop=(kc == 7)),
                     reads=[ones_f, pa_sq], writes=[p])
            k.op("act", lambda e: e.activation(out=pa_r[:, 0:n], in_=p[:, 0:n], func=AF.Sqrt, scale=1.0 / D, bias=EPS), reads=[p], writes=[pa_r])
            k.op("dve", lambda e: e.reciprocal(out=pa_r[:, 0:n], in_=pa_r[:, 0:n]), reads=[pa_r], writes=[pa_r])
            for kc in range(8):
                tmp = k.nxt("pa_tmp", pa_tmp)
                k.op("dve", lambda e, kc=kc: e.tensor_tensor(out=tmp[:, 0:n], in0=xb[:, kc, 0:n], in1=pa_r[:, 0:n], op=ALU.mult),
                     reads=[xb, pa_r], writes=[tmp])
                k.op("act", lambda e, kc=kc: e.activation(out=pa_h[:, kc, 0:n], in_=tmp[:, 0:n], func=AF.Identity,
                                                          scale=A1[l][:, kc, s:s + 1], bias=modT[l][:, kc, s:s + 1]),
                     reads=[tmp, A1[l], modT[l]], writes=[pa_h])

            if DBG_STOP <= 2:
                continue

            def fm(Wb, c0, ncols=128):
                p = ps()
                for kc in range(8):
                    k.op("pe", lambda e, kc=kc: e.matmul(p[0:ncols, 0:n], Wb[:, kc, c0:c0 + ncols], pa_h[:, kc, 0:n], start=(kc == 0), stop=(kc == 7)),
                         reads=[Wb, pa_h], writes=[p])
                return p

            for which, dst in ((0, QT), (1, KT)):
                for h in range(4):
                    c0 = which * 512 + h * 128
                    p1 = fm(WI, c0)
                    p2 = fm(WIP, c0)
                    t1 = k.nxt("pa_t1", pa_t1)
                    t2 = k.nxt("pa_t2", pa_t2)
                    ob = k.nxt("pa_ob", pa_ob)
                    k.op("dve", lambda e: e.tensor_tensor(out=t1[:, 0:n], in0=p1[:, 0:n], in1=pa_cos[:, 0:n], op=ALU.mult), reads=[p1, pa_cos], writes=[t1])
                    k.op("dve", lambda e: e.tensor_tensor(out=t2[:, 0:n], in0=p2[:, 0:n], in1=pa_sin[:, 0:n], op=ALU.mult), reads=[p2, pa_sin], writes=[t2])
                    k.op("pool", lambda e: e.tensor_tensor(out=ob[:, 0:n], in0=t1[:, 0:n], in1=t2[:, 0:n], op=ALU.add), reads=[t1, t2], writes=[ob])
                    k.dma("sp", dst[h][:, t0:t0 + n], ob[:, 0:n], reads=[ob], writes=[dst])
            if DBG_STOP <= 3:
                continue
            pc = padcol(t0)
            for j in range(8):
                ncols = 128 if j < 7 else 64
                p1 = fm(WI, RW0 + j * 128, ncols)
                of = k.nxt("pa_of", pa_of)
                evac_copy(of[0:ncols, 0:n], p1[0:ncols, 0:n], [p1], [of])
                k.dma("sp", RZT[j * 128:j * 128 + ncols, pc:pc + n], of[0:ncols, 0:n], reads=[of], writes=[RZT])
            for j in range(4):
                p1 = fm(WI, ML0 + j * 128)
                of = k.nxt("pa_of", pa_of)
                evac_copy(of[:, 0:n], p1[:, 0:n], [p1], [of])
                k.dma("sp", MQKT[j * 128:(j + 1) * 128, pc:pc + n], of[:, 0:n], reads=[of], writes=[MQKT])
            for j in range(2):
                p1 = fm(WI, ML0 + 768 + j * 128)
                of = k.nxt("pa_of", pa_of)
                k.op("act", lambda e: e.activation(out=of[:, 0:n], in_=p1[:, 0:n], func=AF.Sigmoid), reads=[p1], writes=[of])
                k.dma("sp", MOT[j * 128:(j + 1) * 128, t0:t0 + n], of[:, 0:n], reads=[of], writes=[MOT])
            if DBG_STOP <= 4:
                continue
            for j in range(n // 128):
                r0 = t0 + j * 128
                p1 = ps()
                for kc in range(8):
                    k.op("pe", lambda e, kc=kc: e.matmul(p1[:, 0:512], pa_h[:, kc, j * 128:(j + 1) * 128], WI[:, kc, 1024:1536], start=(kc == 0), stop=(kc == 7)),
                         reads=[WI, pa_h], writes=[p1])
                va = k.nxt("pa_va", pa_va)
                evac_copy(va[:, :, 0:128], p1[:, 0:512].rearrange("p (h e) -> p h e", e=128), [p1], [va])
                k.dma("sp", VA[r0:r0 + 128, :], va[:].rearrange("p h e -> p (h e)"), reads=[va], writes=[VA])
            for j in range(2):
                p1 = fm(WI, ML0 + 512 + j * 128)
                of = k.nxt("pa_of", pa_of)
                evac_copy(of[:, 0:n], p1[:, 0:n], [p1], [of])
                k.dma("sp", MVT[j * 128:(j + 1) * 128, t0:t0 + n], of[:, 0:n], reads=[of], writes=[MVT])
            p1 = fm(WI, ML0 + 1024, 16)
            of = k.nxt("pa_of", pa_of)
            evac_copy(of[0:16, 0:n], p1[0:16, 0:n], [p1], [of])
            k.dma("sp", MGT[:, t0:t0 + n], of[0:16, 0:n], reads=[of], writes=[MGT])

    def phase_B(l):
        at_k = k.sbuf("at_k", [128, NT], BF16)
        at_q = k.sbuf("at_q", [128, NT], BF16)
        at_v = k.sbuf("at_v", [128, NTILE, 130], BF16)
        at_p = [k.sbuf("at_p%d" % i, [128, 512], BF16) for i in range(3)]
        at_o = [k.sbuf("at_o%d" % i, [128, 4, 128], F32) for i in range(2)]
        at_a = k.sbuf("at_a", [128, 4, 128], F32)
        at_sq = k.sbuf("at_sq", [128, 4, 128], F32)
        at_ss = k.sbuf("at_ss", [128, 4], F32)
        at_rec = k.sbuf("at_rec", [128, 4], F32)
        at_ob = [k.sbuf("at_ob%d" % i, [128, 512], BF16) for i in range(2)]
        dl = k.sbuf("dl", [128, 256], F32)
        dl_t = k.sbuf("dl_t", [128, 2, 64], F32)
        dl_s = k.sbuf("dl_s", [128, 2], F32)
        neglam = k.sbuf("neglam", [128, 1], F32)
        subg = k.sbuf("subg", [128, 1], F32)
        ACC = [k.psum("acc%d" % i, [128, 512], F32) for i in range(0)]

        lam_init = 0.8 - 0.6 * math.exp(-0.3 * l)
        k.dma("sp", dl[:], da_lam_in[l].partition_broadcast(128), reads=[da_lam_in], writes=[dl])
        dl4 = dl[:, :].rearrange("p (a b d) -> p a b d", a=2, b=2)
        k.op("dve", lambda e: e.tensor_tensor(out=dl_t[:], in0=dl4[:, :, 0, :], in1=dl4[:, :, 1, :], op=ALU.mult), reads=[dl], writes=[dl_t])
        k.op("dve", lambda e: e.tensor_reduce(out=dl_s[:], in_=dl_t[:], axis=AX.X, op=ALU.add), reads=[dl_t], writes=[dl_s])
        k.op("act", lambda e: e.activation(out=dl_s[:], in_=dl_s[:], func=AF.Exp), reads=[dl_s], writes=[dl_s])
        k.op("dve", lambda e: e.scalar_tensor_tensor(out=neglam[:], in0=dl_s[:, 1:2], scalar=-lam_init, in1=dl_s[:, 0:1], op0=ALU.add, op1=ALU.subtract),
             reads=[dl_s], writes=[neglam])
        k.op("dve", lambda e: e.tensor_scalar(out=subg[:], in0=sublnT[:, l:l + 1], scalar1=(1.0 - lam_init), scalar2=None, op0=ALU.mult),
             reads=[sublnT], writes=[subg])
        qsets = [(256 + 512 * i, 512, list(range(NTILE))) for i in range(8)]
        if l < DEPTH - 1:
            qsets = [(0, 256, [0, 1])] + qsets
        for h in range(4):
            k.dma("sp", at_k[:], KT[h], reads=[KT], writes=[at_k])
            k.dma("pool", at_q[:], QT[h], reads=[QT], writes=[at_q])
            k.dma("sp", at_v[:], VA.t.ap().rearrange("(t p) (h e) -> p t h e", p=128, e=130)[:, :, h, :], reads=[VA], writes=[at_v])
            for (q0, nq, kts) in qsets:
                nj = nq // 128
                for m in range(2):
                    accs = PS[0:4]
                    osb = at_o[m]
                    for ki, kt in enumerate(kts):
                        sp_ = k.nxt("psB", PS[4:8])
                        k.op("pe", lambda e: e.matmul(sp_[:, 0:nq], at_k[m * 64:(m + 1) * 64, kt * 128:(kt + 1) * 128], at_q[m * 64:(m + 1) * 64, q0:q0 + nq],
                                                      start=True, stop=True), reads=[at_k, at_q], writes=[sp_])
                        pt = k.nxt("at_p", at_p)
                        k.op("act", lambda e: e.activation(out=pt[:, 0:nq], in_=sp_[:, 0:nq], func=AF.Exp, scale=0.125), reads=[sp_], writes=[pt])
                        for j in range(nj):
                            acc = accs[j]
                            k.op("pe", lambda e, j=j: e.matmul(acc[:, 0:129], pt[:, j * 128:(j + 1) * 128], at_v[:, kt, 0:129],
                                                               start=(ki == 0), stop=(ki == len(kts) - 1)), reads=[pt, at_v], writes=[acc])
                    for j in range(nj):
                        acc = accs[j]
                        c0 = 0
                        k.op("dve", lambda e, j=j: e.reciprocal(out=at_rec[:, j:j + 1], in_=acc[:, c0 + 128:c0 + 129]), reads=[acc], writes=[at_rec])
                        k.op("dve", lambda e, j=j: e.tensor_scalar(out=osb[:, j, :], in0=acc[:, c0:c0 + 128], scalar1=at_rec[:, j:j + 1], scalar2=None, op0=ALU.mult),
                             reads=[acc, at_rec], writes=[osb])
                k.op("dve", lambda e: e.scalar_tensor_tensor(out=at_a[:, 0:nj, :], in0=at_o[1][:, 0:nj, :], scalar=neglam[:, 0:1], in1=at_o[0][:, 0:nj, :],
                                                             op0=ALU.mult, op1=ALU.add), reads=[at_o[0], at_o[1], neglam], writes=[at_a])
                k.op("pool", lambda e: e.tensor_tensor(out=at_sq[:, 0:nj, :], in0=at_a[:, 0:nj, :], in1=at_a[:, 0:nj, :], op=ALU.mult), reads=[at_a], writes=[at_sq])
                k.op("dve", lambda e: e.tensor_reduce(out=at_ss[:, 0:nj], in_=at_sq[:, 0:nj, :], axis=AX.X, op=ALU.add), reads=[at_sq], writes=[at_ss])
                k.op("act", lambda e: e.activation(out=at_ss[:, 0:nj], in_=at_ss[:, 0:nj], func=AF.Sqrt, scale=1.0 / 128, bias=EPS), reads=[at_ss], writes=[at_ss])
                k.op("dve", lambda e: e.reciprocal(out=at_ss[:, 0:nj], in_=at_ss[:, 0:nj]), reads=[at_ss], writes=[at_ss])
                k.op("dve", lambda e: e.tensor_tensor(out=at_a[:, 0:nj, :], in0=at_a[:, 0:nj, :], in1=at_ss[:, 0:nj].unsqueeze(2).to_broadcast([128, nj, 128]), op=ALU.mult),
                     reads=[at_a, at_ss], writes=[at_a])
                pt_ = k.nxt("psB", PS[4:8])
                for j in range(nj):
                    k.op("pe", lambda e, j=j: e.transpose(pt_[:, j * 128:(j + 1) * 128], at_a[:, j, :], ident[:]), reads=[at_a, ident], writes=[pt_])
                ob = k.nxt("at_ob", at_ob)
                k.op("act", lambda e: e.activation(out=ob[:, 0:nq], in_=pt_[:, 0:nq], func=AF.Identity, scale=subg[:, 0:1]), reads=[pt_, subg], writes=[ob])
                k.dma("sp", MIXT[h * 128:(h + 1) * 128, q0:q0 + nq], ob[:, 0:nq], reads=[ob], writes=[MIXT])

    mcw_in = ein("mcwT", [L, 128, 4, 3])
    mcb_in = ein("mcbT", [L, 128, 4])
    mgb_in = ein("mgbT", [L, 16, 1])
    mng_in = ein("mngT", [L, 128, 2])
    triu_in = ein("triu", [128, 128])
    tril_in = ein("tril", [128, 128])
    triu = k.sbuf("triu", [128, 128], F32)
    tril = k.sbuf("tril", [128, 128], F32)
    k.dma("sp", triu[:], triu_in[:], reads=[triu_in], writes=[triu])
    k.dma("sp", tril[:], tril_in[:], reads=[tril_in], writes=[tril])
    ORDER = [list(range(NTILE)), [1, 0] + list(range(NTILE - 1, 1, -1))]

    def phase_D(l):
        mcw = k.sbuf("mcw", [128, 4, 3], F32)
        mcb = k.sbuf("mcb", [128, 4], F32)
        mgb = k.sbuf("mgb", [16, 1], F32)
        mng = k.sbuf("mng", [128, 2], F32)
        k.dma("sp", mcw[:], mcw_in[l], reads=[mcw_in], writes=[mcw])
        k.dma("sp", mcb[:], mcb_in[l], reads=[mcb_in], writes=[mcb])
        k.dma("sp", mgb[:], mgb_in[l], reads=[mgb_in], writes=[mgb])
        k.dma("sp", mng[:], mng_in[l], reads=[mng_in], writes=[mng])
        mq = k.sbuf("mq_all", [128, 4, NT], BF16)
        zc = [k.sbuf("md_z%d" % i, [128, 514], F32) for i in range(2)]
        ac = [k.sbuf("md_a%d" % i, [128, 512], F32) for i in range(2)]
        for c in range(4):
            for (t0, n) in BLOCKS:
                z = k.nxt("md_z", zc)
                a = k.nxt("md_a", ac)
                pc = padcol(t0)
                k.dma("sp", z[:, 0:n + 2], MQKT[c * 128:(c + 1) * 128, pc - 1:pc + n + 1], reads=[MQKT], writes=[z])
                k.op("dve", lambda e: e.tensor_scalar(out=a[:, 0:n], in0=z[:, 0:n], scalar1=mcw[:, c, 0:1], scalar2=None, op0=ALU.mult), reads=[z, mcw], writes=[a])
                k.op("dve", lambda e: e.scalar_tensor_tensor(out=a[:, 0:n], in0=z[:, 1:n + 1], scalar=mcw[:, c, 1:2], in1=a[:, 0:n], op0=ALU.mult, op1=ALU.add), reads=[z, mcw, a], writes=[a])
                k.op("dve", lambda e: e.scalar_tensor_tensor(out=a[:, 0:n], in0=z[:, 2:n + 2], scalar=mcw[:, c, 2:3], in1=a[:, 0:n], op0=ALU.mult, op1=ALU.add), reads=[z, mcw, a], writes=[a])
                if c < 2:
                    k.op("act", lambda e: e.activation(out=mq[:, c, t0:t0 + n], in_=a[:, 0:n], func=AF.Silu, bias=mcb[:, c:c + 1], scale=1.0), reads=[a, mcb], writes=[mq])
                else:
                    k.op("act", lambda e: e.activation(out=a[:, 0:n], in_=a[:, 0:n], func=AF.Silu, bias=mcb[:, c:c + 1], scale=1.0), reads=[a, mcb], writes=[a])
                    k.op("dve", lambda e: e.tensor_scalar(out=mq[:, c, t0:t0 + n], in0=a[:, 0:n], scalar1=0.125, scalar2=None, op0=ALU.mult), reads=[a], writes=[mq])
        if DSTOP <= 1:
            return
        GI = k.sbuf("md_gi", [16, NT], F32)
        GL = k.sbuf("md_gl", [16, NT], F32)
        k.dma("sp", GI[:], MGT[:], reads=[MGT], writes=[GI])
        k.op("dve", lambda e: e.tensor_scalar(out=GI[:], in0=GI[:], scalar1=mgb[:, 0:1], scalar2=None, op0=ALU.add), reads=[GI, mgb], writes=[GI])
        k.op("act", lambda e: e.activation(out=GL[:], in_=GI[:], func=AF.Exp, scale=-1.0), reads=[GI], writes=[GL])
        k.op("act", lambda e: e.activation(out=GL[:], in_=GL[:], func=AF.Ln, bias=1.0, scale=1.0), reads=[GL], writes=[GL])
        k.op("dve", lambda e: e.tensor_scalar(out=GL[:], in0=GL[:], scalar1=-1.0, scalar2=None, op0=ALU.mult), reads=[GL], writes=[GL])
        ES = k.sbuf("md_es", [128, NTILE, 8], F32)
        EB = k.sbuf("md_eb", [128, NTILE, 8], F32)
        EBL = k.sbuf("md_ebl", [128, NTILE, 8], F32)
        VAUG = k.sbuf("md_vaug", [128, NTILE, 4, 66], BF16)
        k.op("pool", lambda e: e.memset(VAUG[:], 1.0), writes=[VAUG])
        gtok = [k.sbuf("md_gtok%d" % i, [128, 32], F32) for i in range(2)]
        cs = [k.sbuf("md_cs%d" % i, [128, 16], F32) for i in range(2)]
        vt = [k.sbuf("md_vt%d" % i, [128, 2, 128], F32) for i in range(2)]
        for t in range(NTILE):
            tsl = slice(t * 128, (t + 1) * 128)
            p = ps()
            k.op("pe", lambda e: e.transpose(p[:, 0:16], GI[0:16, tsl], ident[0:16, 0:16]), reads=[GI, ident], writes=[p])
            k.op("pe", lambda e: e.transpose(p[:, 16:32], GL[0:16, tsl], ident[0:16, 0:16]), reads=[GL, ident], writes=[p])
            g = k.nxt("md_gtok", gtok)
            k.op("act", lambda e: e.copy(out=g[:], in_=p[:, 0:32]), reads=[p], writes=[g])
            p2 = ps()
            k.op("pe", lambda e: e.matmul(p2[:, 0:4], triu[:], g[:, 20:24], start=True, stop=True), reads=[triu, g], writes=[p2])
            k.op("pe", lambda e: e.matmul(p2[:, 4:8], tril[:], g[:, 28:32], start=True, stop=True), reads=[tril, g], writes=[p2])
            k.op("pe", lambda e: e.matmul(p2[:, 8:12], ones_f[:], g[:, 20:24], start=True, stop=True), reads=[ones_f, g], writes=[p2])
            k.op("pe", lambda e: e.matmul(p2[:, 12:16], ones_f[:], g[:, 28:32], start=True, stop=True), reads=[ones_f, g], writes=[p2])
            c_ = k.nxt("md_cs", cs)
            k.op("dve", lambda e: e.tensor_copy(out=c_[:], in_=p2[:, 0:16]), reads=[p2], writes=[c_])
            k.op("dve", lambda e: e.tensor_tensor(out=ES[:, t, 0:4], in0=g[:, 0:4], in1=c_[:, 0:4], op=ALU.subtract), reads=[g, c_], writes=[ES])
            k.op("dve", lambda e: e.tensor_tensor(out=ES[:, t, 4:8], in0=g[:, 8:12], in1=c_[:, 4:8], op=ALU.subtract), reads=[g, c_], writes=[ES])
            k.op("act", lambda e: e.activation(out=ES[:, t, :], in_=ES[:, t, :], func=AF.Exp), reads=[ES], writes=[ES])
            k.op("act", lambda e: e.activation(out=EB[:, t, :], in_=c_[:, 0:8], func=AF.Exp), reads=[c_], writes=[EB])
            k.op("act", lambda e: e.activation(out=EBL[:, t, :], in_=c_[:, 8:16], func=AF.Exp), reads=[c_], writes=[EBL])
            v_ = k.nxt("md_vt", vt)
            k.dma("sp", v_[:], MVT.t.ap().rearrange("(c p) t -> p c t", p=128)[:, :, tsl], reads=[MVT], writes=[v_])
            p3 = ps()
            for c in range(2):
                k.op("pe", lambda e, c=c: e.transpose(p3[:, c * 128:(c + 1) * 128], v_[:, c, :], ident[:]), reads=[v_, ident], writes=[p3])
            k.op("dve", lambda e: e.tensor_copy(out=VAUG[:, t, :, 0:64], in_=p3[:, 0:256].rearrange("p (h d) -> p h d", d=64)), reads=[p3], writes=[VAUG])
        if DSTOP <= 2:
            return
        CT = [k.sbuf("md_ct%d" % d, [128, 2, 66], F32) for d in range(2)]
        CTb = [k.sbuf("md_ctb%d" % d, [128, 2, 66], BF16) for d in range(2)]
        keP = [k.sbuf("md_kep%d" % d, [128, 4, 128], BF16) for d in range(2)]
        for d in range(2):
            k.op("pool", lambda e, d=d: e.memset(CT[d][:], 0.0), writes=[CT[d]])
            k.op("pool", lambda e, d=d: e.memset(CTb[d][:], 0.0), writes=[CTb[d]])
            k.op("pool", lambda e, d=d: e.memset(keP[d][:], 0.0), writes=[keP[d]])
        Sp = [k.sbuf("md_sp%d" % i, [128, 4, 128], BF16) for i in range(2)]
        dn = [k.sbuf("md_dn%d" % i, [128, 4], F32) for i in range(2)]
        hv = [k.sbuf("md_hv%d" % i, [128, 4, 64], F32) for i in range(2)]
        dn2 = [k.sbuf("md_dn2%d" % i, [128, 4], F32) for i in range(2)]
        tot = [k.sbuf("md_tot%d" % i, [128, 4, 66], F32) for i in range(2)]
        Ft = [k.sbuf("md_F%d" % i, [128, 2], F32) for i in range(2)]
        ctmp = [k.sbuf("md_ctmp%d" % i, [128, 2, 66], F32) for i in range(2)]
        masks = [triu, tril]
        for i in range(NTILE):
            for d in range(2):
                t = ORDER[d][i]
                tsl = slice(t * 128, (t + 1) * 128)
                pk = ps()
                for c in range(2):
                    k.op("pe", lambda e, c=c: e.matmul(pk[:, c * 128:(c + 1) * 128], mq[:, 2 + c, tsl], identb[:], start=True, stop=True), reads=[mq, identb], writes=[pk])
                for h in range(4):
                    hb = (h % 2) * 64
                    k.op("dve", lambda e, h=h, hb=hb: e.tensor_scalar(out=keP[d][:, h, hb:hb + 64], in0=pk[:, h * 64:(h + 1) * 64], scalar1=ES[:, t, d * 4 + h:d * 4 + h + 1], scalar2=None, op0=ALU.mult),
                         reads=[pk, ES], writes=[keP[d]])
                pS2 = [ps(), ps()]
                for h in range(4):
                    hb = (h % 2) * 64
                    k.op("pe", lambda e, h=h, hb=hb: e.matmul(pS2[h % 2][:, (h // 2) * 128:(h // 2 + 1) * 128], mq[hb:hb + 64, 2 + h // 2, tsl], mq[hb:hb + 64, h // 2, tsl], start=True, stop=True),
                         reads=[mq], writes=[pS2[h % 2]])
                S_ = k.nxt("md_sp", Sp)
                for h in range(4):
                    k.op("dve", lambda e, h=h: e.scalar_tensor_tensor(out=S_[:, h, :], in0=pS2[h % 2][:, (h // 2) * 128:(h // 2 + 1) * 128], scalar=ES[:, t, d * 4 + h:d * 4 + h + 1], in1=masks[d][:],
                                                                      op0=ALU.mult, op1=ALU.mult), reads=[pS2[h % 2], ES, masks[d]], writes=[S_])
                pN = ps()
                pR2 = [ps(), ps()]
                for h in range(4):
                    hb = (h % 2) * 64
                    k.op("pe", lambda e, h=h: e.matmul(pN[:, h * 66:h * 66 + 65], S_[:, h, :], VAUG[:, t, h, 0:65], start=True, stop=True), reads=[S_, VAUG], writes=[pN])
                    k.op("pe", lambda e, h=h, hb=hb: e.matmul(pR2[h % 2][:, (h // 2) * 66:(h // 2) * 66 + 65], mq[hb:hb + 64, h // 2, tsl], CTb[d][hb:hb + 64, h // 2, 0:65], start=True, stop=True),
                         reads=[mq, CTb[d]], writes=[pR2[h % 2]])
                tot_ = k.nxt("md_tot", tot)
                totv = tot_[:].rearrange("p (a b) e -> p a b e", b=2)
                for par in range(2):
                    k.op("act", lambda e, par=par: e.copy(out=totv[:, :, par, 0:65], in_=pR2[par][:, 0:132].rearrange("p (a e) -> p a e", e=66)[:, :, 0:65]), reads=[pR2[par]], writes=[tot_])
                k.op("dve", lambda e: e.tensor_tensor(out=tot_[:, :, 0:65], in0=pN[:, 0:264].rearrange("p (h e) -> p h e", e=66)[:, :, 0:65], in1=tot_[:, :, 0:65], op=ALU.add), reads=[pN, tot_], writes=[tot_])
                pNv = tot_
                dn_ = k.nxt("md_dn", dn)
                hv_ = k.nxt("md_hv", hv)
                k.op("dve", lambda e: e.tensor_tensor(out=dn_[:], in0=pNv[:, :, 64], in1=EB[:, t, d * 4:(d + 1) * 4], op=ALU.mult), reads=[tot_, EB], writes=[dn_])
                dn2_ = k.nxt("md_dn2", dn2)
                k.op("dve", lambda e: e.tensor_scalar(out=dn2_[:], in0=dn_[:], scalar1=-1.0, scalar2=None, op0=ALU.mult), reads=[dn_], writes=[dn2_])
                k.op("dve", lambda e: e.tensor_tensor(out=dn_[:], in0=dn_[:], in1=dn2_[:], op=ALU.max), reads=[dn_, dn2_], writes=[dn_])
                k.op("dve", lambda e: e.tensor_scalar(out=dn_[:], in0=dn_[:], scalar1=1.0, scalar2=None, op0=ALU.max), reads=[dn_], writes=[dn_])
                k.op("dve", lambda e: e.reciprocal(out=dn_[:], in_=dn_[:]), reads=[dn_], writes=[dn_])
                k.op("dve", lambda e: e.tensor_tensor(out=dn_[:], in0=dn_[:], in1=EB[:, t, d * 4:(d + 1) * 4], op=ALU.mult), reads=[dn_, EB], writes=[dn_])
                k.op("dve", lambda e: e.tensor_tensor(out=hv_[:], in0=pNv[:, :, 0:64], in1=dn_[:].unsqueeze(2).to_broadcast([128, 4, 64]), op=ALU.mult), reads=[tot_, dn_], writes=[hv_])
                k.dma("sp", HFB[d][tsl, :], hv_[:].rearrange("p h e -> p (h e)"), reads=[hv_], writes=[HFB[d]])
                pC = ps()
                for pp in range(2):
                    k.op("pe", lambda e, pp=pp: e.matmul(pC[:, pp * 66:pp * 66 + 65], keP[d][:, 2 * pp, :], VAUG[:, t, 2 * pp, 0:65], start=True, stop=False), reads=[keP[d], VAUG], writes=[pC])
                    k.op("pe", lambda e, pp=pp: e.matmul(pC[:, pp * 66:pp * 66 + 65], keP[d][:, 2 * pp + 1, :], VAUG[:, t, 2 * pp + 1, 0:65], start=False, stop=True), reads=[keP[d], VAUG], writes=[pC])
                F_ = k.nxt("md_F", Ft)
                ebl2 = EBL[:, t, d * 4:(d + 1) * 4].rearrange("p (a b) -> p a b", b=2)
                k.op("dve", lambda e: e.tensor_copy(out=F_[0:64, :], in_=ebl2[0:64, :, 0]), reads=[EBL], writes=[F_])
                k.op("dve", lambda e: e.tensor_copy(out=F_[64:128, :], in_=ebl2[64:128, :, 1]), reads=[EBL], writes=[F_])
                ct_ = k.nxt("md_ctmp", ctmp)
                k.op("dve", lambda e: e.tensor_tensor(out=ct_[:, :, 0:65], in0=pC[:, 0:132].rearrange("p (a e) -> p a e", e=66)[:, :, 0:65], in1=CT[d][:, :, 0:65], op=ALU.add), reads=[pC, CT[d]], writes=[ct_])
                k.op("dve", lambda e: e.tensor_tensor(out=CT[d][:, :, 0:65], in0=ct_[:, :, 0:65], in1=F_[:].unsqueeze(2).to_broadcast([128, 2, 65]), op=ALU.mult), reads=[ct_, F_], writes=[CT[d]])
                k.op("act", lambda e: e.copy(out=CTb[d][:, :, 0:65], in_=CT[d][:, :, 0:65]), reads=[CT[d]], writes=[CTb[d]])
        if DSTOP <= 3:
            return
        hf = [k.sbuf("md_hf%d" % i, [128, 4, 64], F32) for i in range(2)]
        hb_ = [k.sbuf("md_hb%d" % i, [128, 4, 64], F32) for i in range(2)]
        st4 = [k.sbuf("md_st%d" % i, [128, 4], F32) for i in range(2)]
        sq4 = [k.sbuf("md_sq%d" % i, [128, 4, 64], F32) for i in range(2)]
        mo = [k.sbuf("md_mo%d" % i, [128, 2, 128], F32) for i in range(2)]
        ob = [k.sbuf("md_ob%d" % i, [128, 2, 128], BF16) for i in range(2)]
        MOTv = MOT.t.ap().rearrange("(c p) t -> p c t", p=128)
        MIXv = MIXT.t.ap()[768:1024, :].rearrange("(c p) t -> p c t", p=128)
        for t in range(NTILE):
            tsl = slice(t * 128, (t + 1) * 128)
            a = k.nxt("md_hf", hf)
            b = k.nxt("md_hb", hb_)
            s4 = k.nxt("md_st", st4)
            q4 = k.nxt("md_sq", sq4)
            k.dma("sp", a[:].rearrange("p h e -> p (h e)"), HFB[0][tsl, :], reads=[HFB[0]], writes=[a])
            k.dma("sp", b[:].rearrange("p h e -> p (h e)"), HFB[1][tsl, :], reads=[HFB[1]], writes=[b])
            k.op("dve", lambda e: e.tensor_tensor(out=a[:], in0=a[:], in1=b[:], op=ALU.add), reads=[a, b], writes=[a])
            k.op("dve", lambda e: e.tensor_reduce(out=s4[:], in_=a[:], axis=AX.X, op=ALU.add), reads=[a], writes=[s4])
            k.op("dve", lambda e: e.tensor_scalar(out=s4[:], in0=s4[:], scalar1=-1.0 / 64, scalar2=None, op0=ALU.mult), reads=[s4], writes=[s4])
            k.op("dve", lambda e: e.tensor_tensor(out=a[:], in0=a[:], in1=s4[:].unsqueeze(2).to_broadcast([128, 4, 64]), op=ALU.add), reads=[a, s4], writes=[a])
            k.op("pool", lambda e: e.tensor_tensor(out=q4[:], in0=a[:], in1=a[:], op=ALU.mult), reads=[a], writes=[q4])
            k.op("dve", lambda e: e.tensor_reduce(out=s4[:], in_=q4[:], axis=AX.X, op=ALU.add), reads=[q4], writes=[s4])
            k.op("act", lambda e: e.activation(out=s4[:], in_=s4[:], func=AF.Sqrt, scale=1.0 / 64, bias=EPS), reads=[s4], writes=[s4])
            k.op("dve", lambda e: e.reciprocal(out=s4[:], in_=s4[:]), reads=[s4], writes=[s4])
            k.op("dve", lambda e: e.tensor_tensor(out=a[:], in0=a[:], in1=s4[:].unsqueeze(2).to_broadcast([128, 4, 64]), op=ALU.mult), reads=[a, s4], writes=[a])
            p = ps()
            av = a[:].rearrange("p h e -> p (h e)")
            for c in range(2):
                k.op("pe", lambda e, c=c: e.transpose(p[:, c * 128:(c + 1) * 128], av[:, c * 128:(c + 1) * 128], ident[:]), reads=[a, ident], writes=[p])
            m_ = k.nxt("md_mo", mo)
            o_ = k.nxt("md_ob", ob)
            k.dma("sp", m_[:], MOTv[:, :, tsl], reads=[MOT], writes=[m_])
            for c in range(2):
                k.op("dve", lambda e, c=c: e.scalar_tensor_tensor(out=o_[:, c, :], in0=p[:, c * 128:(c + 1) * 128], scalar=mng[:, c:c + 1], in1=m_[:, c, :], op0=ALU.mult, op1=ALU.mult),
                     reads=[p, mng, m_], writes=[o_])
            k.dma("sp", MIXv[:, :, tsl], o_[:], reads=[o_], writes=[MIXT])

    rp_in = ein("rw_par", [L, 64, 48])
    w2p_in = ein("rw_w2p", [L, 2, 64, 256])
    a2p_in = ein("rw_a2p", [L, 2, 64, 256])
    g2_in = ein("rw_g2", [L, 64, 256])
    lnp_in = ein("rw_lnp", [L, 128, 2, 2])
    rmask_in = ein("rw_mask", [2, 64, 5, 64])
    reset_in = ein("rw_reset", [64, 512])
    RS = k.dram("RS", [2, 4, 4, 64, NT], BF16)
    VS = k.dram("VS", [4, 64, NT], BF16)
    WLD = k.dram("WLD", [2, 4, 64, 68], F32)
    BON = k.dram("BON", [256, NT], F32)
    GGd = k.dram("GGd", [256, NT], F32)
    YS = [k.dram("YSf", [NT, 256], F32), k.dram("YSb", [NT, 256], F32)]
    CH_ORDER = [list(range(68)), [3, 2, 1, 0] + list(range(67, 3, -1))]

    def phase_C(l):
        RP = k.sbuf("rc_rp", [64, 48], F32)
        W2P = k.sbuf("rc_w2p", [64, 2, 256], F32)
        A2P = k.sbuf("rc_a2p", [64, 2, 256], F32)
        G2 = k.sbuf("rc_g2", [64, 256], F32)
        RESET = k.sbuf("rc_reset", [64, 512], F32)
        ONES = k.sbuf("rc_ones", [64, 512], F32)
        omka = k.sbuf("rc_omka", [64, 4], F32)
        k.dma("sp", RP[:], rp_in[l], reads=[rp_in], writes=[RP])
        for d in range(2):
            k.dma("sp", W2P[:, d, :], w2p_in[l][d], reads=[w2p_in], writes=[W2P])
            k.dma("sp", A2P[:, d, :], a2p_in[l][d], reads=[a2p_in], writes=[A2P])
        k.dma("sp", G2[:], g2_in[l], reads=[g2_in], writes=[G2])
        k.dma("sp", RESET[:], reset_in[:], reads=[reset_in], writes=[RESET])
        k.op("pool", lambda e: e.memset(ONES[:], 1.0), writes=[ONES])
        k.op("dve", lambda e: e.tensor_scalar(out=omka[:], in0=RP[:, 35:39], scalar1=-1.0, scalar2=1.0, op0=ALU.mult, op1=ALU.add), reads=[RP], writes=[omka])

        def mk(name, shape, dt, nb=2):
            return [k.sbuf("rc_%s%d" % (name, i), shape, dt) for i in range(nb)]
        zin = mk("zin", [64, 514], F32, 3)
        nm = mk("nm", [64, 512], F32, 2)
        wds = k.sbuf("rc_wds", [64, 512], F32)
        ads = k.sbuf("rc_ads", [64, 512], F32)
        gds = k.sbuf("rc_gds", [64, 512], F32)
        rs_ = k.sbuf("rc_rs", [64, 512], F32)
        ks_ = k.sbuf("rc_ks", [64, 512], F32)
        vs_ = k.sbuf("rc_vs", [64, 512], F32)
        kk_ = k.sbuf("rc_kk", [64, 512], F32)
        t_a = mk("ta", [64, 512], F32, 2)
        t_b = mk("tb", [64, 512], F32, 2)
        t_c = mk("tc", [64, 512], F32, 2)
        lw_ = mk("lw", [64, 512], F32, 2)
        LW = mk("LW", [64, 512], F32, 2)
        km = [k.sbuf("rc_km%d" % d, [64, 512], F32) for d in range(2)]
        aa = mk("aa", [64, 512], F32, 2)
        ob4 = mk("ob4", [64, 4, 512], BF16, 2)
        vb = mk("vb", [64, 512], BF16, 2)
        wl_sb = k.sbuf("rc_wl", [64, 8, 68], F32)

        def shift(dst, row0, mucol, pc, n):
            z = k.nxt("rc_zin", zin)
            m_ = k.nxt("rc_nm", nm)
            k.dma("sp", z[:, 0:n + 2], RZT[row0:row0 + 64, pc - 1:pc + n + 1], reads=[RZT], writes=[z])
            k.op("pool", lambda e: e.tensor_tensor(out=m_[:, 0:n], in0=z[:, 0:n], in1=z[:, 2:n + 2], op=ALU.add), reads=[z], writes=[m_])
            k.op("dve", lambda e: e.scalar_tensor_tensor(out=m_[:, 0:n], in0=m_[:, 0:n], scalar=0.5, in1=z[:, 1:n + 1], op0=ALU.mult, op1=ALU.subtract), reads=[m_, z], writes=[m_])
            k.op("dve", lambda e: e.scalar_tensor_tensor(out=dst[:, 0:n], in0=m_[:, 0:n], scalar=RP[:, mucol:mucol + 1], in1=z[:, 1:n + 1], op0=ALU.mult, op1=ALU.add), reads=[m_, z, RP], writes=[dst])

        for (t0, n) in BLOCKS:
            pc = padcol(t0)
            nch = n // 64
            c0 = t0 // 64
            shift(wds, 768, 12, pc, n)
            shift(ads, 832, 13, pc, n)
            shift(gds, 896, 14, pc, n)
            k.op("act", lambda e: e.activation(out=wds[:, 0:n], in_=wds[:, 0:n], func=AF.Tanh), reads=[wds], writes=[wds])
            k.op("act", lambda e: e.activation(out=gds[:, 0:n], in_=gds[:, 0:n], func=AF.Sigmoid), reads=[gds], writes=[gds])
            for h in range(4):
                hs = slice(h * 64, (h + 1) * 64)
                shift(rs_, h * 64, 0 + h, pc, n)
                shift(ks_, 256 + h * 64, 4 + h, pc, n)
                shift(vs_, 512 + h * 64, 8 + h, pc, n)
                p = ps()
                k.op("pe", lambda e: e.matmul(p[0:64, 0:n], G2[:, hs], gds[:, 0:n], start=True, stop=True), reads=[G2, gds], writes=[p])
                ta = k.nxt("rc_ta", t_a)
                k.op("act", lambda e: e.copy(out=ta[:, 0:n], in_=p[0:64, 0:n]), reads=[p], writes=[ta])
                k.dma("sp", GGd[hs, t0:t0 + n], ta[:, 0:n], reads=[ta], writes=[GGd])
                vb_ = k.nxt("rc_vb", vb)
                k.op("pool", lambda e: e.tensor_copy(out=vb_[:, 0:n], in_=vs_[:, 0:n]), reads=[vs_], writes=[vb_])
                k.dma("sp", VS[h][:, t0:t0 + n], vb_[:, 0:n], reads=[vb_], writes=[VS])
                tb = k.nxt("rc_tb", t_b)
                tc_ = k.nxt("rc_tc", t_c)
                k.op("dve", lambda e: e.tensor_scalar(out=tb[:, 0:n], in0=ks_[:, 0:n], scalar1=RP[:, 31 + h:32 + h], scalar2=None, op0=ALU.mult), reads=[ks_, RP], writes=[tb])
                k.op("pool", lambda e: e.tensor_tensor(out=tc_[:, 0:n], in0=tb[:, 0:n], in1=tb[:, 0:n], op=ALU.mult), reads=[tb], writes=[tc_])
                p = ps()
                k.op("pe", lambda e: e.matmul(p[0:64, 0:n], ONES[:, 0:64], tc_[:, 0:n], start=True, stop=True), reads=[ONES, tc_], writes=[p])
                k.op("act", lambda e: e.activation(out=tc_[:, 0:n], in_=p[0:64, 0:n], func=AF.Sqrt), reads=[p], writes=[tc_])
                k.op("dve", lambda e: e.tensor_scalar(out=tc_[:, 0:n], in0=tc_[:, 0:n], scalar1=1e-12, scalar2=None, op0=ALU.max), reads=[tc_], writes=[tc_])
                k.op("dve", lambda e: e.reciprocal(out=tc_[:, 0:n], in_=tc_[:, 0:n]), reads=[tc_], writes=[tc_])
                k.op("dve", lambda e: e.tensor_tensor(out=kk_[:, 0:n], in0=tb[:, 0:n], in1=tc_[:, 0:n], op=ALU.mult), reads=[tb, tc_], writes=[kk_])
                for d in range(2):
                    p = ps()
                    k.op("pe", lambda e: e.matmul(p[0:64, 0:n], W2P[:, d, hs], wds[:, 0:n], start=True, stop=True), reads=[W2P, wds], writes=[p])
                    lw = k.nxt("rc_lw", lw_)
                    k.op("act", lambda e: e.activation(out=lw[:, 0:n], in_=p[0:64, 0:n], func=AF.Sigmoid, bias=RP[:, 15 + d * 4 + h:16 + d * 4 + h], scale=1.0), reads=[p, RP], writes=[lw])
                    k.op("dve", lambda e: e.tensor_scalar(out=lw[:, 0:n], in0=lw[:, 0:n], scalar1=-0.606531, scalar2=None, op0=ALU.mult), reads=[lw], writes=[lw])
                    LWt = k.nxt("rc_LW", LW)
                    k.op("dve", lambda e: e.tensor_tensor_scan(out=LWt[:, 0:n], data0=RESET[:, 0:n], data1=lw[:, 0:n], initial=0.0, op0=ALU.mult, op1=ALU.add), reads=[RESET, lw], writes=[LWt])
                    LW3 = LWt[:, 0:n].rearrange("p (c t) -> p c t", t=64)
                    if d == 1:
                        tb2 = k.nxt("rc_tb", t_b)
                        k.op("dve", lambda e: e.tensor_tensor(out=tb2[:, 0:n].rearrange("p (c t) -> p c t", t=64), in0=LW3[:, :, 63:64].to_broadcast([64, nch, 64]), in1=LW3, op=ALU.subtract),
                             reads=[LWt], writes=[tb2])
                        k.op("dve", lambda e: e.tensor_tensor(out=LWt[:, 0:n], in0=tb2[:, 0:n], in1=lw[:, 0:n], op=ALU.add), reads=[tb2, lw], writes=[LWt])
                    tot_ap = LW3[:, :, 63] if d == 0 else LW3[:, :, 0]
                    k.op("act", lambda e: e.activation(out=wl_sb[:, d * 4 + h, c0:c0 + nch], in_=tot_ap, func=AF.Exp), reads=[LWt], writes=[wl_sb])
                    p = ps()
                    k.op("pe", lambda e: e.matmul(p[0:64, 0:n], A2P[:, d, hs], ads[:, 0:n], start=True, stop=True), reads=[A2P, ads], writes=[p])
                    a_ = k.nxt("rc_aa", aa)
                    k.op("act", lambda e: e.activation(out=a_[:, 0:n], in_=p[0:64, 0:n], func=AF.Sigmoid, bias=RP[:, 23 + d * 4 + h:24 + d * 4 + h], scale=1.0), reads=[p, RP], writes=[a_])
                    k.op("dve", lambda e: e.tensor_scalar(out=km[d][:, 0:n], in0=a_[:, 0:n], scalar1=RP[:, 35 + h:36 + h], scalar2=omka[:, h:h + 1], op0=ALU.mult, op1=ALU.add), reads=[a_, RP, omka], writes=[km[d]])
                    k.op("pool", lambda e: e.tensor_tensor(out=km[d][:, 0:n], in0=km[d][:, 0:n], in1=ks_[:, 0:n], op=ALU.mult), reads=[km[d], ks_], writes=[km[d]])
                    k.op("pool", lambda e: e.tensor_tensor(out=a_[:, 0:n], in0=a_[:, 0:n], in1=kk_[:, 0:n], op=ALU.mult), reads=[a_, kk_], writes=[a_])
                    e1 = k.nxt("rc_ta", t_a)
                    e2 = k.nxt("rc_tc", t_c)
                    k.op("act", lambda e: e.activation(out=e1[:, 0:n], in_=LWt[:, 0:n], func=AF.Exp), reads=[LWt], writes=[e1])
                    k.op("act", lambda e: e.activation(out=e2[:, 0:n], in_=LWt[:, 0:n], func=AF.Exp, scale=-1.0), reads=[LWt], writes=[e2])
                    k.op("dve", lambda e: e.tensor_tensor(out=lw[:, 0:n], in0=LWt[:, 0:n], in1=lw[:, 0:n], op=ALU.subtract), reads=[LWt, lw], writes=[lw])
                    k.op("act", lambda e: e.activation(out=lw[:, 0:n], in_=lw[:, 0:n], func=AF.Exp), reads=[lw], writes=[lw])
                    o4 = k.nxt("rc_ob4", ob4)
                    k.op("dve", lambda e: e.tensor_tensor(out=o4[:, 0, 0:n], in0=kk_[:, 0:n], in1=lw[:, 0:n], op=ALU.mult), reads=[kk_, lw], writes=[o4])
                    k.op("pool", lambda e: e.tensor_tensor(out=o4[:, 1, 0:n], in0=rs_[:, 0:n], in1=e1[:, 0:n], op=ALU.mult), reads=[rs_, e1], writes=[o4])
                    k.op("dve", lambda e: e.tensor_tensor(out=o4[:, 2, 0:n], in0=a_[:, 0:n], in1=e2[:, 0:n], op=ALU.mult), reads=[a_, e2], writes=[o4])
                    k.op("pool", lambda e: e.tensor_tensor(out=o4[:, 3, 0:n], in0=km[d][:, 0:n], in1=e2[:, 0:n], op=ALU.mult), reads=[km[d], e2], writes=[o4])
                    for q in range(4):
                        k.dma("sp", RS[d][h][q][:, t0:t0 + n], o4[:, q, 0:n], reads=[o4], writes=[RS])
                tb = k.nxt("rc_tb", t_b)
                k.op("dve", lambda e: e.tensor_tensor(out=tb[:, 0:n], in0=km[0][:, 0:n], in1=km[1][:, 0:n], op=ALU.add), reads=[km[0], km[1]], writes=[tb])
                k.op("dve", lambda e: e.scalar_tensor_tensor(out=tb[:, 0:n], in0=rs_[:, 0:n], scalar=RP[:, 43 + h:44 + h], in1=tb[:, 0:n], op0=ALU.mult, op1=ALU.mult), reads=[rs_, RP, tb], writes=[tb])
                p = ps()
                k.op("pe", lambda e: e.matmul(p[0:64, 0:n], ONES[:, 0:64], tb[:, 0:n], start=True, stop=True), reads=[ONES, tb], writes=[p])
                ta = k.nxt("rc_ta", t_a)
                k.op("dve", lambda e: e.tensor_tensor(out=ta[:, 0:n], in0=p[0:64, 0:n], in1=vs_[:, 0:n], op=ALU.mult), reads=[p, vs_], writes=[ta])
                k.dma("sp", BON[hs, t0:t0 + n], ta[:, 0:n], reads=[ta], writes=[BON])
        if DSTOP <= 1:
            return
        RM = k.sbuf("rs_mask", [64, 2, 5, 64], F32)
        for d in range(2):
            k.dma("sp", RM[:, d, :, :], rmask_in[d], reads=[rmask_in], writes=[RM])
        X = [k.sbuf("rs_x%d" % i, [64, 4, 4, 64], BF16) for i in range(4)]
        XV = [k.sbuf("rs_xv%d" % i, [64, 4, 64], BF16) for i in range(4)]
        S0 = k.sbuf("rs_s0", [64, 8, 64], F32)
        S0b = k.sbuf("rs_s0b", [64, 8, 64], BF16)
        k.op("pool", lambda e: e.memset(S0[:], 0.0), writes=[S0])
        k.op("pool", lambda e: e.memset(S0b[:], 0.0), writes=[S0b])
        PQ = [k.sbuf("rs_pq%d" % i, [64, 2, 8, 64], BF16) for i in range(2)]
        A2 = k.sbuf("rs_a2", [64, 2, 8, 64], BF16)
        A3 = k.sbuf("rs_a3", [64, 8, 64], BF16)
        TOK = k.sbuf("rs_tok", [64, 3, 8, 64], BF16)
        G = k.sbuf("rs_g", [64, 8, 64], F32)
        Gb = k.sbuf("rs_gb", [64, 8, 64], BF16)
        nUb = k.sbuf("rs_nub", [64, 8, 64], BF16)
        Ysb = [k.sbuf("rs_y%d" % i, [64, 8, 64], F32) for i in range(2)]
        stmp = k.sbuf("rs_stmp", [64, 8, 64], F32)
        RSv = [[RS[d][h] for h in range(4)] for d in range(2)]
        for i in range(NSTEP):
            xs, xvs = [], []
            for d in range(2):
                c = CH_ORDER[d][i]
                x_ = k.nxt("rs_x", X)
                xv_ = k.nxt("rs_xv", XV)
                for h in range(4):
                    k.dma("sp" if h % 2 == 0 else "pool", x_[:, h, :, :], RS[d][h].rearrange("q p t -> p q t")[:, :, c * 64:(c + 1) * 64], reads=[RS], writes=[x_])
                k.dma("sp", xv_[:], VS.t.ap().rearrange("h p t -> p h t")[:, :, c * 64:(c + 1) * 64], reads=[VS], writes=[xv_])
                xs.append(x_)
                xvs.append(xv_)
            if SSTOP <= 1:
                continue
            pT1 = [ps(), ps()]
            pT2 = [ps(), ps()]
            pT3 = ps()
            for d in range(2):
                x_ = xs[d]
                for h in range(4):
                    cs_ = slice(h * 64, (h + 1) * 64)
                    cs2 = slice(256 + h * 64, 256 + (h + 1) * 64)
                    k.op("pe", lambda e: e.matmul(pT1[d][0:64, cs_], x_[:, h, 2, :], x_[:, h, 0, :], start=True, stop=True), reads=[x_], writes=[pT1[d]])
                    k.op("pe", lambda e: e.matmul(pT1[d][0:64, cs2], x_[:, h, 0, :], x_[:, h, 2, :], start=True, stop=True), reads=[x_], writes=[pT1[d]])
                    k.op("pe", lambda e: e.matmul(pT2[d][0:64, cs_], x_[:, h, 3, :], x_[:, h, 0, :], start=True, stop=True), reads=[x_], writes=[pT2[d]])
                    k.op("pe", lambda e: e.matmul(pT2[d][0:64, cs2], x_[:, h, 2, :], x_[:, h, 1, :], start=True, stop=True), reads=[x_], writes=[pT2[d]])
                    k.op("pe", lambda e: e.matmul(pT3[0:64, d * 256 + h * 64:d * 256 + (h + 1) * 64], x_[:, h, 3, :], x_[:, h, 1, :], start=True, stop=True), reads=[x_], writes=[pT3])
            pq = k.nxt("rs_pq", PQ)
            for d in range(2):
                k.op("dve", lambda e, d=d: e.tensor_tensor(out=pq[:, :, d * 4:(d + 1) * 4, :], in0=pT1[d][0:64, :].rearrange("p (a h t) -> p a h t", a=2, h=4),
                                                           in1=RM[:, d, 0:2, :].unsqueeze(2).to_broadcast([64, 2, 4, 64]), op=ALU.mult), reads=[pT1[d], RM], writes=[pq])
                k.op("dve", lambda e, d=d: e.tensor_tensor(out=A2[:, :, d * 4:(d + 1) * 4, :], in0=pT2[d][0:64, :].rearrange("p (a h t) -> p a h t", a=2, h=4),
                                                           in1=RM[:, d, 2:4, :].unsqueeze(2).to_broadcast([64, 2, 4, 64]), op=ALU.mult), reads=[pT2[d], RM], writes=[A2])
                k.op("dve", lambda e, d=d: e.tensor_tensor(out=A3[:, d * 4:(d + 1) * 4, :], in0=pT3[0:64, d * 256:(d + 1) * 256].rearrange("p (h t) -> p h t", h=4),
                                                           in1=RM[:, d, 4:5, :].to_broadcast([64, 4, 64]), op=ALU.mult), reads=[pT3, RM], writes=[A3])
            if SSTOP <= 2:
                continue
            pK = [ps(), ps()]
            for d in range(2):
                x_ = xs[d]
                for h in range(4):
                    dh = d * 4 + h
                    k.op("pe", lambda e: e.matmul(pK[0][0:64, dh * 64:(dh + 1) * 64], x_[:, h, 2, :], identb[0:64, 0:64], start=True, stop=True), reads=[x_, identb], writes=[pK[0]])
                    k.op("pe", lambda e: e.matmul(pK[1][0:64, dh * 64:(dh + 1) * 64], x_[:, h, 3, :], identb[0:64, 0:64], start=True, stop=True), reads=[x_, identb], writes=[pK[1]])
            pV = ps()
            for d in range(2):
                for h in range(4):
                    dh = d * 4 + h
                    k.op("pe", lambda e: e.matmul(pV[0:64, dh * 64:(dh + 1) * 64], xvs[d][:, h, :], identb[0:64, 0:64], start=True, stop=True), reads=[xvs[d], identb], writes=[pV])
            k.op("act", lambda e: e.copy(out=TOK[:, 0, :, :], in_=pK[0][0:64, :].rearrange("p (a t) -> p a t", t=64)), reads=[pK[0]], writes=[TOK])
            k.op("act", lambda e: e.copy(out=TOK[:, 1, :, :], in_=pK[1][0:64, :].rearrange("p (a t) -> p a t", t=64)), reads=[pK[1]], writes=[TOK])
            k.op("dve", lambda e: e.tensor_copy(out=TOK[:, 2, :, :], in_=pV[0:64, :].rearrange("p (a t) -> p a t", t=64)), reads=[pV], writes=[TOK])
            if SSTOP <= 3:
                continue
            pG = ps()
            for d in range(2):
                for h in range(4):
                    dh = d * 4 + h
                    o_ = pG[0:64, dh * 64:(dh + 1) * 64]
                    k.op("pe", lambda e: e.matmul(o_, xs[d][:, h, 0, :], S0b[:, dh, :], start=True, stop=False), reads=[xs[d], S0b], writes=[pG])
                    k.op("pe", lambda e: e.matmul(o_, A2[:, 0, dh, :], TOK[:, 2, dh, :], start=False, stop=True), reads=[A2, TOK], writes=[pG])
            k.op("dve", lambda e: e.tensor_copy(out=G[:], in_=pG[0:64, :].rearrange("p (a t) -> p a t", t=64)), reads=[pG], writes=[G])
            k.op("act", lambda e: e.copy(out=Gb[:], in_=pG[0:64, :].rearrange("p (a t) -> p a t", t=64)), reads=[pG], writes=[Gb])
            if SSTOP <= 4:
                continue
            cur = pq
            for kk in range(6):
                pD = ps()
                for dh in range(8):
                    k.op("pe", lambda e, dh=dh: e.matmul(pD[0:64, dh * 64:(dh + 1) * 64], cur[:, 0, dh, :], Gb[:, dh, :], start=True, stop=True), reads=[cur, Gb], writes=[pD])
                if kk < 5:
                    pP = ps()
                    pQ = ps()
                    for dh in range(8):
                        k.op("pe", lambda e, dh=dh: e.matmul(pP[0:64, dh * 64:(dh + 1) * 64], cur[:, 1, dh, :], cur[:, 0, dh, :], start=True, stop=True), reads=[cur], writes=[pP])
                        k.op("pe", lambda e, dh=dh: e.matmul(pQ[0:64, dh * 64:(dh + 1) * 64], cur[:, 0, dh, :], cur[:, 1, dh, :], start=True, stop=True), reads=[cur], writes=[pQ])
                k.op("dve", lambda e: e.tensor_tensor(out=G[:], in0=pD[0:64, :].rearrange("p (a t) -> p a t", t=64), in1=G[:], op=ALU.add), reads=[pD, G], writes=[G])
                k.op("pool", lambda e: e.tensor_copy(out=Gb[:], in_=G[:]), reads=[G], writes=[Gb])
                if kk < 5:
                    nx = k.nxt("rs_pq", PQ)
                    k.op("act", lambda e: e.copy(out=nx[:, 0, :, :], in_=pP[0:64, :].rearrange("p (a t) -> p a t", t=64)), reads=[pP], writes=[nx])
                    k.op("dve", lambda e: e.tensor_copy(out=nx[:, 1, :, :], in_=pQ[0:64, :].rearrange("p (a t) -> p a t", t=64)), reads=[pQ], writes=[nx])
                    cur = nx
            k.op("act", lambda e: e.activation(out=nUb[:], in_=G[:], func=AF.Copy, scale=-1.0), reads=[G], writes=[nUb])
            if SSTOP <= 5:
                continue
            pY = ps()
            for d in range(2):
                for h in range(4):
                    dh = d * 4 + h
                    o_ = pY[0:64, dh * 64:(dh + 1) * 64]
                    k.op("pe", lambda e: e.matmul(o_, xs[d][:, h, 1, :], S0b[:, dh, :], start=True, stop=False), reads=[xs[d], S0b], writes=[pY])
                    k.op("pe", lambda e: e.matmul(o_, A2[:, 1, dh, :], Gb[:, dh, :], start=False, stop=False), reads=[A2, Gb], writes=[pY])
                    k.op("pe", lambda e: e.matmul(o_, A3[:, dh, :], TOK[:, 2, dh, :], start=False, stop=True), reads=[A3, TOK], writes=[pY])
            y_ = k.nxt("rs_y", Ysb)
            k.op("act", lambda e: e.copy(out=y_[:], in_=pY[0:64, :].rearrange("p (a t) -> p a t", t=64)), reads=[pY], writes=[y_])
            for d in range(2):
                c = CH_ORDER[d][i]
                k.dma("sp", YS[d][c * 64:(c + 1) * 64, :].rearrange("p (h t) -> p h t", t=64), y_[:, d * 4:(d + 1) * 4, :], reads=[y_], writes=[YS[d]])
            if SSTOP <= 6:
                continue
            pS = ps()
            for d in range(2):
                for h in range(4):
                    dh = d * 4 + h
                    o_ = pS[0:64, dh * 64:(dh + 1) * 64]
                    k.op("pe", lambda e: e.matmul(o_, TOK[:, 0, dh, :], nUb[:, dh, :], start=True, stop=False), reads=[TOK, nUb], writes=[pS])
                    k.op("pe", lambda e: e.matmul(o_, TOK[:, 1, dh, :], TOK[:, 2, dh, :], start=False, stop=True), reads=[TOK], writes=[pS])
            k.op("dve", lambda e: e.tensor_tensor(out=stmp[:], in0=pS[0:64, :].rearrange("p (a t) -> p a t", t=64), in1=S0[:], op=ALU.add), reads=[pS, S0], writes=[stmp])
            for d in range(2):
                c = CH_ORDER[d][i]
                k.op("dve", lambda e, d=d, c=c: e.tensor_tensor(out=S0[:, d * 4:(d + 1) * 4, :], in0=stmp[:, d * 4:(d + 1) * 4, :],
                                                                in1=wl_sb[:, d * 4:(d + 1) * 4, c:c + 1].to_broadcast([64, 4, 64]), op=ALU.mult), reads=[stmp, wl_sb], writes=[S0])
            k.op("act", lambda e: e.copy(out=S0b[:], in_=S0[:]), reads=[S0], writes=[S0b])
        if DSTOP <= 2:
            return
        LNP = k.sbuf("rf_lnp", [128, 2, 2], F32)
        k.dma("sp", LNP[:], lnp_in[l], reads=[lnp_in], writes=[LNP])
        yf = mk("yf", [128, 4, 64], F32, 2)
        yb = mk("yb", [128, 4, 64], F32, 2)
        s4_ = mk("s4", [128, 4], F32, 2)
        q4_ = mk("q4", [128, 4, 64], F32, 2)
        bg = mk("bg", [128, 2, 2, 128], F32, 2)
        o2 = mk("o2", [128, 2, 128], F32, 2)
        ob = mk("ob", [128, 2, 128], BF16, 2)
        BONv = BON.t.ap().rearrange("(c p) t -> p c t", p=128)
        GGv = GGd.t.ap().rearrange("(c p) t -> p c t", p=128)
        MIXr = MIXT.t.ap()[512:768, :].rearrange("(c p) t -> p c t", p=128)
        for t in range(NTILE):
            tsl = slice(t * 128, (t + 1) * 128)
            a = k.nxt("rc_yf", yf)
            b = k.nxt("rc_yb", yb)
            s4 = k.nxt("rc_s4", s4_)
            q4 = k.nxt("rc_q4", q4_)
            k.dma("sp", a[:].rearrange("p h e -> p (h e)"), YS[0][tsl, :], reads=[YS[0]], writes=[a])
            k.dma("sp", b[:].rearrange("p h e -> p (h e)"), YS[1][tsl, :], reads=[YS[1]], writes=[b])
            k.op("dve", lambda e: e.tensor_tensor(out=a[:], in0=a[:], in1=b[:], op=ALU.add), reads=[a, b], writes=[a])
            k.op("dve", lambda e: e.tensor_reduce(out=s4[:], in_=a[:], axis=AX.X, op=ALU.add), reads=[a], writes=[s4])
            k.op("dve", lambda e: e.tensor_scalar(out=s4[:], in0=s4[:], scalar1=-1.0 / 64, scalar2=None, op0=ALU.mult), reads=[s4], writes=[s4])
            k.op("dve", lambda e: e.tensor_tensor(out=a[:], in0=a[:], in1=s4[:].unsqueeze(2).to_broadcast([128, 4, 64]), op=ALU.add), reads=[a, s4], writes=[a])
            k.op("pool", lambda e: e.tensor_tensor(out=q4[:], in0=a[:], in1=a[:], op=ALU.mult), reads=[a], writes=[q4])
            k.op("dve", lambda e: e.tensor_reduce(out=s4[:], in_=q4[:], axis=AX.X, op=ALU.add), reads=[q4], writes=[s4])
            k.op("act", lambda e: e.activation(out=s4[:], in_=s4[:], func=AF.Sqrt, scale=1.0 / 64, bias=64e-5), reads=[s4], writes=[s4])
            k.op("dve", lambda e: e.reciprocal(out=s4[:], in_=s4[:]), reads=[s4], writes=[s4])
            k.op("dve", lambda e: e.tensor_tensor(out=a[:], in0=a[:], in1=s4[:].unsqueeze(2).to_broadcast([128, 4, 64]), op=ALU.mult), reads=[a, s4], writes=[a])
            p = ps()
            av = a[:].rearrange("p h e -> p (h e)")
            for c in range(2):
                k.op("pe", lambda e, c=c: e.transpose(p[:, c * 128:(c + 1) * 128], av[:, c * 128:(c + 1) * 128], ident[:]), reads=[a, ident], writes=[p])
            g_ = k.nxt("rc_bg", bg)
            k.dma("sp", g_[:, 0, :, :], BONv[:, :, tsl], reads=[BON], writes=[g_])
            k.dma("sp", g_[:, 1, :, :], GGv[:, :, tsl], reads=[GGd], writes=[g_])
            o_ = k.nxt("rc_o2", o2)
            ob_ = k.nxt("rc_ob", ob)
            for c in range(2):
                k.op("act", lambda e, c=c: e.activation(out=o_[:, c, :], in_=p[:, c * 128:(c + 1) * 128], func=AF.Identity, scale=LNP[:, c, 0:1], bias=LNP[:, c, 1:2]), reads=[p, LNP], writes=[o_])
            k.op("dve", lambda e: e.tensor_tensor(out=o_[:], in0=o_[:], in1=g_[:, 0, :, :], op=ALU.add), reads=[o_, g_], writes=[o_])
            k.op("dve", lambda e: e.tensor_tensor(out=ob_[:], in0=o_[:], in1=g_[:, 1, :, :], op=ALU.mult), reads=[o_, g_], writes=[ob_])
            k.dma("sp", MIXr[:, :, tsl], ob_[:], reads=[ob_], writes=[MIXT])

    def phase_E(l):
        WO = k.sbuf("WO", [128, 8, D], BF16)
        for kc in range(8):
            k.dma("pool", WO[:, kc, :], w_out[l][kc * 128:(kc + 1) * 128, :], reads=[w_out], writes=[WO])
        mx = [k.sbuf("pe_mx%d" % i, [128, 8, 512], BF16) for i in range(2)]
        xb_ = [k.sbuf("pe_x%d" % i, [128, 8, 512], F32) for i in range(2)]
        MIXv8 = MIXT.t.ap().rearrange("(kc p) t -> p kc t", p=128)
        for (t0, n) in BLOCKS:
            s_ = 1 if t0 == 0 else 0
            if s_ == 1 and l == DEPTH - 1:
                continue
            m_ = k.nxt("pe_mx", mx)
            xb = k.nxt("pe_x", xb_)
            k.dma("sp", m_[:, :, 0:n], MIXv8[:, :, t0:t0 + n], reads=[MIXT], writes=[m_])
            k.dma("sp", xb[:, :, 0:n], XTv[:, :, t0:t0 + n], reads=[XT], writes=[xb])
            for oc in range(8):
                p = ps()
                for kc in range(8):
                    k.op("pe", lambda e, kc=kc: e.matmul(p[:, 0:n], WO[:, kc, oc * 128:(oc + 1) * 128], m_[:, kc, 0:n], start=(kc == 0), stop=(kc == 7)), reads=[WO, m_], writes=[p])
                k.op("dve", lambda e: e.scalar_tensor_tensor(out=xb[:, oc, 0:n], in0=p[:, 0:n], scalar=modT[l][:, 16 + oc, s_:s_ + 1], in1=xb[:, oc, 0:n], op0=ALU.mult, op1=ALU.add),
                     reads=[p, modT[l], xb], writes=[xb])
            k.dma("sp", XTv[:, :, t0:t0 + n], xb[:, :, 0:n], reads=[xb], writes=[XT])

    rw_in = ein("routerT", [L, 128, 8, 36])
    rb_in = ein("router_b", [L, 36])
    wgu_in = ein("exp_w_gu", [L, 32, D, D])
    wdn_in = ein("exp_w_down", [L, 32, 512, D])

    def phase_F(l):
        RW = k.sbuf("pf_rw", [128, 8, 36], F32)
        RB = k.sbuf("pf_rb", [128, 36], F32)
        k.dma("sp", RW[:], rw_in[l], reads=[rw_in], writes=[RW])
        k.dma("sp", RB[:], rb_in[l].partition_broadcast(128), reads=[rb_in], writes=[RB])
        xb = k.sbuf("pf_x", [128, 8, 512], F32)
        sq = k.sbuf("pf_sq", [128, 8, 512], F32)
        rr = k.sbuf("pf_r", [128, 512], F32)
        hf = k.sbuf("pf_hf", [128, 8, 512], F32)
        hbf = k.sbuf("pf_hb", [128, 8, 512], BF16)
        WT = k.sbuf("pf_wt", [128, 4, 32], F32)
        lg = k.sbuf("pf_lg", [128, 36], F32)
        gm = k.sbuf("pf_gm", [128, 1], F32)
        gmask = k.sbuf("pf_gmask", [128, 4], F32)
        gex = k.sbuf("pf_gex", [128, 4], F32)
        gw = k.sbuf("pf_gw", [128, 1], F32)
        e84 = k.sbuf("pf_e84", [128, 8, 4], F32)
        es = k.sbuf("pf_es", [128, 8], F32)
        m1 = k.sbuf("pf_m1", [128, 1], F32)
        m2 = k.sbuf("pf_m2", [128, 1], F32)
        k1 = k.sbuf("pf_k1", [128, 8], F32)
        k2 = k.sbuf("pf_k2", [128, 8], F32)
        es2 = k.sbuf("pf_es2", [128, 8], F32)
        p2 = k.sbuf("pf_p2", [128, 1], F32)
        w1 = k.sbuf("pf_w1", [128, 1], F32)
        w2 = k.sbuf("pf_w2", [128, 1], F32)
        wj = k.sbuf("pf_wj", [128, 8], F32)
        WGU = [k.sbuf("pf_wgu%d" % i, [128, 8, D], BF16) for i in range(2)]
        WDN = [k.sbuf("pf_wdn%d" % i, [128, 4, D], BF16) for i in range(2)]
        sg = [k.sbuf("pf_sg%d" % i, [128, 512], F32) for i in range(2)]
        act_ = [k.sbuf("pf_act%d" % i, [128, 4, 512], BF16) for i in range(2)]
        yacc = k.sbuf("pf_yacc", [128, 4, D], F32)
        for (t0, n) in BLOCKS:
            s_ = 1 if t0 == 0 else 0
            if s_ == 1 and l == DEPTH - 1:
                continue
            nj = n // 128
            k.dma("sp", xb[:, :, 0:n], XTv[:, :, t0:t0 + n], reads=[XT], writes=[xb])
            k.op("act", lambda e: e.activation(out=sq[:, :, 0:n], in_=xb[:, :, 0:n], func=AF.Square), reads=[xb], writes=[sq])
            p = ps()
            for kc in range(8):
                k.op("pe", lambda e, kc=kc: e.matmul(p[:, 0:n], ones_f[:], sq[:, kc, 0:n], start=(kc == 0), stop=(kc == 7)), reads=[ones_f, sq], writes=[p])
            k.op("act", lambda e: e.activation(out=rr[:, 0:n], in_=p[:, 0:n], func=AF.Sqrt, scale=1.0 / D, bias=EPS), reads=[p], writes=[rr])
            k.op("dve", lambda e: e.reciprocal(out=rr[:, 0:n], in_=rr[:, 0:n]), reads=[rr], writes=[rr])
            for kc in range(8):
                k.op("dve", lambda e, kc=kc: e.tensor_tensor(out=sq[:, kc, 0:n], in0=xb[:, kc, 0:n], in1=rr[:, 0:n], op=ALU.mult), reads=[xb, rr], writes=[sq])
                k.op("act", lambda e, kc=kc: e.activation(out=hf[:, kc, 0:n], in_=sq[:, kc, 0:n], func=AF.Identity, scale=A2[l][:, kc, s_:s_ + 1], bias=modT[l][:, 24 + kc, s_:s_ + 1]),
                     reads=[sq, A2[l], modT[l]], writes=[hf])
            k.op("pool", lambda e: e.tensor_copy(out=hbf[:, :, 0:n], in_=hf[:, :, 0:n]), reads=[hf], writes=[hbf])
            for j in range(nj):
                jsl = slice(j * 128, (j + 1) * 128)
                p = ps()
                for kc in range(8):
                    k.op("pe", lambda e, kc=kc: e.matmul(p[:, 0:36], hf[:, kc, jsl], RW[:, kc, :], start=(kc == 0), stop=(kc == 7)), reads=[hf, RW], writes=[p])
                k.op("dve", lambda e: e.tensor_tensor(out=lg[:], in0=p[:, 0:36], in1=RB[:], op=ALU.add), reads=[p, RB], writes=[lg])
                k.op("dve", lambda e: e.tensor_reduce(out=gm[:], in_=lg[:, 0:4], axis=AX.X, op=ALU.max), reads=[lg], writes=[gm])
                k.op("dve", lambda e: e.tensor_scalar(out=gmask[:], in0=lg[:, 0:4], scalar1=gm[:, 0:1], scalar2=None, op0=ALU.is_equal), reads=[lg, gm], writes=[gmask])
                k.op("dve", lambda e: e.tensor_scalar(out=gex[:], in0=lg[:, 0:4], scalar1=gm[:, 0:1], scalar2=None, op0=ALU.subtract), reads=[lg, gm], writes=[gex])
                k.op("act", lambda e: e.activation(out=gex[:], in_=gex[:], func=AF.Exp), reads=[gex], writes=[gex])
                k.op("dve", lambda e: e.tensor_reduce(out=gw[:], in_=gex[:], axis=AX.X, op=ALU.add), reads=[gex], writes=[gw])
                k.op("dve", lambda e: e.reciprocal(out=gw[:], in_=gw[:]), reads=[gw], writes=[gw])
                k.op("dve", lambda e: e.tensor_tensor(out=e84[:], in0=lg[:, 4:36].rearrange("p (g j) -> p j g", j=8), in1=gmask[:].unsqueeze(1).to_broadcast([128, 8, 4]), op=ALU.mult),
                     reads=[lg, gmask], writes=[e84])
                k.op("dve", lambda e: e.tensor_reduce(out=es[:], in_=e84[:], axis=AX.X, op=ALU.add), reads=[e84], writes=[es])
                k.op("dve", lambda e: e.tensor_reduce(out=m1[:], in_=es[:], axis=AX.X, op=ALU.max), reads=[es], writes=[m1])
                k.op("dve", lambda e: e.tensor_scalar(out=k1[:], in0=es[:], scalar1=m1[:, 0:1], scalar2=None, op0=ALU.is_equal), reads=[es, m1], writes=[k1])
                k.op("dve", lambda e: e.scalar_tensor_tensor(out=es2[:], in0=k1[:], scalar=-1e30, in1=es[:], op0=ALU.mult, op1=ALU.add), reads=[k1, es], writes=[es2])
                k.op("dve", lambda e: e.tensor_reduce(out=m2[:], in_=es2[:], axis=AX.X, op=ALU.max), reads=[es2], writes=[m2])
                k.op("dve", lambda e: e.tensor_scalar(out=k2[:], in0=es2[:], scalar1=m2[:, 0:1], scalar2=None, op0=ALU.is_equal), reads=[es2, m2], writes=[k2])
                k.op("dve", lambda e: e.tensor_tensor(out=p2[:], in0=m2[:], in1=m1[:], op=ALU.subtract), reads=[m1, m2], writes=[p2])
                k.op("act", lambda e: e.activation(out=p2[:], in_=p2[:], func=AF.Exp), reads=[p2], writes=[p2])
                k.op("dve", lambda e: e.tensor_scalar(out=w1[:], in0=p2[:], scalar1=1.0, scalar2=None, op0=ALU.add), reads=[p2], writes=[w1])
                k.op("dve", lambda e: e.reciprocal(out=w1[:], in_=w1[:]), reads=[w1], writes=[w1])
                k.op("dve", lambda e: e.tensor_tensor(out=w1[:], in0=w1[:], in1=gw[:], op=ALU.mult), reads=[w1, gw], writes=[w1])
                k.op("dve", lambda e: e.tensor_tensor(out=w2[:], in0=w1[:], in1=p2[:], op=ALU.mult), reads=[w1, p2], writes=[w2])
                k.op("dve", lambda e: e.tensor_scalar(out=wj[:], in0=k1[:], scalar1=w1[:, 0:1], scalar2=None, op0=ALU.mult), reads=[k1, w1], writes=[wj])
                k.op("dve", lambda e: e.scalar_tensor_tensor(out=wj[:], in0=k2[:], scalar=w2[:, 0:1], in1=wj[:], op0=ALU.mult, op1=ALU.add), reads=[k2, w2, wj], writes=[wj])
                k.op("dve", lambda e, j=j: e.tensor_tensor(out=WT[:, j, :].rearrange("p (g j) -> p g j", j=8), in0=gmask[:].unsqueeze(2).to_broadcast([128, 4, 8]),
                                                           in1=wj[:].unsqueeze(1).to_broadcast([128, 4, 8]), op=ALU.mult), reads=[gmask, wj], writes=[WT])
            for ex in range(32):
                wg_ = k.nxt("pf_wgu", WGU)
                wd_ = k.nxt("pf_wdn", WDN)
                for kc in range(8):
                    k.dma("pool", wg_[:, kc, :], wgu_in[l][ex][kc * 128:(kc + 1) * 128, :], reads=[wgu_in], writes=[wg_])
                for hk in range(4):
                    k.dma("pool", wd_[:, hk, :], wdn_in[l][ex][hk * 128:(hk + 1) * 128, :], reads=[wdn_in], writes=[wd_])
                a_ = k.nxt("pf_act", act_)
                for hc in range(4):
                    pg = ps()
                    pu = ps()
                    for kc in range(8):
                        k.op("pe", lambda e, kc=kc: e.matmul(pg[:, 0:n], wg_[:, kc, hc * 128:(hc + 1) * 128], hbf[:, kc, 0:n], start=(kc == 0), stop=(kc == 7)), reads=[wg_, hbf], writes=[pg])
                    for kc in range(8):
                        k.op("pe", lambda e, kc=kc: e.matmul(pu[:, 0:n], wg_[:, kc, 512 + hc * 128:512 + (hc + 1) * 128], hbf[:, kc, 0:n], start=(kc == 0), stop=(kc == 7)), reads=[wg_, hbf], writes=[pu])
                    s2 = k.nxt("pf_sg", sg)
                    k.op("act", lambda e: e.activation(out=s2[:, 0:n], in_=pg[:, 0:n], func=AF.Silu), reads=[pg], writes=[s2])
                    k.op("dve", lambda e, hc=hc: e.tensor_tensor(out=a_[:, hc, 0:n], in0=pu[:, 0:n], in1=s2[:, 0:n], op=ALU.mult), reads=[pu, s2], writes=[a_])
                for j in range(nj):
                    jsl = slice(j * 128, (j + 1) * 128)
                    for half in range(2):
                        py = ps()
                        for hk in range(4):
                            k.op("pe", lambda e, hk=hk: e.matmul(py[:, :], a_[:, hk, jsl], wd_[:, hk, half * 512:(half + 1) * 512], start=(hk == 0), stop=(hk == 3)), reads=[a_, wd_], writes=[py])
                        if ex == 0:
                            k.op("dve", lambda e: e.tensor_scalar(out=yacc[:, j, half * 512:(half + 1) * 512], in0=py[:, :], scalar1=WT[:, j, ex:ex + 1], scalar2=None, op0=ALU.mult),
                                 reads=[py, WT], writes=[yacc])
                        else:
                            k.op("dve", lambda e: e.scalar_tensor_tensor(out=yacc[:, j, half * 512:(half + 1) * 512], in0=py[:, :], scalar=WT[:, j, ex:ex + 1], in1=yacc[:, j, half * 512:(half + 1) * 512],
                                                                         op0=ALU.mult, op1=ALU.add), reads=[py, WT, yacc], writes=[yacc])
            for j in range(nj):
                jsl = slice(j * 128, (j + 1) * 128)
                for half in range(2):
                    p = ps()
                    for q in range(4):
                        oc = half * 4 + q
                        k.op("pe", lambda e, q=q, oc=oc: e.transpose(p[:, q * 128:(q + 1) * 128], yacc[:, j, oc * 128:(oc + 1) * 128], ident[:]), reads=[yacc, ident], writes=[p])
                    for q in range(4):
                        oc = half * 4 + q
                        k.op("dve", lambda e, q=q, oc=oc: e.scalar_tensor_tensor(out=xb[:, oc, jsl], in0=p[:, q * 128:(q + 1) * 128], scalar=modT[l][:, 40 + oc, s_:s_ + 1], in1=xb[:, oc, jsl],
                                                                                 op0=ALU.mult, op1=ALU.add), reads=[p, modT[l], xb], writes=[xb])
            k.dma("sp", XTv[:, :, t0:t0 + n], xb[:, :, 0:n], reads=[xb], writes=[XT])

    for l in range(n_layers):
        if stage >= 1:
            with k.scope():
                phase_A(l)
        if stage >= 2 and 'B' not in DBG_SKIP:
            with k.scope():
                phase_B(l)
        if stage >= 3 and 'D' not in DBG_SKIP:
            with k.scope():
                phase_D(l)
        if stage >= 4 and 'C' not in DBG_SKIP:
            with k.scope():
                phase_C(l)
        if stage >= 5:
            with k.scope():
                phase_E(l)
        if stage >= 6:
            with k.scope():
                phase_F(l)

    with k.scope():
        if dbg_out:
            tq = k.sbuf("dbgq", [128, NT], BF16)
            tf = k.sbuf("dbgf", [128, NPAD], F32)

        def dump_bf(src_ap, srcbuf, dst_ap, dstbuf):
            k.dma("sp", tq[:], src_ap, reads=[srcbuf], writes=[tq])
            k.op("dve", lambda e: e.tensor_copy(out=tf[:, 0:NT], in_=tq[:]), reads=[tq], writes=[tf])
            k.dma("sp", dst_ap, tf[:, 0:NT], reads=[tf], writes=[dstbuf])

        def dump_f(src, dst, rows, cols):
            for r0 in range(0, rows, 128):
                nr = min(128, rows - r0)
                k.dma("sp", tf[0:nr, 0:cols], src[r0:r0 + nr, :], reads=[src], writes=[tf])
                k.dma("sp", dst[r0:r0 + nr, :], tf[0:nr, 0:cols], reads=[tf], writes=[dst])
        for name in dbg_out:
            if name == "QT":
                dump_bf(QT[0], QT, dbg_out["QT"][:], dbg_out["QT"])
            if name == "KT":
                dump_bf(KT[1], KT, dbg_out["KT"][:], dbg_out["KT"])
            if name == "MIXT":
                for r0 in range(0, 1024, 128):
                    dump_bf(MIXT[r0:r0 + 128, :], MIXT, dbg_out["MIXT"][r0:r0 + 128, :], dbg_out["MIXT"])
            if name == "RZT":
                dump_f(RZT, dbg_out["RZT"], 960, NPAD)
            if name == "MQKT":
                dump_f(MQKT, dbg_out["MQKT"], 512, NPAD)
            if name == "MGT":
                dump_f(MGT, dbg_out["MGT"], 16, NT)
            if name == "XT":
                dump_f(XT, dbg_out["XT"], D, NT)

    with k.scope():
        fin_x = [k.sbuf("finx%d" % i, [128, 8, 512], F32) for i in range(2)]
        fin_sq = k.sbuf("finsq", [128, 8, 512], F32)
        fin_r = k.sbuf("finr", [128, 512], F32)
        fin_o = [k.sbuf("fino%d" % i, [128, D], F32) for i in range(2)]
        for (t0, n) in BLOCKS[1:]:
            xb = k.nxt("finx", fin_x)
            k.dma("sp", xb[:], XTv[:, :, t0:t0 + n], reads=[XT], writes=[xb])
            k.op("act", lambda e: e.activation(out=fin_sq[:], in_=xb[:], func=AF.Square), reads=[xb], writes=[fin_sq])
            p = ps()
            for kc in range(8):
                k.op("pe", lambda e, kc=kc: e.matmul(p[:, :], ones_f[:], fin_sq[:, kc, :], start=(kc == 0), stop=(kc == 7)),
                     reads=[ones_f, fin_sq], writes=[p])
            k.op("act", lambda e: e.activation(out=fin_r[:], in_=p[:, :], func=AF.Sqrt, scale=1.0 / D, bias=EPS), reads=[p], writes=[fin_r])
            k.op("dve", lambda e: e.reciprocal(out=fin_r[:], in_=fin_r[:]), reads=[fin_r], writes=[fin_r])
            for kc in range(8):
                k.op("dve", lambda e, kc=kc: e.scalar_tensor_tensor(out=xb[:, kc, :], in0=xb[:, kc, :], scalar=fg[:, kc:kc + 1], in1=fin_r[:],
                                                                    op0=ALU.mult, op1=ALU.mult), reads=[xb, fg, fin_r], writes=[xb])
            for j in range(n // 128):
                fo = k.nxt("fino", fin_o)
                for half in range(2):
                    p = ps()
                    for q in range(4):
                        kc = half * 4 + q
                        k.op("pe", lambda e, kc=kc, q=q, j=j: e.transpose(p[:, q * 128:(q + 1) * 128], xb[:, kc, j * 128:(j + 1) * 128], ident[:]),
                             reads=[xb, ident], writes=[p])
                    if half == 0:
                        k.op("act", lambda e: e.copy(out=fo[:, 0:512], in_=p[:, :]), reads=[p], writes=[fo])
                    else:
                        k.op("dve", lambda e: e.tensor_copy(out=fo[:, 512:1024], in_=p[:, :]), reads=[p], writes=[fo])
                r0 = t0 - TC + j * 128
                k.dma("sp", out[r0:r0 + 128, :], fo[:], reads=[fo], writes=[out])


def host_inputs(inputs, b):
    f = np.float32
    L = DEPTH
    c2 = np.stack([inputs["c"][b], inputs["c_ctx"]], 0).astype(f)
    c2T = np.ascontiguousarray(c2.reshape(2, 8, 128).transpose(2, 1, 0))
    perm = rope_partner_perm()
    w_in = inputs["w_in"]
    qk = w_in[:, :, 0:1024].reshape(L, D, 16, 64)
    w_inp = np.ascontiguousarray(qk[:, :, :, perm].reshape(L, D, 1024))
    cos, sin = rope_tables()
    m = {
        "x": np.ascontiguousarray(inputs["x"][b]),
        "ctx": np.ascontiguousarray(inputs["ctx"][b]),
        "c2T": c2T,
        "ada_w": inputs["ada_w"],
        "ada_bT": np.ascontiguousarray(inputs["ada_b"].reshape(L, 48, 128).transpose(0, 2, 1)),
        "n1gT": np.ascontiguousarray(inputs["norm1_g"].reshape(L, 8, 128).transpose(0, 2, 1)),
        "n2gT": np.ascontiguousarray(inputs["norm2_g"].reshape(L, 8, 128).transpose(0, 2, 1)),
        "w_in": w_in,
        "w_inp": w_inp,
        "w_out": inputs["w_out"],
        "cos_t": cos,
        "sin_t": sin,
        "ident": np.eye(128, dtype=f),
        "fgT": np.ascontiguousarray(inputs["final_g"].reshape(8, 128).T),
        "da_lambda": np.ascontiguousarray(inputs["da_lambda"].reshape(L, 256)),
        "sublnT": np.ascontiguousarray(inputs["da_subln_g"].T),
        "mcwT": np.ascontiguousarray(inputs["ml_conv_w"].reshape(L, 3, 4, 128).transpose(0, 3, 2, 1)),
        "mcbT": np.ascontiguousarray(inputs["ml_conv_b"].reshape(L, 4, 128).transpose(0, 2, 1)),
        "mgbT": np.ascontiguousarray(inputs["ml_gate_b"].reshape(L, 16, 1)),
        "mngT": np.ascontiguousarray(inputs["ml_norm_g"].reshape(L, 2, 128).transpose(0, 2, 1)),
        "triu": np.triu(np.ones((128, 128), f)),
        "tril": np.tril(np.ones((128, 128), f)),
        "routerT": np.ascontiguousarray(np.concatenate([inputs["router_wg"], inputs["router_we"]], -1).reshape(L, 8, 128, 36).transpose(0, 2, 1, 3)),
        "router_b": np.ascontiguousarray(np.concatenate([inputs["router_bg"], inputs["router_be"]], -1)),
        "rw_par": rw_par(inputs),
        "rw_w2p": rw_pad(inputs["rw_w2"]),
        "rw_a2p": rw_pad(inputs["rw_a2"]),
        "rw_g2": inputs["rw_g2"],
        "rw_lnp": np.ascontiguousarray(np.stack([inputs["rw_ln_g"].reshape(L, 2, 128), inputs["rw_ln_b"].reshape(L, 2, 128)], -1).transpose(0, 2, 1, 3)),
        "rw_mask": rw_masks(),
        "rw_reset": rw_reset(),
        "exp_w_gu": inputs["exp_w_gu"],
        "exp_w_down": inputs["exp_w_down"],
    }
    return m


def rw_par(inputs):
    L = DEPTH
    P = np.zeros((L, 64, 48), np.float32)
    mu = inputs["rw_shift_mu"]
    for h in range(4):
        P[:, :, 0 + h] = mu[:, h * 64:(h + 1) * 64]
        P[:, :, 4 + h] = mu[:, 256 + h * 64:256 + (h + 1) * 64]
        P[:, :, 8 + h] = mu[:, 512 + h * 64:512 + (h + 1) * 64]
        P[:, :, 31 + h] = inputs["rw_k_k"][:, h * 64:(h + 1) * 64]
        P[:, :, 35 + h] = inputs["rw_k_a"][:, h * 64:(h + 1) * 64]
        P[:, :, 43 + h] = inputs["rw_r_k"][:, h, :]
        for d in range(2):
            P[:, :, 15 + d * 4 + h] = inputs["rw_w0"][:, d, h * 64:(h + 1) * 64]
            P[:, :, 23 + d * 4 + h] = inputs["rw_a0"][:, d, h * 64:(h + 1) * 64]
    P[:, :, 12] = mu[:, 768:832]
    P[:, :, 13] = mu[:, 832:896]
    P[:, :, 14] = mu[:, 896:960]
    return P


def rw_pad(w):
    L = w.shape[0]
    o = np.zeros((L, 2, 64, 256), np.float32)
    o[:, 0, 0:32] = w[:, 0]
    o[:, 1, 32:64] = w[:, 1]
    return o


def rw_masks():
    f = np.float32
    su = np.triu(np.ones((64, 64), f), 1)
    iu = np.triu(np.ones((64, 64), f), 0)
    sl = np.tril(np.ones((64, 64), f), -1)
    il = np.tril(np.ones((64, 64), f), 0)
    m = np.zeros((2, 64, 5, 64), f)
    m[0, :, 0] = -su; m[0, :, 1] = -sl; m[0, :, 2] = su; m[0, :, 3] = -iu; m[0, :, 4] = iu
    m[1, :, 0] = -sl; m[1, :, 1] = -su; m[1, :, 2] = sl; m[1, :, 3] = -il; m[1, :, 4] = il
    return m


def rw_reset():
    r = np.ones((64, 512), np.float32)
    r[:, ::64] = 0.0
    return r


def kernel(**inputs):
    inputs = {k_: np.asarray(v) for k_, v in inputs.items()}
    nc = build()
    in_maps = [host_inputs(inputs, b) for b in range(8)]
    res = run_bass_kernel_spmd(nc, in_maps, core_ids=list(range(8)))
    return np.stack([r["out"] for r in res.results], 0).astype(np.float32)
```

```python
import math
import os
DBG_STOP = int(os.environ.get('DBG_STOP', '99'))
DBG_SKIP = os.environ.get('DBG_SKIP', '')
DSTOP = int(os.environ.get('DSTOP', '99'))
SSTOP = int(os.environ.get('SSTOP', '99'))
NSTEP = int(os.environ.get('NSTEP', '68'))
import numpy as np
from contextlib import ExitStack
import concourse.bass as bass
import concourse.mybir as mybir
from concourse.alu_op_type import AluOpType as ALU
from concourse.bass_utils import run_bass_kernel_spmd

AF = mybir.ActivationFunctionType
AX = mybir.AxisListType
F32 = mybir.dt.float32
BF16 = mybir.dt.bfloat16
I32 = mybir.dt.int32
U32 = mybir.dt.uint32

D = 1024
T = 4096
TC = 256
NT = T + TC
NTILE = NT // 128
DEPTH = 4
IN_COLS = 3536
DA0, RW0, ML0 = 0, 1536, 2496
EPS = 1e-6
BLOCKS = [(0, 256)] + [(256 + 512 * i, 512) for i in range(8)]
NPAD = 4356


def padcol(t):
    return t + 1 if t < 256 else t + 3


class Buf:
    def __init__(self, t, name=""):
        self.t = t
        self.name = name
        self.w = {}
        self.r = {}

    def __getitem__(self, idx):
        return self.t[idx]


class Ctx:
    NPOOL = 12

    def __init__(self, nc, stack, same_engine_sync=True):
        self.nc = nc
        self.st = stack
        self.same = same_engine_sync
        self.eng = dict(pe=nc.tensor, dve=nc.vector, act=nc.scalar, pool=nc.gpsimd, sp=nc.sync)
        self.semh = {}
        self.cnt = {}
        for e in self.eng:
            self.semh[e] = stack.enter_context(nc.semaphore("s_" + e))
            self.cnt[e] = 0
        self.seen = {e: {} for e in self.eng}
        self.dq = {}
        for q in ("sp", "pool", "act"):
            lst = []
            for i in range(self.NPOOL):
                key = "d_%s_%d" % (q, i)
                self.semh[key] = stack.enter_context(nc.semaphore(key))
                self.cnt[key] = 0
                lst.append(key)
            self.dq[q] = [lst, 0]
        self.ninstr = 0
        self.rr = {}

    def sbuf(self, name, shape, dt):
        self.uid = getattr(self, "uid", 0) + 1
        return Buf(self.st.enter_context(self.nc.sbuf_tensor("sb%d_%s" % (self.uid, name), list(shape), dt)), name)

    def psum(self, name, shape, dt=F32):
        b = Buf(self.st.enter_context(self.nc.psum_tensor("pp_" + name, list(shape), dt)), name)
        b.psum = True
        return b

    def dram(self, name, shape, dt, kind="Internal"):
        return Buf(self.nc.dram_tensor(name, list(shape), dt, kind=kind), name)

    def _wait(self, e, deps):
        eng = self.eng[e]
        seen = self.seen[e]
        for key, val in deps.items():
            if key == e and (e == "pe" or not self.same):
                continue
            if seen.get(key, 0) >= val:
                continue
            eng.wait_ge(self.semh[key], val)
            seen[key] = val

    @staticmethod
    def _merge(d, s):
        for k, v in s.items():
            if d.get(k, 0) < v:
                d[k] = v

    def _deps(self, reads, writes):
        deps = {}
        for b in reads:
            self._merge(deps, b.w)
            if getattr(b, "psum", False):
                self._merge(deps, b.r)
        for b in writes:
            self._merge(deps, b.w)
            self._merge(deps, b.r)
        return deps

    def _mark(self, key, val, reads, writes):
        for b in reads:
            if b.r.get(key, 0) < val:
                b.r[key] = val
        for b in writes:
            if b.w.get(key, 0) < val:
                b.w[key] = val

    def op(self, e, fn, reads=(), writes=()):
        self._wait(e, self._deps(reads, writes))
        ins = fn(self.eng[e])
        self.cnt[e] += 1
        ins.then_inc(self.semh[e], 1)
        self._mark(e, self.cnt[e], reads, writes)
        self.ninstr += 1
        return ins

    def dma(self, q, out, in_, reads=(), writes=(), **kw):
        lst, rr = self.dq[q]
        key = lst[rr % len(lst)]
        self.dq[q][1] = rr + 1
        deps = self._deps(reads, writes)
        deps[key] = max(deps.get(key, 0), self.cnt[key])
        self._wait(q, deps)
        ins = self.eng[q].dma_start(out=out, in_=in_, **kw)
        self.cnt[key] += 16
        ins.then_inc(self.semh[key], 16)
        self._mark(key, self.cnt[key], reads, writes)
        self.ninstr += 1
        return ins

    def finish(self, e="sp"):
        deps = {}
        for q in self.dq:
            for key in self.dq[q][0]:
                if self.cnt[key]:
                    deps[key] = self.cnt[key]
        for k in self.eng:
            if self.cnt[k]:
                deps[k] = self.cnt[k]
        self._wait(e, deps)

    def barrier(self):
        deps = {}
        for q in self.dq:
            for key in self.dq[q][0]:
                if self.cnt[key]:
                    deps[key] = self.cnt[key]
        for e in self.eng:
            if self.cnt[e]:
                deps[e] = self.cnt[e]
        for e in self.eng:
            self._wait(e, deps)

    def scope(self):
        ctx = self

        class _S:
            def __enter__(s_):
                s_.old = ctx.st
                s_.sub = ExitStack()
                ctx.st = s_.sub
                return s_

            def __exit__(s_, *a):
                if a[0] is None:
                    ctx.barrier()
                s_.sub.close()
                ctx.st = s_.old
                return False
        return _S()

    def nxt(self, name, lst):
        i = self.rr.get(name, 0)
        self.rr[name] = i + 1
        return lst[i % len(lst)]


def rope_tables():
    nf = 16
    inv = 10000.0 ** (-np.arange(nf, dtype=np.float32) / nf)
    t = np.arange(T)
    row = (t // 64).astype(np.float32)
    col = (t % 64).astype(np.float32)
    cos = np.ones((128, NT), np.float32)
    sin = np.zeros((128, NT), np.float32)
    for p in range(128):
        d = p % 64
        pos = row if d < 32 else col
        j = d % 32
        f = j % 16
        ang = pos * inv[f]
        cos[p, TC:] = np.cos(ang)
        s = np.sin(ang)
        sin[p, TC:] = -s if j < 16 else s
    return cos, sin


def rope_partner_perm():
    perm = np.zeros(64, np.int64)
    for d in range(64):
        j = d % 32
        perm[d] = d + 16 if j < 16 else d - 16
    return perm


def build(n_layers=DEPTH, stage=99, dbg=()):
    nc = bass.Bass("TRN2", target_bir_lowering=False)
    st = ExitStack()
    with st:
        k = Ctx(nc, st)
        build_body(nc, k, n_layers, stage, dbg)
        k.finish()
        print("instructions:", k.ninstr, {e: k.cnt[e] for e in k.eng})
    return nc


def build_body(nc, k, n_layers, stage, dbg):
    L = DEPTH
    ein = lambda name, shape, dt=F32: k.dram(name, shape, dt, kind="ExternalInput")
    x_in = ein("x", [T, D])
    ctx_in = ein("ctx", [TC, D])
    c2T_in = ein("c2T", [128, 8, 2])
    ada_w = ein("ada_w", [L, D, 6 * D])
    ada_bT = ein("ada_bT", [L, 128, 48])
    n1gT = ein("n1gT", [L, 128, 8])
    n2gT = ein("n2gT", [L, 128, 8])
    w_in = ein("w_in", [L, D, IN_COLS])
    w_inp = ein("w_inp", [L, D, 1024])
    w_out = ein("w_out", [L, D, D])
    cos_in = ein("cos_t", [128, NT])
    sin_in = ein("sin_t", [128, NT])
    ident_in = ein("ident", [128, 128])
    fgT = ein("fgT", [128, 8])
    out = k.dram("out", [T, D], F32, kind="ExternalOutput")
    dbg_out = {}
    for name, shape in dbg:
        dbg_out[name] = k.dram("dbg_" + name, shape, F32, kind="ExternalOutput")

    XT = k.dram("XT", [D, NT], F32)
    QT = k.dram("QT", [4, 128, NT], BF16)
    KT = k.dram("KT", [4, 128, NT], BF16)
    VA = k.dram("VA", [NT, 4 * 130], BF16)
    MIXT = k.dram("MIXT", [D, NT], BF16)

    ident = k.sbuf("ident", [128, 128], F32)
    identb = k.sbuf("identb", [128, 128], BF16)
    ones_f = k.sbuf("ones_f", [128, 128], F32)
    k.dma("sp", ident[:], ident_in[:], reads=[ident_in], writes=[ident])
    k.op("dve", lambda e: e.tensor_copy(out=identb[:], in_=ident[:]), reads=[ident], writes=[identb])
    k.op("dve", lambda e: e.memset(ones_f[:], 1.0), writes=[ones_f])

    PS = [k.psum("ps%d" % i, [128, 512], F32) for i in range(8)]

    def ps():
        return k.nxt("ps", PS)

    c2T = k.sbuf("c2T", [128, 8, 2], F32)
    sc2T = k.sbuf("sc2T", [128, 8, 2], F32)
    k.dma("sp", c2T[:], c2T_in[:], reads=[c2T_in], writes=[c2T])
    k.op("act", lambda e: e.activation(out=sc2T[:], in_=c2T[:], func=AF.Silu), reads=[c2T], writes=[sc2T])
    modT = [k.sbuf("modT%d" % l, [128, 48, 2], F32) for l in range(L)]
    adab = [k.sbuf("adab%d" % l, [128, 48], F32) for l in range(L)]
    n1g = [k.sbuf("n1g%d" % l, [128, 8], F32) for l in range(L)]
    n2g = [k.sbuf("n2g%d" % l, [128, 8], F32) for l in range(L)]
    A1 = [k.sbuf("A1_%d" % l, [128, 8, 2], F32) for l in range(L)]
    A2 = [k.sbuf("A2_%d" % l, [128, 8, 2], F32) for l in range(L)]
    fg = k.sbuf("fg", [128, 8], F32)
    k.dma("sp", fg[:], fgT[:], reads=[fgT], writes=[fg])
    with k.scope():
        adaw_sb = [k.sbuf("adaw%d" % i, [128, 8, 768], F32) for i in range(2)]
        for l in range(n_layers):
            k.dma("sp", adab[l][:], ada_bT[l], reads=[ada_bT], writes=[adab[l]])
            k.dma("sp", n1g[l][:], n1gT[l], reads=[n1gT], writes=[n1g[l]])
            k.dma("sp", n2g[l][:], n2gT[l], reads=[n2gT], writes=[n2g[l]])
            for piece in range(8):
                wsb = k.nxt("adaw", adaw_sb)
                for kc in range(8):
                    k.dma("sp" if kc % 2 == 0 else "pool", wsb[:, kc, :],
                          ada_w[l][kc * 128:(kc + 1) * 128, piece * 768:(piece + 1) * 768],
                          reads=[ada_w], writes=[wsb])
                p = ps()
                for cc in range(6):
                    for kc in range(8):
                        k.op("pe", lambda e, cc=cc, kc=kc: e.matmul(
                            p[:, cc * 2:(cc + 1) * 2], wsb[:, kc, cc * 128:(cc + 1) * 128], sc2T[:, kc, :],
                            start=(kc == 0), stop=(kc == 7)), reads=[wsb, sc2T], writes=[p])
                k.op("dve", lambda e, piece=piece: e.tensor_tensor(
                    out=modT[l][:, piece * 6:(piece + 1) * 6, :],
                    in0=p[:, 0:12].rearrange("p (c s) -> p c s", s=2),
                    in1=adab[l][:, piece * 6:(piece + 1) * 6].unsqueeze(2).to_broadcast([128, 6, 2]),
                    op=ALU.add), reads=[p, adab[l]], writes=[modT[l]])
            for (A, g, c0) in ((A1[l], n1g[l], 8), (A2[l], n2g[l], 32)):
                k.op("dve", lambda e, A=A, g=g, c0=c0: e.scalar_tensor_tensor(
                    out=A[:], in0=modT[l][:, c0:c0 + 8, :], scalar=1.0,
                    in1=g[:].unsqueeze(2).to_broadcast([128, 8, 2]), op0=ALU.add, op1=ALU.mult),
                    reads=[modT[l], g], writes=[A])
        if "modT" in dbg_out:
            k.dma("sp", dbg_out["modT"][:], modT[0][:], reads=[modT[0]], writes=[dbg_out["modT"]])

        xin_sb = [k.sbuf("xin%d" % i, [128, D], F32) for i in range(2)]
        xtr_sb = [k.sbuf("xtr%d" % i, [128, 8, 128], F32) for i in range(2)]
        XTv = XT.t.ap().rearrange("(kc p) t -> p kc t", p=128)
        for tt in range(NTILE):
            xs = k.nxt("xin", xin_sb)
            src = ctx_in[tt * 128:(tt + 1) * 128, :] if tt < 2 else x_in[(tt - 2) * 128:(tt - 1) * 128, :]
            k.dma("sp" if tt % 2 == 0 else "pool", xs[:], src, reads=[], writes=[xs])
            xo = k.nxt("xtr", xtr_sb)
            for half in range(2):
                p = ps()
                for j in range(4):
                    kc = half * 4 + j
                    k.op("pe", lambda e, kc=kc, j=j: e.transpose(p[:, j * 128:(j + 1) * 128], xs[:, kc * 128:(kc + 1) * 128], ident[:]),
                         reads=[xs, ident], writes=[p])
                k.op("act" if half == 0 else "dve",
                     (lambda e, half=half: e.copy(out=xo[:, half * 4:(half + 1) * 4, :], in_=p[:, :].rearrange("p (j t) -> p j t", t=128))) if half == 0 else
                     (lambda e, half=half: e.tensor_copy(out=xo[:, half * 4:(half + 1) * 4, :], in_=p[:, :].rearrange("p (j t) -> p j t", t=128))),
                     reads=[p], writes=[xo])
            k.dma("sp", XTv[:, :, tt * 128:(tt + 1) * 128], xo[:], reads=[xo], writes=[XT])

    da_lam_in = ein("da_lambda", [L, 256])
    sublnT_in = ein("sublnT", [128, L])
    RZT = k.dram("RZT", [960, NPAD], F32)
    MQKT = k.dram("MQKT", [512, NPAD], F32)
    MOT = k.dram("MOT", [256, NT], F32)
    MVT = k.dram("MVT", [256, NT], F32)
    MGT = k.dram("MGT", [16, NT], F32)
    HFB = [k.dram("HF", [NT, 256], F32), k.dram("HB", [NT, 256], F32)]
    zero_sb = k.sbuf("zero_sb", [128, 8], F32)
    k.op("dve", lambda e: e.memset(zero_sb[:], 0.0), writes=[zero_sb])
    for (dst, rows) in (() if 'z' in DBG_SKIP else ((RZT, 960), (MQKT, 512))):
        for r0 in range(0, rows, 128):
            nr = min(128, rows - r0)
            for c0, w in ((0, 1), (257, 2), (4355, 1)):
                k.dma("sp", dst[r0:r0 + nr, c0:c0 + w], zero_sb[0:nr, 0:w], reads=[zero_sb], writes=[dst], allow_slow_non_contiguous=True)
    sublnT = k.sbuf("sublnT", [128, L], F32)
    k.dma("sp", sublnT[:], sublnT_in[:], reads=[sublnT_in], writes=[sublnT])

    def phase_A(l):
        WI = k.sbuf("WI", [128, 8, IN_COLS], BF16)
        WIP = k.sbuf("WIP", [128, 8, 1024], BF16)
        pa_x = [k.sbuf("pa_x%d" % i, [128, 8, 512], F32) for i in range(2)]
        pa_sq = k.sbuf("pa_sq", [128, 8, 512], F32)
        pa_r = k.sbuf("pa_r", [128, 512], F32)
        pa_tmp = [k.sbuf("pa_tmp%d" % i, [128, 512], F32) for i in range(2)]
        pa_h = k.sbuf("pa_h", [128, 8, 512], BF16)
        pa_cos = k.sbuf("pa_cos", [128, 512], F32)
        pa_sin = k.sbuf("pa_sin", [128, 512], F32)
        pa_t1 = [k.sbuf("pa_t1_%d" % i, [128, 512], F32) for i in range(2)]
        pa_t2 = [k.sbuf("pa_t2_%d" % i, [128, 512], F32) for i in range(2)]
        pa_ob = [k.sbuf("pa_ob%d" % i, [128, 512], BF16) for i in range(3)]
        pa_of = [k.sbuf("pa_of%d" % i, [128, 512], F32) for i in range(3)]
        pa_va = [k.sbuf("pa_va%d" % i, [128, 4, 130], BF16) for i in range(2)]
        for b_ in ([] if 'm' in DBG_SKIP else pa_va):
            k.op("pool", lambda e, b_=b_: e.memset(b_[:], 1.0), writes=[b_])

        evac_rr = [0]

        def evac_copy(dst_ap, src_ap, reads, writes):
            evac_rr[0] += 1
            if evac_rr[0] % 2 == 0:
                k.op("act", lambda e: e.copy(out=dst_ap, in_=src_ap), reads=reads, writes=writes)
            else:
                k.op("dve", lambda e: e.tensor_copy(out=dst_ap, in_=src_ap), reads=reads, writes=writes)

        for kc in range(0 if 'w' in DBG_SKIP else 8):
            for c0 in range(0, IN_COLS, 1768):
                k.dma("pool", WI[:, kc, c0:c0 + 1768], w_in[l][kc * 128:(kc + 1) * 128, c0:c0 + 1768], reads=[w_in], writes=[WI])
            k.dma("pool", WIP[:, kc, :], w_inp[l][kc * 128:(kc + 1) * 128, :], reads=[w_inp], writes=[WIP])
        if DBG_STOP <= 1:
            return
        for (t0, n) in BLOCKS:
            s = 1 if t0 == 0 else 0
            xb = k.nxt("pa_x", pa_x)
            k.dma("sp", xb[:, :, 0:n], XTv[:, :, t0:t0 + n], reads=[XT], writes=[xb])
            k.dma("sp", pa_cos[:, 0:n], cos_in[:, t0:t0 + n], reads=[cos_in], writes=[pa_cos])
            k.dma("sp", pa_sin[:, 0:n], sin_in[:, t0:t0 + n], reads=[sin_in], writes=[pa_sin])
            k.op("act", lambda e: e.activation(out=pa_sq[:, :, 0:n], in_=xb[:, :, 0:n], func=AF.Square), reads=[xb], writes=[pa_sq])
            p = ps()
            for kc in range(8):
                k.op("pe", lambda e, kc=kc: e.matmul(p[:, 0:n], ones_f[:], pa_sq[:, kc, 0:n], start=(kc == 0), stop=(kc == 7)),
                     reads=[ones_f, pa_sq], writes=[p])
            k.op("act", lambda e: e.activation(out=pa_r[:, 0:n], in_=p[:, 0:n], func=AF.Sqrt, scale=1.0 / D, bias=EPS), reads=[p], writes=[pa_r])
            k.op("dve", lambda e: e.reciprocal(out=pa_r[:, 0:n], in_=pa_r[:, 0:n]), reads=[pa_r], writes=[pa_r])
            for kc in range(8):
                tmp = k.nxt("pa_tmp", pa_tmp)
                k.op("dve", lambda e, kc=kc: e.tensor_tensor(out=tmp[:, 0:n], in0=xb[:, kc, 0:n], in1=pa_r[:, 0:n], op=ALU.mult),
                     reads=[xb, pa_r], writes=[tmp])
                k.op("act", lambda e, kc=kc: e.activation(out=pa_h[:, kc, 0:n], in_=tmp[:, 0:n], func=AF.Identity,
                                                          scale=A1[l][:, kc, s:s + 1], bias=modT[l][:, kc, s:s + 1]),
                     reads=[tmp, A1[l], modT[l]], writes=[pa_h])

            if DBG_STOP <= 2:
                continue

            def fm(Wb, c0, ncols=128):
                p = ps()
                for kc in range(8):
                    k.op("pe", lambda e, kc=kc: e.matmul(p[0:ncols, 0:n], Wb[:, kc, c0:c0 + ncols], pa_h[:, kc, 0:n], start=(kc == 0), stop=(kc == 7)),
                         reads=[Wb, pa_h], writes=[p])
                return p

            for which, dst in ((0, QT), (1, KT)):
                for h in range(4):
                    c0 = which * 512 + h * 128
                    p1 = fm(WI, c0)
                    p2 = fm(WIP, c0)
                    t1 = k.nxt("pa_t1", pa_t1)
                    t2 = k.nxt("pa_t2", pa_t2)
                    ob = k.nxt("pa_ob", pa_ob)
                    k.op("dve", lambda e: e.tensor_tensor(out=t1[:, 0:n], in0=p1[:, 0:n], in1=pa_cos[:, 0:n], op=ALU.mult), reads=[p1, pa_cos], writes=[t1])
                    k.op("dve", lambda e: e.tensor_tensor(out=t2[:, 0:n], in0=p2[:, 0:n], in1=pa_sin[:, 0:n], op=ALU.mult), reads=[p2, pa_sin], writes=[t2])
                    k.op("pool", lambda e: e.tensor_tensor(out=ob[:, 0:n], in0=t1[:, 0:n], in1=t2[:, 0:n], op=ALU.add), reads=[t1, t2], writes=[ob])
                    k.dma("sp", dst[h][:, t0:t0 + n], ob[:, 0:n], reads=[ob], writes=[dst])
            if DBG_STOP <= 3:
                continue
            pc = padcol(t0)
            for j in range(8):
                ncols = 128 if j < 7 else 64
                p1 = fm(WI, RW0 + j * 128, ncols)
                of = k.nxt("pa_of", pa_of)
                evac_copy(of[0:ncols, 0:n], p1[0:ncols, 0:n], [p1], [of])
                k.dma("sp", RZT[j * 128:j * 128 + ncols, pc:pc + n], of[0:ncols, 0:n], reads=[of], writes=[RZT])
            for j in range(4):
                p1 = fm(WI, ML0 + j * 128)
                of = k.nxt("pa_of", pa_of)
                evac_copy(of[:, 0:n], p1[:, 0:n], [p1], [of])
                k.dma("sp", MQKT[j * 128:(j + 1) * 128, pc:pc + n], of[:, 0:n], reads=[of], writes=[MQKT])
            for j in range(2):
                p1 = fm(WI, ML0 + 768 + j * 128)
                of = k.nxt("pa_of", pa_of)
                k.op("act", lambda e: e.activation(out=of[:, 0:n], in_=p1[:, 0:n], func=AF.Sigmoid), reads=[p1], writes=[of])
                k.dma("sp", MOT[j * 128:(j + 1) * 128, t0:t0 + n], of[:, 0:n], reads=[of], writes=[MOT])
            if DBG_STOP <= 4:
                continue
            for j in range(n // 128):
                r0 = t0 + j * 128
                p1 = ps()
                for kc in range(8):
                    k.op("pe", lambda e, kc=kc: e.matmul(p1[:, 0:512], pa_h[:, kc, j * 128:(j + 1) * 128], WI[:, kc, 1024:1536], start=(kc == 0), stop=(kc == 7)),
                         reads=[WI, pa_h], writes=[p1])
                va = k.nxt("pa_va", pa_va)
                evac_copy(va[:, :, 0:128], p1[:, 0:512].rearrange("p (h e) -> p h e", e=128), [p1], [va])
                k.dma("sp", VA[r0:r0 + 128, :], va[:].rearrange("p h e -> p (h e)"), reads=[va], writes=[VA])
            for j in range(2):
                p1 = fm(WI, ML0 + 512 + j * 128)
                of = k.nxt("pa_of", pa_of)
                evac_copy(of[:, 0:n], p1[:, 0:n], [p1], [of])
                k.dma("sp", MVT[j * 128:(j + 1) * 128, t0:t0 + n], of[:, 0:n], reads=[of], writes=[MVT])
            p1 = fm(WI, ML0 + 1024, 16)
            of = k.nxt("pa_of", pa_of)
            evac_copy(of[0:16, 0:n], p1[0:16, 0:n], [p1], [of])
            k.dma("sp", MGT[:, t0:t0 + n], of[0:16, 0:n], reads=[of], writes=[MGT])

    def phase_B(l):
        at_k = k.sbuf("at_k", [128, NT], BF16)
        at_q = k.sbuf("at_q", [128, NT], BF16)
        at_v = k.sbuf("at_v", [128, NTILE, 130], BF16)
        at_p = [k.sbuf("at_p%d" % i, [128, 512], BF16) for i in range(3)]
        at_o = [k.sbuf("at_o%d" % i, [128, 4, 128], F32) for i in range(2)]
        at_a = k.sbuf("at_a", [128, 4, 128], F32)
        at_sq = k.sbuf("at_sq", [128, 4, 128], F32)
        at_ss = k.sbuf("at_ss", [128, 4], F32)
        at_rec = k.sbuf("at_rec", [128, 4], F32)
        at_ob = [k.sbuf("at_ob%d" % i, [128, 512], BF16) for i in range(2)]
        dl = k.sbuf("dl", [128, 256], F32)
        dl_t = k.sbuf("dl_t", [128, 2, 64], F32)
        dl_s = k.sbuf("dl_s", [128, 2], F32)
        neglam = k.sbuf("neglam", [128, 1], F32)
        subg = k.sbuf("subg", [128, 1], F32)
        ACC = [k.psum("acc%d" % i, [128, 512], F32) for i in range(0)]

        lam_init = 0.8 - 0.6 * math.exp(-0.3 * l)
        k.dma("sp", dl[:], da_lam_in[l].partition_broadcast(128), reads=[da_lam_in], writes=[dl])
        dl4 = dl[:, :].rearrange("p (a b d) -> p a b d", a=2, b=2)
        k.op("dve", lambda e: e.tensor_tensor(out=dl_t[:], in0=dl4[:, :, 0, :], in1=dl4[:, :, 1, :], op=ALU.mult), reads=[dl], writes=[dl_t])
        k.op("dve", lambda e: e.tensor_reduce(out=dl_s[:], in_=dl_t[:], axis=AX.X, op=ALU.add), reads=[dl_t], writes=[dl_s])
        k.op("act", lambda e: e.activation(out=dl_s[:], in_=dl_s[:], func=AF.Exp), reads=[dl_s], writes=[dl_s])
        k.op("dve", lambda e: e.scalar_tensor_tensor(out=neglam[:], in0=dl_s[:, 1:2], scalar=-lam_init, in1=dl_s[:, 0:1], op0=ALU.add, op1=ALU.subtract),
             reads=[dl_s], writes=[neglam])
        k.op("dve", lambda e: e.tensor_scalar(out=subg[:], in0=sublnT[:, l:l + 1], scalar1=(1.0 - lam_init), scalar2=None, op0=ALU.mult),
             reads=[sublnT], writes=[subg])
        qsets = [(256 + 512 * i, 512, list(range(NTILE))) for i in range(8)]
        if l < DEPTH - 1:
            qsets = [(0, 256, [0, 1])] + qsets
        for h in range(4):
            k.dma("sp", at_k[:], KT[h], reads=[KT], writes=[at_k])
            k.dma("pool", at_q[:], QT[h], reads=[QT], writes=[at_q])
            k.dma("sp", at_v[:], VA.t.ap().rearrange("(t p) (h e) -> p t h e", p=128, e=130)[:, :, h, :], reads=[VA], writes=[at_v])
            for (q0, nq, kts) in qsets:
                nj = nq // 128
                for m in range(2):
                    accs = PS[0:4]
                    osb = at_o[m]
                    for ki, kt in enumerate(kts):
                        sp_ = k.nxt("psB", PS[4:8])
                        k.op("pe", lambda e: e.matmul(sp_[:, 0:nq], at_k[m * 64:(m + 1) * 64, kt * 128:(kt + 1) * 128], at_q[m * 64:(m + 1) * 64, q0:q0 + nq],
                                                      start=True, stop=True), reads=[at_k, at_q], writes=[sp_])
                        pt = k.nxt("at_p", at_p)
                        k.op("act", lambda e: e.activation(out=pt[:, 0:nq], in_=sp_[:, 0:nq], func=AF.Exp, scale=0.125), reads=[sp_], writes=[pt])
                        for j in range(nj):
                            acc = accs[j]
                            k.op("pe", lambda e, j=j: e.matmul(acc[:, 0:129], pt[:, j * 128:(j + 1) * 128], at_v[:, kt, 0:129],
                                                               start=(ki == 0), stop=(ki == len(kts) - 1)), reads=[pt, at_v], writes=[acc])
                    for j in range(nj):
                        acc = accs[j]
                        c0 = 0
                        k.op("dve", lambda e, j=j: e.reciprocal(out=at_rec[:, j:j + 1], in_=acc[:, c0 + 128:c0 + 129]), reads=[acc], writes=[at_rec])
                        k.op("dve", lambda e, j=j: e.tensor_scalar(out=osb[:, j, :], in0=acc[:, c0:c0 + 128], scalar1=at_rec[:, j:j + 1], scalar2=None, op0=ALU.mult),
                             reads=[acc, at_rec], writes=[osb])
                k.op("dve", lambda e: e.scalar_tensor_tensor(out=at_a[:, 0:nj, :], in0=at_o[1][:, 0:nj, :], scalar=neglam[:, 0:1], in1=at_o[0][:, 0:nj, :],
                                                             op0=ALU.mult, op1=ALU.add), reads=[at_o[0], at_o[1], neglam], writes=[at_a])
                k.op("pool", lambda e: e.tensor_tensor(out=at_sq[:, 0:nj, :], in0=at_a[:, 0:nj, :], in1=at_a[:, 0:nj, :], op=ALU.mult), reads=[at_a], writes=[at_sq])
                k.op("dve", lambda e: e.tensor_reduce(out=at_ss[:, 0:nj], in_=at_sq[:, 0:nj, :], axis=AX.X, op=ALU.add), reads=[at_sq], writes=[at_ss])
                k.op("act", lambda e: e.activation(out=at_ss[:, 0:nj], in_=at_ss[:, 0:nj], func=AF.Sqrt, scale=1.0 / 128, bias=EPS), reads=[at_ss], writes=[at_ss])
                k.op("dve", lambda e: e.reciprocal(out=at_ss[:, 0:nj], in_=at_ss[:, 0:nj]), reads=[at_ss], writes=[at_ss])
                k.op("dve", lambda e: e.tensor_tensor(out=at_a[:, 0:nj, :], in0=at_a[:, 0:nj, :], in1=at_ss[:, 0:nj].unsqueeze(2).to_broadcast([128, nj, 128]), op=ALU.mult),
                     reads=[at_a, at_ss], writes=[at_a])
                pt_ = k.nxt("psB", PS[4:8])
                for j in range(nj):
                    k.op("pe", lambda e, j=j: e.transpose(pt_[:, j * 128:(j + 1) * 128], at_a[:, j, :], ident[:]), reads=[at_a, ident], writes=[pt_])
                ob = k.nxt("at_ob", at_ob)
                k.op("act", lambda e: e.activation(out=ob[:, 0:nq], in_=pt_[:, 0:nq], func=AF.Identity, scale=subg[:, 0:1]), reads=[pt_, subg], writes=[ob])
                k.dma("sp", MIXT[h * 128:(h + 1) * 128, q0:q0 + nq], ob[:, 0:nq], reads=[ob], writes=[MIXT])

    mcw_in = ein("mcwT", [L, 128, 4, 3])
    mcb_in = ein("mcbT", [L, 128, 4])
    mgb_in = ein("mgbT", [L, 16, 1])
    mng_in = ein("mngT", [L, 128, 2])
    triu_in = ein("triu", [128, 128])
    tril_in = ein("tril", [128, 128])
    triu = k.sbuf("triu", [128, 128], F32)
    tril = k.sbuf("tril", [128, 128], F32)
    k.dma("sp", triu[:], triu_in[:], reads=[triu_in], writes=[triu])
    k.dma("sp", tril[:], tril_in[:], reads=[tril_in], writes=[tril])
    ORDER = [list(range(NTILE)), [1, 0] + list(range(NTILE - 1, 1, -1))]

    def phase_D(l):
        mcw = k.sbuf("mcw", [128, 4, 3], F32)
        mcb = k.sbuf("mcb", [128, 4], F32)
        mgb = k.sbuf("mgb", [16, 1], F32)
        mng = k.sbuf("mng", [128, 2], F32)
        k.dma("sp", mcw[:], mcw_in[l], reads=[mcw_in], writes=[mcw])
        k.dma("sp", mcb[:], mcb_in[l], reads=[mcb_in], writes=[mcb])
        k.dma("sp", mgb[:], mgb_in[l], reads=[mgb_in], writes=[mgb])
        k.dma("sp", mng[:], mng_in[l], reads=[mng_in], writes=[mng])
        mq = k.sbuf("mq_all", [128, 4, NT], BF16)
        zc = [k.sbuf("md_z%d" % i, [128, 514], F32) for i in range(2)]
        ac = [k.sbuf("md_a%d" % i, [128, 512], F32) for i in range(2)]
        for c in range(4):
            for (t0, n) in BLOCKS:
                z = k.nxt("md_z", zc)
                a = k.nxt("md_a", ac)
                pc = padcol(t0)
                k.dma("sp", z[:, 0:n + 2], MQKT[c * 128:(c + 1) * 128, pc - 1:pc + n + 1], reads=[MQKT], writes=[z])
                k.op("dve", lambda e: e.tensor_scalar(out=a[:, 0:n], in0=z[:, 0:n], scalar1=mcw[:, c, 0:1], scalar2=None, op0=ALU.mult), reads=[z, mcw], writes=[a])
                k.op("dve", lambda e: e.scalar_tensor_tensor(out=a[:, 0:n], in0=z[:, 1:n + 1], scalar=mcw[:, c, 1:2], in1=a[:, 0:n], op0=ALU.mult, op1=ALU.add), reads=[z, mcw, a], writes=[a])
                k.op("dve", lambda e: e.scalar_tensor_tensor(out=a[:, 0:n], in0=z[:, 2:n + 2], scalar=mcw[:, c, 2:3], in1=a[:, 0:n], op0=ALU.mult, op1=ALU.add), reads=[z, mcw, a], writes=[a])
                if c < 2:
                    k.op("act", lambda e: e.activation(out=mq[:, c, t0:t0 + n], in_=a[:, 0:n], func=AF.Silu, bias=mcb[:, c:c + 1], scale=1.0), reads=[a, mcb], writes=[mq])
                else:
                    k.op("act", lambda e: e.activation(out=a[:, 0:n], in_=a[:, 0:n], func=AF.Silu, bias=mcb[:, c:c + 1], scale=1.0), reads=[a, mcb], writes=[a])
                    k.op("dve", lambda e: e.tensor_scalar(out=mq[:, c, t0:t0 + n], in0=a[:, 0:n], scalar1=0.125, scalar2=None, op0=ALU.mult), reads=[a], writes=[mq])
        if DSTOP <= 1:
            return
        GI = k.sbuf("md_gi", [16, NT], F32)
        GL = k.sbuf("md_gl", [16, NT], F32)
        k.dma("sp", GI[:], MGT[:], reads=[MGT], writes=[GI])
        k.op("dve", lambda e: e.tensor_scalar(out=GI[:], in0=GI[:], scalar1=mgb[:, 0:1], scalar2=None, op0=ALU.add), reads=[GI, mgb], writes=[GI])
        k.op("act", lambda e: e.activation(out=GL[:], in_=GI[:], func=AF.Exp, scale=-1.0), reads=[GI], writes=[GL])
        k.op("act", lambda e: e.activation(out=GL[:], in_=GL[:], func=AF.Ln, bias=1.0, scale=1.0), reads=[GL], writes=[GL])
        k.op("dve", lambda e: e.tensor_scalar(out=GL[:], in0=GL[:], scalar1=-1.0, scalar2=None, op0=ALU.mult), reads=[GL], writes=[GL])
        ES = k.sbuf("md_es", [128, NTILE, 8], F32)
        EB = k.sbuf("md_eb", [128, NTILE, 8], F32)
        EBL = k.sbuf("md_ebl", [128, NTILE, 8], F32)
        VAUG = k.sbuf("md_vaug", [128, NTILE, 4, 66], BF16)
        k.op("pool", lambda e: e.memset(VAUG[:], 1.0), writes=[VAUG])
        gtok = [k.sbuf("md_gtok%d" % i, [128, 32], F32) for i in range(2)]
        cs = [k.sbuf("md_cs%d" % i, [128, 16], F32) for i in range(2)]
        vt = [k.sbuf("md_vt%d" % i, [128, 2, 128], F32) for i in range(2)]
        for t in range(NTILE):
            tsl = slice(t * 128, (t + 1) * 128)
            p = ps()
            k.op("pe", lambda e: e.transpose(p[:, 0:16], GI[0:16, tsl], ident[0:16, 0:16]), reads=[GI, ident], writes=[p])
            k.op("pe", lambda e: e.transpose(p[:, 16:32], GL[0:16, tsl], ident[0:16, 0:16]), reads=[GL, ident], writes=[p])
            g = k.nxt("md_gtok", gtok)
            k.op("act", lambda e: e.copy(out=g[:], in_=p[:, 0:32]), reads=[p], writes=[g])
            p2 = ps()
            k.op("pe", lambda e: e.matmul(p2[:, 0:4], triu[:], g[:, 20:24], start=True, stop=True), reads=[triu, g], writes=[p2])
            k.op("pe", lambda e: e.matmul(p2[:, 4:8], tril[:], g[:, 28:32], start=True, stop=True), reads=[tril, g], writes=[p2])
            k.op("pe", lambda e: e.matmul(p2[:, 8:12], ones_f[:], g[:, 20:24], start=True, stop=True), reads=[ones_f, g], writes=[p2])
            k.op("pe", lambda e: e.matmul(p2[:, 12:16], ones_f[:], g[:, 28:32], start=True, stop=True), reads=[ones_f, g], writes=[p2])
            c_ = k.nxt("md_cs", cs)
            k.op("dve", lambda e: e.tensor_copy(out=c_[:], in_=p2[:, 0:16]), reads=[p2], writes=[c_])
            k.op("dve", lambda e: e.tensor_tensor(out=ES[:, t, 0:4], in0=g[:, 0:4], in1=c_[:, 0:4], op=ALU.subtract), reads=[g, c_], writes=[ES])
            k.op("dve", lambda e: e.tensor_tensor(out=ES[:, t, 4:8], in0=g[:, 8:12], in1=c_[:, 4:8], op=ALU.subtract), reads=[g, c_], writes=[ES])
            k.op("act", lambda e: e.activation(out=ES[:, t, :], in_=ES[:, t, :], func=AF.Exp), reads=[ES], writes=[ES])
            k.op("act", lambda e: e.activation(out=EB[:, t, :], in_=c_[:, 0:8], func=AF.Exp), reads=[c_], writes=[EB])
            k.op("act", lambda e: e.activation(out=EBL[:, t, :], in_=c_[:, 8:16], func=AF.Exp), reads=[c_], writes=[EBL])
            v_ = k.nxt("md_vt", vt)
            k.dma("sp", v_[:], MVT.t.ap().rearrange("(c p) t -> p c t", p=128)[:, :, tsl], reads=[MVT], writes=[v_])
            p3 = ps()
            for c in range(2):
                k.op("pe", lambda e, c=c: e.transpose(p3[:, c * 128:(c + 1) * 128], v_[:, c, :], ident[:]), reads=[v_, ident], writes=[p3])
            k.op("dve", lambda e: e.tensor_copy(out=VAUG[:, t, :, 0:64], in_=p3[:, 0:256].rearrange("p (h d) -> p h d", d=64)), reads=[p3], writes=[VAUG])
        if DSTOP <= 2:
            return
        CT = [k.sbuf("md_ct%d" % d, [128, 2, 66], F32) for d in range(2)]
        CTb = [k.sbuf("md_ctb%d" % d, [128, 2, 66], BF16) for d in range(2)]
        keP = [k.sbuf("md_kep%d" % d, [128, 4, 128], BF16) for d in range(2)]
        for d in range(2):
            k.op("pool", lambda e, d=d: e.memset(CT[d][:], 0.0), writes=[CT[d]])
            k.op("pool", lambda e, d=d: e.memset(CTb[d][:], 0.0), writes=[CTb[d]])
            k.op("pool", lambda e, d=d: e.memset(keP[d][:], 0.0), writes=[keP[d]])
        Sp = [k.sbuf("md_sp%d" % i, [128, 4, 128], BF16) for i in range(2)]
        dn = [k.sbuf("md_dn%d" % i, [128, 4], F32) for i in range(2)]
        hv = [k.sbuf("md_hv%d" % i, [128, 4, 64], F32) for i in range(2)]
        dn2 = [k.sbuf("md_dn2%d" % i, [128, 4], F32) for i in range(2)]
        tot = [k.sbuf("md_tot%d" % i, [128, 4, 66], F32) for i in range(2)]
        Ft = [k.sbuf("md_F%d" % i, [128, 2], F32) for i in range(2)]
        ctmp = [k.sbuf("md_ctmp%d" % i, [128, 2, 66], F32) for i in range(2)]
        masks = [triu, tril]
        for i in range(NTILE):
            for d in range(2):
                t = ORDER[d][i]
                tsl = slice(t * 128, (t + 1) * 128)
                pk = ps()
                for c in range(2):
                    k.op("pe", lambda e, c=c: e.matmul(pk[:, c * 128:(c + 1) * 128], mq[:, 2 + c, tsl], identb[:], start=True, stop=True), reads=[mq, identb], writes=[pk])
                for h in range(4):
                    hb = (h % 2) * 64
                    k.op("dve", lambda e, h=h, hb=hb: e.tensor_scalar(out=keP[d][:, h, hb:hb + 64], in0=pk[:, h * 64:(h + 1) * 64], scalar1=ES[:, t, d * 4 + h:d * 4 + h + 1], scalar2=None, op0=ALU.mult),
                         reads=[pk, ES], writes=[keP[d]])
                pS2 = [ps(), ps()]
                for h in range(4):
                    hb = (h % 2) * 64
                    k.op("pe", lambda e, h=h, hb=hb: e.matmul(pS2[h % 2][:, (h // 2) * 128:(h // 2 + 1) * 128], mq[hb:hb + 64, 2 + h // 2, tsl], mq[hb:hb + 64, h // 2, tsl], start=True, stop=True),
                         reads=[mq], writes=[pS2[h % 2]])
                S_ = k.nxt("md_sp", Sp)
                for h in range(4):
                    k.op("dve", lambda e, h=h: e.scalar_tensor_tensor(out=S_[:, h, :], in0=pS2[h % 2][:, (h // 2) * 128:(h // 2 + 1) * 128], scalar=ES[:, t, d * 4 + h:d * 4 + h + 1], in1=masks[d][:],
                                                                      op0=ALU.mult, op1=ALU.mult), reads=[pS2[h % 2], ES, masks[d]], writes=[S_])
                pN = ps()
                pR2 = [ps(), ps()]
                for h in range(4):
                    hb = (h % 2) * 64
                    k.op("pe", lambda e, h=h: e.matmul(pN[:, h * 66:h * 66 + 65], S_[:, h, :], VAUG[:, t, h, 0:65], start=True, stop=True), reads=[S_, VAUG], writes=[pN])
                    k.op("pe", lambda e, h=h, hb=hb: e.matmul(pR2[h % 2][:, (h // 2) * 66:(h // 2) * 66 + 65], mq[hb:hb + 64, h // 2, tsl], CTb[d][hb:hb + 64, h // 2, 0:65], start=True, stop=True),
                         reads=[mq, CTb[d]], writes=[pR2[h % 2]])
                tot_ = k.nxt("md_tot", tot)
                totv = tot_[:].rearrange("p (a b) e -> p a b e", b=2)
                for par in range(2):
                    k.op("act", lambda e, par=par: e.copy(out=totv[:, :, par, 0:65], in_=pR2[par][:, 0:132].rearrange("p (a e) -> p a e", e=66)[:, :, 0:65]), reads=[pR2[par]], writes=[tot_])
                k.op("dve", lambda e: e.tensor_tensor(out=tot_[:, :, 0:65], in0=pN[:, 0:264].rearrange("p (h e) -> p h e", e=66)[:, :, 0:65], in1=tot_[:, :, 0:65], op=ALU.add), reads=[pN, tot_], writes=[tot_])
                pNv = tot_
                dn_ = k.nxt("md_dn", dn)
                hv_ = k.nxt("md_hv", hv)
                k.op("dve", lambda e: e.tensor_tensor(out=dn_[:], in0=pNv[:, :, 64], in1=EB[:, t, d * 4:(d + 1) * 4], op=ALU.mult), reads=[tot_, EB], writes=[dn_])
                dn2_ = k.nxt("md_dn2", dn2)
                k.op("dve", lambda e: e.tensor_scalar(out=dn2_[:], in0=dn_[:], scalar1=-1.0, scalar2=None, op0=ALU.mult), reads=[dn_], writes=[dn2_])
                k.op("dve", lambda e: e.tensor_tensor(out=dn_[:], in0=dn_[:], in1=dn2_[:], op=ALU.max), reads=[dn_, dn2_], writes=[dn_])
                k.op("dve", lambda e: e.tensor_scalar(out=dn_[:], in0=dn_[:], scalar1=1.0, scalar2=None, op0=ALU.max), reads=[dn_], writes=[dn_])
                k.op("dve", lambda e: e.reciprocal(out=dn_[:], in_=dn_[:]), reads=[dn_], writes=[dn_])
                k.op("dve", lambda e: e.tensor_tensor(out=dn_[:], in0=dn_[:], in1=EB[:, t, d * 4:(d + 1) * 4], op=ALU.mult), reads=[dn_, EB], writes=[dn_])
                k.op("dve", lambda e: e.tensor_tensor(out=hv_[:], in0=pNv[:, :, 0:64], in1=dn_[:].unsqueeze(2).to_broadcast([128, 4, 64]), op=ALU.mult), reads=[tot_, dn_], writes=[hv_])
                k.dma("sp", HFB[d][tsl, :], hv_[:].rearrange("p h e -> p (h e)"), reads=[hv_], writes=[HFB[d]])
                pC = ps()
                for pp in range(2):
                    k.op("pe", lambda e, pp=pp: e.matmul(pC[:, pp * 66:pp * 66 + 65], keP[d][:, 2 * pp, :], VAUG[:, t, 2 * pp, 0:65], start=True, stop=False), reads=[keP[d], VAUG], writes=[pC])
                    k.op("pe", lambda e, pp=pp: e.matmul(pC[:, pp * 66:pp * 66 + 65], keP[d][:, 2 * pp + 1, :], VAUG[:, t, 2 * pp + 1, 0:65], start=False, stop=True), reads=[keP[d], VAUG], writes=[pC])
                F_ = k.nxt("md_F", Ft)
                ebl2 = EBL[:, t, d * 4:(d + 1) * 4].rearrange("p (a b) -> p a b", b=2)
                k.op("dve", lambda e: e.tensor_copy(out=F_[0:64, :], in_=ebl2[0:64, :, 0]), reads=[EBL], writes=[F_])
                k.op("dve", lambda e: e.tensor_copy(out=F_[64:128, :], in_=ebl2[64:128, :, 1]), reads=[EBL], writes=[F_])
                ct_ = k.nxt("md_ctmp", ctmp)
                k.op("dve", lambda e: e.tensor_tensor(out=ct_[:, :, 0:65], in0=pC[:, 0:132].rearrange("p (a e) -> p a e", e=66)[:, :, 0:65], in1=CT[d][:, :, 0:65], op=ALU.add), reads=[pC, CT[d]], writes=[ct_])
                k.op("dve", lambda e: e.tensor_tensor(out=CT[d][:, :, 0:65], in0=ct_[:, :, 0:65], in1=F_[:].unsqueeze(2).to_broadcast([128, 2, 65]), op=ALU.mult), reads=[ct_, F_], writes=[CT[d]])
                k.op("act", lambda e: e.copy(out=CTb[d][:, :, 0:65], in_=CT[d][:, :, 0:65]), reads=[CT[d]], writes=[CTb[d]])
        if DSTOP <= 3:
            return
        hf = [k.sbuf("md_hf%d" % i, [128, 4, 64], F32) for i in range(2)]
        hb_ = [k.sbuf("md_hb%d" % i, [128, 4, 64], F32) for i in range(2)]
        st4 = [k.sbuf("md_st%d" % i, [128, 4], F32) for i in range(2)]
        sq4 = [k.sbuf("md_sq%d" % i, [128, 4, 64], F32) for i in range(2)]
        mo = [k.sbuf("md_mo%d" % i, [128, 2, 128], F32) for i in range(2)]
        ob = [k.sbuf("md_ob%d" % i, [128, 2, 128], BF16) for i in range(2)]
        MOTv = MOT.t.ap().rearrange("(c p) t -> p c t", p=128)
        MIXv = MIXT.t.ap()[768:1024, :].rearrange("(c p) t -> p c t", p=128)
        for t in range(NTILE):
            tsl = slice(t * 128, (t + 1) * 128)
            a = k.nxt("md_hf", hf)
            b = k.nxt("md_hb", hb_)
            s4 = k.nxt("md_st", st4)
            q4 = k.nxt("md_sq", sq4)
            k.dma("sp", a[:].rearrange("p h e -> p (h e)"), HFB[0][tsl, :], reads=[HFB[0]], writes=[a])
            k.dma("sp", b[:].rearrange("p h e -> p (h e)"), HFB[1][tsl, :], reads=[HFB[1]], writes=[b])
            k.op("dve", lambda e: e.tensor_tensor(out=a[:], in0=a[:], in1=b[:], op=ALU.add), reads=[a, b], writes=[a])
            k.op("dve", lambda e: e.tensor_reduce(out=s4[:], in_=a[:], axis=AX.X, op=ALU.add), reads=[a], writes=[s4])
            k.op("dve", lambda e: e.tensor_scalar(out=s4[:], in0=s4[:], scalar1=-1.0 / 64, scalar2=None, op0=ALU.mult), reads=[s4], writes=[s4])
            k.op("dve", lambda e: e.tensor_tensor(out=a[:], in0=a[:], in1=s4[:].unsqueeze(2).to_broadcast([128, 4, 64]), op=ALU.add), reads=[a, s4], writes=[a])
            k.op("pool", lambda e: e.tensor_tensor(out=q4[:], in0=a[:], in1=a[:], op=ALU.mult), reads=[a], writes=[q4])
            k.op("dve", lambda e: e.tensor_reduce(out=s4[:], in_=q4[:], axis=AX.X, op=ALU.add), reads=[q4], writes=[s4])
            k.op("act", lambda e: e.activation(out=s4[:], in_=s4[:], func=AF.Sqrt, scale=1.0 / 64, bias=EPS), reads=[s4], writes=[s4])
            k.op("dve", lambda e: e.reciprocal(out=s4[:], in_=s4[:]), reads=[s4], writes=[s4])
            k.op("dve", lambda e: e.tensor_tensor(out=a[:], in0=a[:], in1=s4[:].unsqueeze(2).to_broadcast([128, 4, 64]), op=ALU.mult), reads=[a, s4], writes=[a])
            p = ps()
            av = a[:].rearrange("p h e -> p (h e)")
            for c in range(2):
                k.op("pe", lambda e, c=c: e.transpose(p[:, c * 128:(c + 1) * 128], av[:, c * 128:(c + 1) * 128], ident[:]), reads=[a, ident], writes=[p])
            m_ = k.nxt("md_mo", mo)
            o_ = k.nxt("md_ob", ob)
            k.dma("sp", m_[:], MOTv[:, :, tsl], reads=[MOT], writes=[m_])
            for c in range(2):
                k.op("dve", lambda e, c=c: e.scalar_tensor_tensor(out=o_[:, c, :], in0=p[:, c * 128:(c + 1) * 128], scalar=mng[:, c:c + 1], in1=m_[:, c, :], op0=ALU.mult, op1=ALU.mult),
                     reads=[p, mng, m_], writes=[o_])
            k.dma("sp", MIXv[:, :, tsl], o_[:], reads=[o_], writes=[MIXT])

    rp_in = ein("rw_par", [L, 64, 48])
    w2p_in = ein("rw_w2p", [L, 2, 64, 256])
    a2p_in = ein("rw_a2p", [L, 2, 64, 256])
    g2_in = ein("rw_g2", [L, 64, 256])
    lnp_in = ein("rw_lnp", [L, 128, 2, 2])
    rmask_in = ein("rw_mask", [2, 64, 5, 64])
    reset_in = ein("rw_reset", [64, 512])
    RS = k.dram("RS", [2, 4, 4, 64, NT], BF16)
    VS = k.dram("VS", [4, 64, NT], BF16)
    WLD = k.dram("WLD", [2, 4, 64, 68], F32)
    BON = k.dram("BON", [256, NT], F32)
    GGd = k.dram("GGd", [256, NT], F32)
    YS = [k.dram("YSf", [NT, 256], F32), k.dram("YSb", [NT, 256], F32)]
    CH_ORDER = [list(range(68)), [3, 2, 1, 0] + list(range(67, 3, -1))]

    def phase_C(l):
        RP = k.sbuf("rc_rp", [64, 48], F32)
        W2P = k.sbuf("rc_w2p", [64, 2, 256], F32)
        A2P = k.sbuf("rc_a2p", [64, 2, 256], F32)
        G2 = k.sbuf("rc_g2", [64, 256], F32)
        RESET = k.sbuf("rc_reset", [64, 512], F32)
        ONES = k.sbuf("rc_ones", [64, 512], F32)
        omka = k.sbuf("rc_omka", [64, 4], F32)
        k.dma("sp", RP[:], rp_in[l], reads=[rp_in], writes=[RP])
        for d in range(2):
            k.dma("sp", W2P[:, d, :], w2p_in[l][d], reads=[w2p_in], writes=[W2P])
            k.dma("sp", A2P[:, d, :], a2p_in[l][d], reads=[a2p_in], writes=[A2P])
        k.dma("sp", G2[:], g2_in[l], reads=[g2_in], writes=[G2])
        k.dma("sp", RESET[:], reset_in[:], reads=[reset_in], writes=[RESET])
        k.op("pool", lambda e: e.memset(ONES[:], 1.0), writes=[ONES])
        k.op("dve", lambda e: e.tensor_scalar(out=omka[:], in0=RP[:, 35:39], scalar1=-1.0, scalar2=1.0, op0=ALU.mult, op1=ALU.add), reads=[RP], writes=[omka])

        def mk(name, shape, dt, nb=2):
            return [k.sbuf("rc_%s%d" % (name, i), shape, dt) for i in range(nb)]
        zin = mk("zin", [64, 514], F32, 3)
        nm = mk("nm", [64, 512], F32, 2)
        wds = k.sbuf("rc_wds", [64, 512], F32)
        ads = k.sbuf("rc_ads", [64, 512], F32)
        gds = k.sbuf("rc_gds", [64, 512], F32)
        rs_ = k.sbuf("rc_rs", [64, 512], F32)
        ks_ = k.sbuf("rc_ks", [64, 512], F32)
        vs_ = k.sbuf("rc_vs", [64, 512], F32)
        kk_ = k.sbuf("rc_kk", [64, 512], F32)
        t_a = mk("ta", [64, 512], F32, 2)
        t_b = mk("tb", [64, 512], F32, 2)
        t_c = mk("tc", [64, 512], F32, 2)
        lw_ = mk("lw", [64, 512], F32, 2)
        LW = mk("LW", [64, 512], F32, 2)
        km = [k.sbuf("rc_km%d" % d, [64, 512], F32) for d in range(2)]
        aa = mk("aa", [64, 512], F32, 2)
        ob4 = mk("ob4", [64, 4, 512], BF16, 2)
        vb = mk("vb", [64, 512], BF16, 2)
        wl_sb = k.sbuf("rc_wl", [64, 8, 68], F32)

        def shift(dst, row0, mucol, pc, n):
            z = k.nxt("rc_zin", zin)
            m_ = k.nxt("rc_nm", nm)
            k.dma("sp", z[:, 0:n + 2], RZT[row0:row0 + 64, pc - 1:pc + n + 1], reads=[RZT], writes=[z])
            k.op("pool", lambda e: e.tensor_tensor(out=m_[:, 0:n], in0=z[:, 0:n], in1=z[:, 2:n + 2], op=ALU.add), reads=[z], writes=[m_])
            k.op("dve", lambda e: e.scalar_tensor_tensor(out=m_[:, 0:n], in0=m_[:, 0:n], scalar=0.5, in1=z[:, 1:n + 1], op0=ALU.mult, op1=ALU.subtract), reads=[m_, z], writes=[m_])
            k.op("dve", lambda e: e.scalar_tensor_tensor(out=dst[:, 0:n], in0=m_[:, 0:n], scalar=RP[:, mucol:mucol + 1], in1=z[:, 1:n + 1], op0=ALU.mult, op1=ALU.add), reads=[m_, z, RP], writes=[dst])

        for (t0, n) in BLOCKS:
            pc = padcol(t0)
            nch = n // 64
            c0 = t0 // 64
            shift(wds, 768, 12, pc, n)
            shift(ads, 832, 13, pc, n)
            shift(gds, 896, 14, pc, n)
            k.op("act", lambda e: e.activation(out=wds[:, 0:n], in_=wds[:, 0:n], func=AF.Tanh), reads=[wds], writes=[wds])
            k.op("act", lambda e: e.activation(out=gds[:, 0:n], in_=gds[:, 0:n], func=AF.Sigmoid), reads=[gds], writes=[gds])
            for h in range(4):
                hs = slice(h * 64, (h + 1) * 64)
                shift(rs_, h * 64, 0 + h, pc, n)
                shift(ks_, 256 + h * 64, 4 + h, pc, n)
                shift(vs_, 512 + h * 64, 8 + h, pc, n)
                p = ps()
                k.op("pe", lambda e: e.matmul(p[0:64, 0:n], G2[:, hs], gds[:, 0:n], start=True, stop=True), reads=[G2, gds], writes=[p])
                ta = k.nxt("rc_ta", t_a)
                k.op("act", lambda e: e.copy(out=ta[:, 0:n], in_=p[0:64, 0:n]), reads=[p], writes=[ta])
                k.dma("sp", GGd[hs, t0:t0 + n], ta[:, 0:n], reads=[ta], writes=[GGd])
                vb_ = k.nxt("rc_vb", vb)
                k.op("pool", lambda e: e.tensor_copy(out=vb_[:, 0:n], in_=vs_[:, 0:n]), reads=[vs_], writes=[vb_])
                k.dma("sp", VS[h][:, t0:t0 + n], vb_[:, 0:n], reads=[vb_], writes=[VS])
                tb = k.nxt("rc_tb", t_b)
                tc_ = k.nxt("rc_tc", t_c)
                k.op("dve", lambda e: e.tensor_scalar(out=tb[:, 0:n], in0=ks_[:, 0:n], scalar1=RP[:, 31 + h:32 + h], scalar2=None, op0=ALU.mult), reads=[ks_, RP], writes=[tb])
                k.op("pool", lambda e: e.tensor_tensor(out=tc_[:, 0:n], in0=tb[:, 0:n], in1=tb[:, 0:n], op=ALU.mult), reads=[tb], writes=[tc_])
                p = ps()
                k.op("pe", lambda e: e.matmul(p[0:64, 0:n], ONES[:, 0:64], tc_[:, 0:n], start=True, stop=True), reads=[ONES, tc_], writes=[p])
                k.op("act", lambda e: e.activation(out=tc_[:, 0:n], in_=p[0:64, 0:n], func=AF.Sqrt), reads=[p], writes=[tc_])
                k.op("dve", lambda e: e.tensor_scalar(out=tc_[:, 0:n], in0=tc_[:, 0:n], scalar1=1e-12, scalar2=None, op0=ALU.max), reads=[tc_], writes=[tc_])
                k.op("dve", lambda e: e.reciprocal(out=tc_[:, 0:n], in_=tc_[:, 0:n]), reads=[tc_], writes=[tc_])
                k.op("dve", lambda e: e.tensor_tensor(out=kk_[:, 0:n], in0=tb[:, 0:n], in1=tc_[:, 0:n], op=ALU.mult), reads=[tb, tc_], writes=[kk_])
                for d in range(2):
                    p = ps()
                    k.op("pe", lambda e: e.matmul(p[0:64, 0:n], W2P[:, d, hs], wds[:, 0:n], start=True, stop=True), reads=[W2P, wds], writes=[p])
                    lw = k.nxt("rc_lw", lw_)
                    k.op("act", lambda e: e.activation(out=lw[:, 0:n], in_=p[0:64, 0:n], func=AF.Sigmoid, bias=RP[:, 15 + d * 4 + h:16 + d * 4 + h], scale=1.0), reads=[p, RP], writes=[lw])
                    k.op("dve", lambda e: e.tensor_scalar(out=lw[:, 0:n], in0=lw[:, 0:n], scalar1=-0.606531, scalar2=None, op0=ALU.mult), reads=[lw], writes=[lw])
                    LWt = k.nxt("rc_LW", LW)
                    k.op("dve", lambda e: e.tensor_tensor_scan(out=LWt[:, 0:n], data0=RESET[:, 0:n], data1=lw[:, 0:n], initial=0.0, op0=ALU.mult, op1=ALU.add), reads=[RESET, lw], writes=[LWt])
                    LW3 = LWt[:, 0:n].rearrange("p (c t) -> p c t", t=64)
                    if d == 1:
                        tb2 = k.nxt("rc_tb", t_b)
                        k.op("dve", lambda e: e.tensor_tensor(out=tb2[:, 0:n].rearrange("p (c t) -> p c t", t=64), in0=LW3[:, :, 63:64].to_broadcast([64, nch, 64]), in1=LW3, op=ALU.subtract),
                             reads=[LWt], writes=[tb2])
                        k.op("dve", lambda e: e.tensor_tensor(out=LWt[:, 0:n], in0=tb2[:, 0:n], in1=lw[:, 0:n], op=ALU.add), reads=[tb2, lw], writes=[LWt])
                    tot_ap = LW3[:, :, 63] if d == 0 else LW3[:, :, 0]
                    k.op("act", lambda e: e.activation(out=wl_sb[:, d * 4 + h, c0:c0 + nch], in_=tot_ap, func=AF.Exp), reads=[LWt], writes=[wl_sb])
                    p = ps()
                    k.op("pe", lambda e: e.matmul(p[0:64, 0:n], A2P[:, d, hs], ads[:, 0:n], start=True, stop=True), reads=[A2P, ads], writes=[p])
                    a_ = k.nxt("rc_aa", aa)
                    k.op("act", lambda e: e.activation(out=a_[:, 0:n], in_=p[0:64, 0:n], func=AF.Sigmoid, bias=RP[:, 23 + d * 4 + h:24 + d * 4 + h], scale=1.0), reads=[p, RP], writes=[a_])
                    k.op("dve", lambda e: e.tensor_scalar(out=km[d][:, 0:n], in0=a_[:, 0:n], scalar1=RP[:, 35 + h:36 + h], scalar2=omka[:, h:h + 1], op0=ALU.mult, op1=ALU.add), reads=[a_, RP, omka], writes=[km[d]])
                    k.op("pool", lambda e: e.tensor_tensor(out=km[d][:, 0:n], in0=km[d][:, 0:n], in1=ks_[:, 0:n], op=ALU.mult), reads=[km[d], ks_], writes=[km[d]])
                    k.op("pool", lambda e: e.tensor_tensor(out=a_[:, 0:n], in0=a_[:, 0:n], in1=kk_[:, 0:n], op=ALU.mult), reads=[a_, kk_], writes=[a_])
                    e1 = k.nxt("rc_ta", t_a)
                    e2 = k.nxt("rc_tc", t_c)
                    k.op("act", lambda e: e.activation(out=e1[:, 0:n], in_=LWt[:, 0:n], func=AF.Exp), reads=[LWt], writes=[e1])
                    k.op("act", lambda e: e.activation(out=e2[:, 0:n], in_=LWt[:, 0:n], func=AF.Exp, scale=-1.0), reads=[LWt], writes=[e2])
                    k.op("dve", lambda e: e.tensor_tensor(out=lw[:, 0:n], in0=LWt[:, 0:n], in1=lw[:, 0:n], op=ALU.subtract), reads=[LWt, lw], writes=[lw])
                    k.op("act", lambda e: e.activation(out=lw[:, 0:n], in_=lw[:, 0:n], func=AF.Exp), reads=[lw], writes=[lw])
                    o4 = k.nxt("rc_ob4", ob4)
                    k.op("dve", lambda e: e.tensor_tensor(out=o4[:, 0, 0:n], in0=kk_[:, 0:n], in1=lw[:, 0:n], op=ALU.mult), reads=[kk_, lw], writes=[o4])
                    k.op("pool", lambda e: e.tensor_tensor(out=o4[:, 1, 0:n], in0=rs_[:, 0:n], in1=e1[:, 0:n], op=ALU.mult), reads=[rs_, e1], writes=[o4])
                    k.op("dve", lambda e: e.tensor_tensor(out=o4[:, 2, 0:n], in0=a_[:, 0:n], in1=e2[:, 0:n], op=ALU.mult), reads=[a_, e2], writes=[o4])
                    k.op("pool", lambda e: e.tensor_tensor(out=o4[:, 3, 0:n], in0=km[d][:, 0:n], in1=e2[:, 0:n], op=ALU.mult), reads=[km[d], e2], writes=[o4])
                    for q in range(4):
                        k.dma("sp", RS[d][h][q][:, t0:t0 + n], o4[:, q, 0:n], reads=[o4], writes=[RS])
                tb = k.nxt("rc_tb", t_b)
                k.op("dve", lambda e: e.tensor_tensor(out=tb[:, 0:n], in0=km[0][:, 0:n], in1=km[1][:, 0:n], op=ALU.add), reads=[km[0], km[1]], writes=[tb])
                k.op("dve", lambda e: e.scalar_tensor_tensor(out=tb[:, 0:n], in0=rs_[:, 0:n], scalar=RP[:, 43 + h:44 + h], in1=tb[:, 0:n], op0=ALU.mult, op1=ALU.mult), reads=[rs_, RP, tb], writes=[tb])
                p = ps()
                k.op("pe", lambda e: e.matmul(p[0:64, 0:n], ONES[:, 0:64], tb[:, 0:n], start=True, stop=True), reads=[ONES, tb], writes=[p])
                ta = k.nxt("rc_ta", t_a)
                k.op("dve", lambda e: e.tensor_tensor(out=ta[:, 0:n], in0=p[0:64, 0:n], in1=vs_[:, 0:n], op=ALU.mult), reads=[p, vs_], writes=[ta])
                k.dma("sp", BON[hs, t0:t0 + n], ta[:, 0:n], reads=[ta], writes=[BON])
        if DSTOP <= 1:
            return
        RM = k.sbuf("rs_mask", [64, 2, 5, 64], F32)
        for d in range(2):
            k.dma("sp", RM[:, d, :, :], rmask_in[d], reads=[rmask_in], writes=[RM])
        X = [k.sbuf("rs_x%d" % i, [64, 4, 4, 64], BF16) for i in range(4)]
        XV = [k.sbuf("rs_xv%d" % i, [64, 4, 64], BF16) for i in range(4)]
        S0 = k.sbuf("rs_s0", [64, 8, 64], F32)
        S0b = k.sbuf("rs_s0b", [64, 8, 64], BF16)
        k.op("pool", lambda e: e.memset(S0[:], 0.0), writes=[S0])
        k.op("pool", lambda e: e.memset(S0b[:], 0.0), writes=[S0b])
        PQ = [k.sbuf("rs_pq%d" % i, [64, 2, 8, 64], BF16) for i in range(2)]
        A2 = k.sbuf("rs_a2", [64, 2, 8, 64], BF16)
        A3 = k.sbuf("rs_a3", [64, 8, 64], BF16)
        TOK = k.sbuf("rs_tok", [64, 3, 8, 64], BF16)
        G = k.sbuf("rs_g", [64, 8, 64], F32)
        Gb = k.sbuf("rs_gb", [64, 8, 64], BF16)
        nUb = k.sbuf("rs_nub", [64, 8, 64], BF16)
        Ysb = [k.sbuf("rs_y%d" % i, [64, 8, 64], F32) for i in range(2)]
        stmp = k.sbuf("rs_stmp", [64, 8, 64], F32)
        RSv = [[RS[d][h] for h in range(4)] for d in range(2)]
        for i in range(NSTEP):
            xs, xvs = [], []
            for d in range(2):
                c = CH_ORDER[d][i]
                x_ = k.nxt("rs_x", X)
                xv_ = k.nxt("rs_xv", XV)
                for h in range(4):
                    k.dma("sp" if h % 2 == 0 else "pool", x_[:, h, :, :], RS[d][h].rearrange("q p t -> p q t")[:, :, c * 64:(c + 1) * 64], reads=[RS], writes=[x_])
                k.dma("sp", xv_[:], VS.t.ap().rearrange("h p t -> p h t")[:, :, c * 64:(c + 1) * 64], reads=[VS], writes=[xv_])
                xs.append(x_)
                xvs.append(xv_)
            if SSTOP <= 1:
                continue
            pT1 = [ps(), ps()]
            pT2 = [ps(), ps()]
            pT3 = ps()
            for d in range(2):
                x_ = xs[d]
                for h in range(4):
                    cs_ = slice(h * 64, (h + 1) * 64)
                    cs2 = slice(256 + h * 64, 256 + (h + 1) * 64)
                    k.op("pe", lambda e: e.matmul(pT1[d][0:64, cs_], x_[:, h, 2, :], x_[:, h, 0, :], start=True, stop=True), reads=[x_], writes=[pT1[d]])
                    k.op("pe", lambda e: e.matmul(pT1[d][0:64, cs2], x_[:, h, 0, :], x_[:, h, 2, :], start=True, stop=True), reads=[x_], writes=[pT1[d]])
                    k.op("pe", lambda e: e.matmul(pT2[d][0:64, cs_], x_[:, h, 3, :], x_[:, h, 0, :], start=True, stop=True), reads=[x_], writes=[pT2[d]])
                    k.op("pe", lambda e: e.matmul(pT2[d][0:64, cs2], x_[:, h, 2, :], x_[:, h, 1, :], start=True, stop=True), reads=[x_], writes=[pT2[d]])
                    k.op("pe", lambda e: e.matmul(pT3[0:64, d * 256 + h * 64:d * 256 + (h + 1) * 64], x_[:, h, 3, :], x_[:, h, 1, :], start=True, stop=True), reads=[x_], writes=[pT3])
            pq = k.nxt("rs_pq", PQ)
            for d in range(2):
                k.op("dve", lambda e, d=d: e.tensor_tensor(out=pq[:, :, d * 4:(d + 1) * 4, :], in0=pT1[d][0:64, :].rearrange("p (a h t) -> p a h t", a=2, h=4),
                                                           in1=RM[:, d, 0:2, :].unsqueeze(2).to_broadcast([64, 2, 4, 64]), op=ALU.mult), reads=[pT1[d], RM], writes=[pq])
                k.op("dve", lambda e, d=d: e.tensor_tensor(out=A2[:, :, d * 4:(d + 1) * 4, :], in0=pT2[d][0:64, :].rearrange("p (a h t) -> p a h t", a=2, h=4),
                                                           in1=RM[:, d, 2:4, :].unsqueeze(2).to_broadcast([64, 2, 4, 64]), op=ALU.mult), reads=[pT2[d], RM], writes=[A2])
                k.op("dve", lambda e, d=d: e.tensor_tensor(out=A3[:, d * 4:(d + 1) * 4, :], in0=pT3[0:64, d * 256:(d + 1) * 256].rearrange("p (h t) -> p h t", h=4),
                                                           in1=RM[:, d, 4:5, :].to_broadcast([64, 4, 64]), op=ALU.mult), reads=[pT3, RM], writes=[A3])
            if SSTOP <= 2:
                continue
            pK = [ps(), ps()]
            for d in range(2):
                x_ = xs[d]
                for h in range(4):
                    dh = d * 4 + h
                    k.op("pe", lambda e: e.matmul(pK[0][0:64, dh * 64:(dh + 1) * 64], x_[:, h, 2, :], identb[0:64, 0:64], start=True, stop=True), reads=[x_, identb], writes=[pK[0]])
                    k.op("pe", lambda e: e.matmul(pK[1][0:64, dh * 64:(dh + 1) * 64], x_[:, h, 3, :], identb[0:64, 0:64], start=True, stop=True), reads=[x_, identb], writes=[pK[1]])
            pV = ps()
            for d in range(2):
                for h in range(4):
                    dh = d * 4 + h
                    k.op("pe", lambda e: e.matmul(pV[0:64, dh * 64:(dh + 1) * 64], xvs[d][:, h, :], identb[0:64, 0:64], start=True, stop=True), reads=[xvs[d], identb], writes=[pV])
            k.op("act", lambda e: e.copy(out=TOK[:, 0, :, :], in_=pK[0][0:64, :].rearrange("p (a t) -> p a t", t=64)), reads=[pK[0]], writes=[TOK])
            k.op("act", lambda e: e.copy(out=TOK[:, 1, :, :], in_=pK[1][0:64, :].rearrange("p (a t) -> p a t", t=64)), reads=[pK[1]], writes=[TOK])
            k.op("dve", lambda e: e.tensor_copy(out=TOK[:, 2, :, :], in_=pV[0:64, :].rearrange("p (a t) -> p a t", t=64)), reads=[pV], writes=[TOK])
            if SSTOP <= 3:
                continue
            pG = ps()
            for d in range(2):
                for h in range(4):
                    dh = d * 4 + h
                    o_ = pG[0:64, dh * 64:(dh + 1) * 64]
                    k.op("pe", lambda e: e.matmul(o_, xs[d][:, h, 0, :], S0b[:, dh, :], start=True, stop=False), reads=[xs[d], S0b], writes=[pG])
                    k.op("pe", lambda e: e.matmul(o_, A2[:, 0, dh, :], TOK[:, 2, dh, :], start=False, stop=True), reads=[A2, TOK], writes=[pG])
            k.op("dve", lambda e: e.tensor_copy(out=G[:], in_=pG[0:64, :].rearrange("p (a t) -> p a t", t=64)), reads=[pG], writes=[G])
            k.op("act", lambda e: e.copy(out=Gb[:], in_=pG[0:64, :].rearrange("p (a t) -> p a t", t=64)), reads=[pG], writes=[Gb])
            if SSTOP <= 4:
                continue
            cur = pq
            for kk in range(6):
                pD = ps()
                for dh in range(8):
                    k.op("pe", lambda e, dh=dh: e.matmul(pD[0:64, dh * 64:(dh + 1) * 64], cur[:, 0, dh, :], Gb[:, dh, :], start=True, stop=True), reads=[cur, Gb], writes=[pD])
                if kk < 5:
                    pP = ps()
                    pQ = ps()
                    for dh in range(8):
                        k.op("pe", lambda e, dh=dh: e.matmul(pP[0:64, dh * 64:(dh + 1) * 64], cur[:, 1, dh, :], cur[:, 0, dh, :], start=True, stop=True), reads=[cur], writes=[pP])
                        k.op("pe", lambda e, dh=dh: e.matmul(pQ[0:64, dh * 64:(dh + 1) * 64], cur[:, 0, dh, :], cur[:, 1, dh, :], start=True, stop=True), reads=[cur], writes=[pQ])
                k.op("dve", lambda e: e.tensor_tensor(out=G[:], in0=pD[0:64, :].rearrange("p (a t) -> p a t", t=64), in1=G[:], op=ALU.add), reads=[pD, G], writes=[G])
                k.op("pool", lambda e: e.tensor_copy(out=Gb[:], in_=G[:]), reads=[G], writes=[Gb])
                if kk < 5:
                    nx = k.nxt("rs_pq", PQ)
                    k.op("act", lambda e: e.copy(out=nx[:, 0, :, :], in_=pP[0:64, :].rearrange("p (a t) -> p a t", t=64)), reads=[pP], writes=[nx])
                    k.op("dve", lambda e: e.tensor_copy(out=nx[:, 1, :, :], in_=pQ[0:64, :].rearrange("p (a t) -> p a t", t=64)), reads=[pQ], writes=[nx])
                    cur = nx
            k.op("act", lambda e: e.activation(out=nUb[:], in_=G[:], func=AF.Copy, scale=-1.0), reads=[G], writes=[nUb])
            if SSTOP <= 5:
                continue
            pY = ps()
            for d in range(2):
                for h in range(4):
                    dh = d * 4 + h
                    o_ = pY[0:64, dh * 64:(dh + 1) * 64]
                    k.op("pe", lambda e: e.matmul(o_, xs[d][:, h, 1, :], S0b[:, dh, :], start=True, stop=False), reads=[xs[d], S0b], writes=[pY])
                    k.op("pe", lambda e: e.matmul(o_, A2[:, 1, dh, :], Gb[:, dh, :], start=False, stop=False), reads=[A2, Gb], writes=[pY])
                    k.op("pe", lambda e: e.matmul(o_, A3[:, dh, :], TOK[:, 2, dh, :], start=False, stop=True), reads=[A3, TOK], writes=[pY])
            y_ = k.nxt("rs_y", Ysb)
            k.op("act", lambda e: e.copy(out=y_[:], in_=pY[0:64, :].rearrange("p (a t) -> p a t", t=64)), reads=[pY], writes=[y_])
            for d in range(2):
                c = CH_ORDER[d][i]
                k.dma("sp", YS[d][c * 64:(c + 1) * 64, :].rearrange("p (h t) -> p h t", t=64), y_[:, d * 4:(d + 1) * 4, :], reads=[y_], writes=[YS[d]])
            if SSTOP <= 6:
                continue
            pS = ps()
            for d in range(2):
                for h in range(4):
                    dh = d * 4 + h
                    o_ = pS[0:64, dh * 64:(dh + 1) * 64]
                    k.op("pe", lambda e: e.matmul(o_, TOK[:, 0, dh, :], nUb[:, dh, :], start=True, stop=False), reads=[TOK, nUb], writes=[pS])
                    k.op("pe", lambda e: e.matmul(o_, TOK[:, 1, dh, :], TOK[:, 2, dh, :], start=False, stop=True), reads=[TOK], writes=[pS])
            k.op("dve", lambda e: e.tensor_tensor(out=stmp[:], in0=pS[0:64, :].rearrange("p (a t) -> p a t", t=64), in1=S0[:], op=ALU.add), reads=[pS, S0], writes=[stmp])
            for d in range(2):
                c = CH_ORDER[d][i]
                k.op("dve", lambda e, d=d, c=c: e.tensor_tensor(out=S0[:, d * 4:(d + 1) * 4, :], in0=stmp[:, d * 4:(d + 1) * 4, :],
                                                                in1=wl_sb[:, d * 4:(d + 1) * 4, c:c + 1].to_broadcast([64, 4, 64]), op=ALU.mult), reads=[stmp, wl_sb], writes=[S0])
            k.op("act", lambda e: e.copy(out=S0b[:], in_=S0[:]), reads=[S0], writes=[S0b])
        if DSTOP <= 2:
            return
        LNP = k.sbuf("rf_lnp", [128, 2, 2], F32)
        k.dma("sp", LNP[:], lnp_in[l], reads=[lnp_in], writes=[LNP])
        yf = mk("yf", [128, 4, 64], F32, 2)
        yb = mk("yb", [128, 4, 64], F32, 2)
        s4_ = mk("s4", [128, 4], F32, 2)
        q4_ = mk("q4", [128, 4, 64], F32, 2)
        bg = mk("bg", [128, 2, 2, 128], F32, 2)
        o2 = mk("o2", [128, 2, 128], F32, 2)
        ob = mk("ob", [128, 2, 128], BF16, 2)
        BONv = BON.t.ap().rearrange("(c p) t -> p c t", p=128)
        GGv = GGd.t.ap().rearrange("(c p) t -> p c t", p=128)
        MIXr = MIXT.t.ap()[512:768, :].rearrange("(c p) t -> p c t", p=128)
        for t in range(NTILE):
            tsl = slice(t * 128, (t + 1) * 128)
            a = k.nxt("rc_yf", yf)
            b = k.nxt("rc_yb", yb)
            s4 = k.nxt("rc_s4", s4_)
            q4 = k.nxt("rc_q4", q4_)
            k.dma("sp", a[:].rearrange("p h e -> p (h e)"), YS[0][tsl, :], reads=[YS[0]], writes=[a])
            k.dma("sp", b[:].rearrange("p h e -> p (h e)"), YS[1][tsl, :], reads=[YS[1]], writes=[b])
            k.op("dve", lambda e: e.tensor_tensor(out=a[:], in0=a[:], in1=b[:], op=ALU.add), reads=[a, b], writes=[a])
            k.op("dve", lambda e: e.tensor_reduce(out=s4[:], in_=a[:], axis=AX.X, op=ALU.add), reads=[a], writes=[s4])
            k.op("dve", lambda e: e.tensor_scalar(out=s4[:], in0=s4[:], scalar1=-1.0 / 64, scalar2=None, op0=ALU.mult), reads=[s4], writes=[s4])
            k.op("dve", lambda e: e.tensor_tensor(out=a[:], in0=a[:], in1=s4[:].unsqueeze(2).to_broadcast([128, 4, 64]), op=ALU.add), reads=[a, s4], writes=[a])
            k.op("pool", lambda e: e.tensor_tensor(out=q4[:], in0=a[:], in1=a[:], op=ALU.mult), reads=[a], writes=[q4])
            k.op("dve", lambda e: e.tensor_reduce(out=s4[:], in_=q4[:], axis=AX.X, op=ALU.add), reads=[q4], writes=[s4])
            k.op("act", lambda e: e.activation(out=s4[:], in_=s4[:], func=AF.Sqrt, scale=1.0 / 64, bias=64e-5), reads=[s4], writes=[s4])
            k.op("dve", lambda e: e.reciprocal(out=s4[:], in_=s4[:]), reads=[s4], writes=[s4])
            k.op("dve", lambda e: e.tensor_tensor(out=a[:], in0=a[:], in1=s4[:].unsqueeze(2).to_broadcast([128, 4, 64]), op=ALU.mult), reads=[a, s4], writes=[a])
            p = ps()
            av = a[:].rearrange("p h e -> p (h e)")
            for c in range(2):
                k.op("pe", lambda e, c=c: e.transpose(p[:, c * 128:(c + 1) * 128], av[:, c * 128:(c + 1) * 128], ident[:]), reads=[a, ident], writes=[p])
            g_ = k.nxt("rc_bg", bg)
            k.dma("sp", g_[:, 0, :, :], BONv[:, :, tsl], reads=[BON], writes=[g_])
            k.dma("sp", g_[:, 1, :, :], GGv[:, :, tsl], reads=[GGd], writes=[g_])
            o_ = k.nxt("rc_o2", o2)
            ob_ = k.nxt("rc_ob", ob)
            for c in range(2):
                k.op("act", lambda e, c=c: e.activation(out=o_[:, c, :], in_=p[:, c * 128:(c + 1) * 128], func=AF.Identity, scale=LNP[:, c, 0:1], bias=LNP[:, c, 1:2]), reads=[p, LNP], writes=[o_])
            k.op("dve", lambda e: e.tensor_tensor(out=o_[:], in0=o_[:], in1=g_[:, 0, :, :], op=ALU.add), reads=[o_, g_], writes=[o_])
            k.op("dve", lambda e: e.tensor_tensor(out=ob_[:], in0=o_[:], in1=g_[:, 1, :, :], op=ALU.mult), reads=[o_, g_], writes=[ob_])
            k.dma("sp", MIXr[:, :, tsl], ob_[:], reads=[ob_], writes=[MIXT])

    def phase_E(l):
        WO = k.sbuf("WO", [128, 8, D], BF16)
        for kc in range(8):
            k.dma("pool", WO[:, kc, :], w_out[l][kc * 128:(kc + 1) * 128, :], reads=[w_out], writes=[WO])
        mx = [k.sbuf("pe_mx%d" % i, [128, 8, 512], BF16) for i in range(2)]
        xb_ = [k.sbuf("pe_x%d" % i, [128, 8, 512], F32) for i in range(2)]
        MIXv8 = MIXT.t.ap().rearrange("(kc p) t -> p kc t", p=128)
        for (t0, n) in BLOCKS:
            s_ = 1 if t0 == 0 else 0
            if s_ == 1 and l == DEPTH - 1:
                continue
            m_ = k.nxt("pe_mx", mx)
            xb = k.nxt("pe_x", xb_)
            k.dma("sp", m_[:, :, 0:n], MIXv8[:, :, t0:t0 + n], reads=[MIXT], writes=[m_])
            k.dma("sp", xb[:, :, 0:n], XTv[:, :, t0:t0 + n], reads=[XT], writes=[xb])
            for oc in range(8):
                p = ps()
                for kc in range(8):
                    k.op("pe", lambda e, kc=kc: e.matmul(p[:, 0:n], WO[:, kc, oc * 128:(oc + 1) * 128], m_[:, kc, 0:n], start=(kc == 0), stop=(kc == 7)), reads=[WO, m_], writes=[p])
                k.op("dve", lambda e: e.scalar_tensor_tensor(out=xb[:, oc, 0:n], in0=p[:, 0:n], scalar=modT[l][:, 16 + oc, s_:s_ + 1], in1=xb[:, oc, 0:n], op0=ALU.mult, op1=ALU.add),
                     reads=[p, modT[l], xb], writes=[xb])
            k.dma("sp", XTv[:, :, t0:t0 + n], xb[:, :, 0:n], reads=[xb], writes=[XT])

    rw_in = ein("routerT", [L, 128, 8, 36])
    rb_in = ein("router_b", [L, 36])
    wgu_in = ein("exp_w_gu", [L, 32, D, D])
    wdn_in = ein("exp_w_down", [L, 32, 512, D])

    def phase_F(l):
        RW = k.sbuf("pf_rw", [128, 8, 36], F32)
        RB = k.sbuf("pf_rb", [128, 36], F32)
        k.dma("sp", RW[:], rw_in[l], reads=[rw_in], writes=[RW])
        k.dma("sp", RB[:], rb_in[l].partition_broadcast(128), reads=[rb_in], writes=[RB])
        xb = k.sbuf("pf_x", [128, 8, 512], F32)
        sq = k.sbuf("pf_sq", [128, 8, 512], F32)
        rr = k.sbuf("pf_r", [128, 512], F32)
        hf = k.sbuf("pf_hf", [128, 8, 512], F32)
        hbf2 = [k.sbuf("pf_hb%d" % i, [128, 8, 512], BF16) for i in range(2)]
        WT2 = [k.sbuf("pf_wt%d" % i, [128, 4, 32], F32) for i in range(2)]
        lg = k.sbuf("pf_lg", [128, 36], F32)
        gm = k.sbuf("pf_gm", [128, 1], F32)
        gmask = k.sbuf("pf_gmask", [128, 4], F32)
        gex = k.sbuf("pf_gex", [128, 4], F32)
        gw = k.sbuf("pf_gw", [128, 1], F32)
        e84 = k.sbuf("pf_e84", [128, 8, 4], F32)
        es = k.sbuf("pf_es", [128, 8], F32)
        m1 = k.sbuf("pf_m1", [128, 1], F32)
        m2 = k.sbuf("pf_m2", [128, 1], F32)
        k1 = k.sbuf("pf_k1", [128, 8], F32)
        k2 = k.sbuf("pf_k2", [128, 8], F32)
        es2 = k.sbuf("pf_es2", [128, 8], F32)
        p2 = k.sbuf("pf_p2", [128, 1], F32)
        w1 = k.sbuf("pf_w1", [128, 1], F32)
        w2 = k.sbuf("pf_w2", [128, 1], F32)
        wj = k.sbuf("pf_wj", [128, 8], F32)
        WGU = [k.sbuf("pf_wgu%d" % i, [128, 8, D], BF16) for i in range(2)]
        WDN = [k.sbuf("pf_wdn%d" % i, [128, 4, D], BF16) for i in range(2)]
        sg = [k.sbuf("pf_sg%d" % i, [128, 512], F32) for i in range(2)]
        act_ = [k.sbuf("pf_act%d" % i, [128, 4, 512], BF16) for i in range(2)]
        yacc2 = [k.sbuf("pf_yacc%d" % i, [128, 4, D], F32) for i in range(2)]
        blks = [b_ for b_ in BLOCKS if not (b_[0] == 0 and l == DEPTH - 1)]
        groups = [blks[i_:i_ + 2] for i_ in range(0, len(blks), 2)]
        for grp in groups:
            for gi, (t0, n) in enumerate(grp):
                hbf, WT, yacc = hbf2[gi], WT2[gi], yacc2[gi]
                s_ = 1 if t0 == 0 else 0
                nj = n // 128
                k.dma("sp", xb[:, :, 0:n], XTv[:, :, t0:t0 + n], reads=[XT], writes=[xb])
                k.op("act", lambda e: e.activation(out=sq[:, :, 0:n], in_=xb[:, :, 0:n], func=AF.Square), reads=[xb], writes=[sq])
                p = ps()
                for kc in range(8):
                    k.op("pe", lambda e, kc=kc: e.matmul(p[:, 0:n], ones_f[:], sq[:, kc, 0:n], start=(kc == 0), stop=(kc == 7)), reads=[ones_f, sq], writes=[p])
                k.op("act", lambda e: e.activation(out=rr[:, 0:n], in_=p[:, 0:n], func=AF.Sqrt, scale=1.0 / D, bias=EPS), reads=[p], writes=[rr])
                k.op("dve", lambda e: e.reciprocal(out=rr[:, 0:n], in_=rr[:, 0:n]), reads=[rr], writes=[rr])
                for kc in range(8):
                    k.op("dve", lambda e, kc=kc: e.tensor_tensor(out=sq[:, kc, 0:n], in0=xb[:, kc, 0:n], in1=rr[:, 0:n], op=ALU.mult), reads=[xb, rr], writes=[sq])
                    k.op("act", lambda e, kc=kc: e.activation(out=hf[:, kc, 0:n], in_=sq[:, kc, 0:n], func=AF.Identity, scale=A2[l][:, kc, s_:s_ + 1], bias=modT[l][:, 24 + kc, s_:s_ + 1]),
                         reads=[sq, A2[l], modT[l]], writes=[hf])
                k.op("pool", lambda e: e.tensor_copy(out=hbf[:, :, 0:n], in_=hf[:, :, 0:n]), reads=[hf], writes=[hbf])
                for j in range(nj):
                    jsl = slice(j * 128, (j + 1) * 128)
                    p = ps()
                    for kc in range(8):
                        k.op("pe", lambda e, kc=kc: e.matmul(p[:, 0:36], hf[:, kc, jsl], RW[:, kc, :], start=(kc == 0), stop=(kc == 7)), reads=[hf, RW], writes=[p])
                    k.op("dve", lambda e: e.tensor_tensor(out=lg[:], in0=p[:, 0:36], in1=RB[:], op=ALU.add), reads=[p, RB], writes=[lg])
                    k.op("dve", lambda e: e.tensor_reduce(out=gm[:], in_=lg[:, 0:4], axis=AX.X, op=ALU.max), reads=[lg], writes=[gm])
                    k.op("dve", lambda e: e.tensor_scalar(out=gmask[:], in0=lg[:, 0:4], scalar1=gm[:, 0:1], scalar2=None, op0=ALU.is_equal), reads=[lg, gm], writes=[gmask])
                    k.op("dve", lambda e: e.tensor_scalar(out=gex[:], in0=lg[:, 0:4], scalar1=gm[:, 0:1], scalar2=None, op0=ALU.subtract), reads=[lg, gm], writes=[gex])
                    k.op("act", lambda e: e.activation(out=gex[:], in_=gex[:], func=AF.Exp), reads=[gex], writes=[gex])
                    k.op("dve", lambda e: e.tensor_reduce(out=gw[:], in_=gex[:], axis=AX.X, op=ALU.add), reads=[gex], writes=[gw])
                    k.op("dve", lambda e: e.reciprocal(out=gw[:], in_=gw[:]), reads=[gw], writes=[gw])
                    k.op("dve", lambda e: e.tensor_tensor(out=e84[:], in0=lg[:, 4:36].rearrange("p (g j) -> p j g", j=8), in1=gmask[:].unsqueeze(1).to_broadcast([128, 8, 4]), op=ALU.mult),
                         reads=[lg, gmask], writes=[e84])
                    k.op("dve", lambda e: e.tensor_reduce(out=es[:], in_=e84[:], axis=AX.X, op=ALU.add), reads=[e84], writes=[es])
                    k.op("dve", lambda e: e.tensor_reduce(out=m1[:], in_=es[:], axis=AX.X, op=ALU.max), reads=[es], writes=[m1])
                    k.op("dve", lambda e: e.tensor_scalar(out=k1[:], in0=es[:], scalar1=m1[:, 0:1], scalar2=None, op0=ALU.is_equal), reads=[es, m1], writes=[k1])
                    k.op("dve", lambda e: e.scalar_tensor_tensor(out=es2[:], in0=k1[:], scalar=-1e30, in1=es[:], op0=ALU.mult, op1=ALU.add), reads=[k1, es], writes=[es2])
                    k.op("dve", lambda e: e.tensor_reduce(out=m2[:], in_=es2[:], axis=AX.X, op=ALU.max), reads=[es2], writes=[m2])
                    k.op("dve", lambda e: e.tensor_scalar(out=k2[:], in0=es2[:], scalar1=m2[:, 0:1], scalar2=None, op0=ALU.is_equal), reads=[es2, m2], writes=[k2])
                    k.op("dve", lambda e: e.tensor_tensor(out=p2[:], in0=m2[:], in1=m1[:], op=ALU.subtract), reads=[m1, m2], writes=[p2])
                    k.op("act", lambda e: e.activation(out=p2[:], in_=p2[:], func=AF.Exp), reads=[p2], writes=[p2])
                    k.op("dve", lambda e: e.tensor_scalar(out=w1[:], in0=p2[:], scalar1=1.0, scalar2=None, op0=ALU.add), reads=[p2], writes=[w1])
                    k.op("dve", lambda e: e.reciprocal(out=w1[:], in_=w1[:]), reads=[w1], writes=[w1])
                    k.op("dve", lambda e: e.tensor_tensor(out=w1[:], in0=w1[:], in1=gw[:], op=ALU.mult), reads=[w1, gw], writes=[w1])
                    k.op("dve", lambda e: e.tensor_tensor(out=w2[:], in0=w1[:], in1=p2[:], op=ALU.mult), reads=[w1, p2], writes=[w2])
                    k.op("dve", lambda e: e.tensor_scalar(out=wj[:], in0=k1[:], scalar1=w1[:, 0:1], scalar2=None, op0=ALU.mult), reads=[k1, w1], writes=[wj])
                    k.op("dve", lambda e: e.scalar_tensor_tensor(out=wj[:], in0=k2[:], scalar=w2[:, 0:1], in1=wj[:], op0=ALU.mult, op1=ALU.add), reads=[k2, w2, wj], writes=[wj])
                    k.op("dve", lambda e, j=j: e.tensor_tensor(out=WT[:, j, :].rearrange("p (g j) -> p g j", j=8), in0=gmask[:].unsqueeze(2).to_broadcast([128, 4, 8]),
                                                               in1=wj[:].unsqueeze(1).to_broadcast([128, 4, 8]), op=ALU.mult), reads=[gmask, wj], writes=[WT])
            for ex in range(32):
                wg_ = k.nxt("pf_wgu", WGU)
                wd_ = k.nxt("pf_wdn", WDN)
                for kc in range(8):
                    k.dma("pool", wg_[:, kc, :], wgu_in[l][ex][kc * 128:(kc + 1) * 128, :], reads=[wgu_in], writes=[wg_])
                for hk in range(4):
                    k.dma("pool", wd_[:, hk, :], wdn_in[l][ex][hk * 128:(hk + 1) * 128, :], reads=[wdn_in], writes=[wd_])
                for gi, (t0, n) in enumerate(grp):
                    hbf, WT, yacc = hbf2[gi], WT2[gi], yacc2[gi]
                    nj = n // 128
                    a_ = k.nxt("pf_act", act_)
                    for hc in range(4):
                        pg = ps()
                        pu = ps()
                        for kc in range(8):
                            k.op("pe", lambda e, kc=kc: e.matmul(pg[:, 0:n], wg_[:, kc, hc * 128:(hc + 1) * 128], hbf[:, kc, 0:n], start=(kc == 0), stop=(kc == 7)), reads=[wg_, hbf], writes=[pg])
                        for kc in range(8):
                            k.op("pe", lambda e, kc=kc: e.matmul(pu[:, 0:n], wg_[:, kc, 512 + hc * 128:512 + (hc + 1) * 128], hbf[:, kc, 0:n], start=(kc == 0), stop=(kc == 7)), reads=[wg_, hbf], writes=[pu])
                        s2 = k.nxt("pf_sg", sg)
                        k.op("act", lambda e: e.activation(out=s2[:, 0:n], in_=pg[:, 0:n], func=AF.Silu), reads=[pg], writes=[s2])
                        k.op("dve", lambda e, hc=hc: e.tensor_tensor(out=a_[:, hc, 0:n], in0=pu[:, 0:n], in1=s2[:, 0:n], op=ALU.mult), reads=[pu, s2], writes=[a_])
                    for j in range(nj):
                        jsl = slice(j * 128, (j + 1) * 128)
                        for half in range(2):
                            py = ps()
                            for hk in range(4):
                                k.op("pe", lambda e, hk=hk: e.matmul(py[:, :], a_[:, hk, jsl], wd_[:, hk, half * 512:(half + 1) * 512], start=(hk == 0), stop=(hk == 3)), reads=[a_, wd_], writes=[py])
                            if ex == 0:
                                k.op("dve", lambda e: e.tensor_scalar(out=yacc[:, j, half * 512:(half + 1) * 512], in0=py[:, :], scalar1=WT[:, j, ex:ex + 1], scalar2=None, op0=ALU.mult),
                                     reads=[py, WT], writes=[yacc])
                            else:
                                k.op("dve", lambda e: e.scalar_tensor_tensor(out=yacc[:, j, half * 512:(half + 1) * 512], in0=py[:, :], scalar=WT[:, j, ex:ex + 1], in1=yacc[:, j, half * 512:(half + 1) * 512],
                                                                             op0=ALU.mult, op1=ALU.add), reads=[py, WT, yacc], writes=[yacc])
            for gi, (t0, n) in enumerate(grp):
                hbf, WT, yacc = hbf2[gi], WT2[gi], yacc2[gi]
                s_ = 1 if t0 == 0 else 0
                nj = n // 128
                k.dma("sp", xb[:, :, 0:n], XTv[:, :, t0:t0 + n], reads=[XT], writes=[xb])
                for j in range(nj):
                    jsl = slice(j * 128, (j + 1) * 128)
                    for half in range(2):
                        p = ps()
                        for q in range(4):
                            oc = half * 4 + q
                            k.op("pe", lambda e, q=q, oc=oc: e.transpose(p[:, q * 128:(q + 1) * 128], yacc[:, j, oc * 128:(oc + 1) * 128], ident[:]), reads=[yacc, ident], writes=[p])
                        for q in range(4):
                            oc = half * 4 + q
                            k.op("dve", lambda e, q=q, oc=oc: e.scalar_tensor_tensor(out=xb[:, oc, jsl], in0=p[:, q * 128:(q + 1) * 128], scalar=modT[l][:, 40 + oc, s_:s_ + 1], in1=xb[:, oc, jsl],
                                                                                     op0=ALU.mult, op1=ALU.add), reads=[p, modT[l], xb], writes=[xb])
                k.dma("sp", XTv[:, :, t0:t0 + n], xb[:, :, 0:n], reads=[xb], writes=[XT])

    for l in range(n_layers):
        if stage >= 1:
            with k.scope():
                phase_A(l)
        if stage >= 2 and 'B' not in DBG_SKIP:
            with k.scope():
                phase_B(l)
        if stage >= 3 and 'D' not in DBG_SKIP:
            with k.scope():
                phase_D(l)
        if stage >= 4 and 'C' not in DBG_SKIP:
            with k.scope():
                phase_C(l)
        if stage >= 5:
            with k.scope():
                phase_E(l)
        if stage >= 6:
            with k.scope():
                phase_F(l)

    with k.scope():
        if dbg_out:
            tq = k.sbuf("dbgq", [128, NT], BF16)
            tf = k.sbuf("dbgf", [128, NPAD], F32)

        def dump_bf(src_ap, srcbuf, dst_ap, dstbuf):
            k.dma("sp", tq[:], src_ap, reads=[srcbuf], writes=[tq])
            k.op("dve", lambda e: e.tensor_copy(out=tf[:, 0:NT], in_=tq[:]), reads=[tq], writes=[tf])
            k.dma("sp", dst_ap, tf[:, 0:NT], reads=[tf], writes=[dstbuf])

        def dump_f(src, dst, rows, cols):
            for r0 in range(0, rows, 128):
                nr = min(128, rows - r0)
                k.dma("sp", tf[0:nr, 0:cols], src[r0:r0 + nr, :], reads=[src], writes=[tf])
                k.dma("sp", dst[r0:r0 + nr, :], tf[0:nr, 0:cols], reads=[tf], writes=[dst])
        for name in dbg_out:
            if name == "QT":
                dump_bf(QT[0], QT, dbg_out["QT"][:], dbg_out["QT"])
            if name == "KT":
                dump_bf(KT[1], KT, dbg_out["KT"][:], dbg_out["KT"])
            if name == "MIXT":
                for r0 in range(0, 1024, 128):
                    dump_bf(MIXT[r0:r0 + 128, :], MIXT, dbg_out["MIXT"][r0:r0 + 128, :], dbg_out["MIXT"])
            if name == "RZT":
                dump_f(RZT, dbg_out["RZT"], 960, NPAD)
            if name == "MQKT":
                dump_f(MQKT, dbg_out["MQKT"], 512, NPAD)
            if name == "MGT":
                dump_f(MGT, dbg_out["MGT"], 16, NT)
            if name == "XT":
                dump_f(XT, dbg_out["XT"], D, NT)

    with k.scope():
        fin_x = [k.sbuf("finx%d" % i, [128, 8, 512], F32) for i in range(2)]
        fin_sq = k.sbuf("finsq", [128, 8, 512], F32)
        fin_r = k.sbuf("finr", [128, 512], F32)
        fin_o = [k.sbuf("fino%d" % i, [128, D], F32) for i in range(2)]
        for (t0, n) in BLOCKS[1:]:
            xb = k.nxt("finx", fin_x)
            k.dma("sp", xb[:], XTv[:, :, t0:t0 + n], reads=[XT], writes=[xb])
            k.op("act", lambda e: e.activation(out=fin_sq[:], in_=xb[:], func=AF.Square), reads=[xb], writes=[fin_sq])
            p = ps()
            for kc in range(8):
                k.op("pe", lambda e, kc=kc: e.matmul(p[:, :], ones_f[:], fin_sq[:, kc, :], start=(kc == 0), stop=(kc == 7)),
                     reads=[ones_f, fin_sq], writes=[p])
            k.op("act", lambda e: e.activation(out=fin_r[:], in_=p[:, :], func=AF.Sqrt, scale=1.0 / D, bias=EPS), reads=[p], writes=[fin_r])
            k.op("dve", lambda e: e.reciprocal(out=fin_r[:], in_=fin_r[:]), reads=[fin_r], writes=[fin_r])
            for kc in range(8):
                k.op("dve", lambda e, kc=kc: e.scalar_tensor_tensor(out=xb[:, kc, :], in0=xb[:, kc, :], scalar=fg[:, kc:kc + 1], in1=fin_r[:],
                                                                    op0=ALU.mult, op1=ALU.mult), reads=[xb, fg, fin_r], writes=[xb])
            for j in range(n // 128):
                fo = k.nxt("fino", fin_o)
                for half in range(2):
                    p = ps()
                    for q in range(4):
                        kc = half * 4 + q
                        k.op("pe", lambda e, kc=kc, q=q, j=j: e.transpose(p[:, q * 128:(q + 1) * 128], xb[:, kc, j * 128:(j + 1) * 128], ident[:]),
                             reads=[xb, ident], writes=[p])
                    if half == 0:
                        k.op("act", lambda e: e.copy(out=fo[:, 0:512], in_=p[:, :]), reads=[p], writes=[fo])
                    else:
                        k.op("dve", lambda e: e.tensor_copy(out=fo[:, 512:1024], in_=p[:, :]), reads=[p], writes=[fo])
                r0 = t0 - TC + j * 128
                k.dma("sp", out[r0:r0 + 128, :], fo[:], reads=[fo], writes=[out])


def host_inputs(inputs, b):
    f = np.float32
    L = DEPTH
    c2 = np.stack([inputs["c"][b], inputs["c_ctx"]], 0).astype(f)
    c2T = np.ascontiguousarray(c2.reshape(2, 8, 128).transpose(2, 1, 0))
    perm = rope_partner_perm()
    w_in = inputs["w_in"]
    qk = w_in[:, :, 0:1024].reshape(L, D, 16, 64)
    w_inp = np.ascontiguousarray(qk[:, :, :, perm].reshape(L, D, 1024))
    cos, sin = rope_tables()
    m = {
        "x": np.ascontiguousarray(inputs["x"][b]),
        "ctx": np.ascontiguousarray(inputs["ctx"][b]),
        "c2T": c2T,
        "ada_w": inputs["ada_w"],
        "ada_bT": np.ascontiguousarray(inputs["ada_b"].reshape(L, 48, 128).transpose(0, 2, 1)),
        "n1gT": np.ascontiguousarray(inputs["norm1_g"].reshape(L, 8, 128).transpose(0, 2, 1)),
        "n2gT": np.ascontiguousarray(inputs["norm2_g"].reshape(L, 8, 128).transpose(0, 2, 1)),
        "w_in": w_in,
        "w_inp": w_inp,
        "w_out": inputs["w_out"],
        "cos_t": cos,
        "sin_t": sin,
        "ident": np.eye(128, dtype=f),
        "fgT": np.ascontiguousarray(inputs["final_g"].reshape(8, 128).T),
        "da_lambda": np.ascontiguousarray(inputs["da_lambda"].reshape(L, 256)),
        "sublnT": np.ascontiguousarray(inputs["da_subln_g"].T),
        "mcwT": np.ascontiguousarray(inputs["ml_conv_w"].reshape(L, 3, 4, 128).transpose(0, 3, 2, 1)),
        "mcbT": np.ascontiguousarray(inputs["ml_conv_b"].reshape(L, 4, 128).transpose(0, 2, 1)),
        "mgbT": np.ascontiguousarray(inputs["ml_gate_b"].reshape(L, 16, 1)),
        "mngT": np.ascontiguousarray(inputs["ml_norm_g"].reshape(L, 2, 128).transpose(0, 2, 1)),
        "triu": np.triu(np.ones((128, 128), f)),
        "tril": np.tril(np.ones((128, 128), f)),
        "routerT": np.ascontiguousarray(np.concatenate([inputs["router_wg"], inputs["router_we"]], -1).reshape(L, 8, 128, 36).transpose(0, 2, 1, 3)),
        "router_b": np.ascontiguousarray(np.concatenate([inputs["router_bg"], inputs["router_be"]], -1)),
        "rw_par": rw_par(inputs),
        "rw_w2p": rw_pad(inputs["rw_w2"]),
        "rw_a2p": rw_pad(inputs["rw_a2"]),
        "rw_g2": inputs["rw_g2"],
        "rw_lnp": np.ascontiguousarray(np.stack([inputs["rw_ln_g"].reshape(L, 2, 128), inputs["rw_ln_b"].reshape(L, 2, 128)], -1).transpose(0, 2, 1, 3)),
        "rw_mask": rw_masks(),
        "rw_reset": rw_reset(),
        "exp_w_gu": inputs["exp_w_gu"],
        "exp_w_down": inputs["exp_w_down"],
    }
    return m


def rw_par(inputs):
    L = DEPTH
    P = np.zeros((L, 64, 48), np.float32)
    mu = inputs["rw_shift_mu"]
    for h in range(4):
        P[:, :, 0 + h] = mu[:, h * 64:(h + 1) * 64]
        P[:, :, 4 + h] = mu[:, 256 + h * 64:256 + (h + 1) * 64]
        P[:, :, 8 + h] = mu[:, 512 + h * 64:512 + (h + 1) * 64]
        P[:, :, 31 + h] = inputs["rw_k_k"][:, h * 64:(h + 1) * 64]
        P[:, :, 35 + h] = inputs["rw_k_a"][:, h * 64:(h + 1) * 64]
        P[:, :, 43 + h] = inputs["rw_r_k"][:, h, :]
        for d in range(2):
            P[:, :, 15 + d * 4 + h] = inputs["rw_w0"][:, d, h * 64:(h + 1) * 64]
            P[:, :, 23 + d * 4 + h] = inputs["rw_a0"][:, d, h * 64:(h + 1) * 64]
    P[:, :, 12] = mu[:, 768:832]
    P[:, :, 13] = mu[:, 832:896]
    P[:, :, 14] = mu[:, 896:960]
    return P


def rw_pad(w):
    L = w.shape[0]
    o = np.zeros((L, 2, 64, 256), np.float32)
    o[:, 0, 0:32] = w[:, 0]
    o[:, 1, 32:64] = w[:, 1]
    return o


def rw_masks():
    f = np.float32
    su = np.triu(np.ones((64, 64), f), 1)
    iu = np.triu(np.ones((64, 64), f), 0)
    sl = np.tril(np.ones((64, 64), f), -1)
    il = np.tril(np.ones((64, 64), f), 0)
    m = np.zeros((2, 64, 5, 64), f)
    m[0, :, 0] = -su; m[0, :, 1] = -sl; m[0, :, 2] = su; m[0, :, 3] = -iu; m[0, :, 4] = iu
    m[1, :, 0] = -sl; m[1, :, 1] = -su; m[1, :, 2] = sl; m[1, :, 3] = -il; m[1, :, 4] = il
    return m


def rw_reset():
    r = np.ones((64, 512), np.float32)
    r[:, ::64] = 0.0
    return r


def kernel(**inputs):
    inputs = {k_: np.asarray(v) for k_, v in inputs.items()}
    nc = build()
    in_maps = [host_inputs(inputs, b) for b in range(8)]
    res = run_bass_kernel_spmd(nc, in_maps, core_ids=list(range(8)))
    return np.stack([r["out"] for r in res.results], 0).astype(np.float32)
```

```python
import math
import os
DBG_STOP = int(os.environ.get('DBG_STOP', '99'))
DBG_SKIP = os.environ.get('DBG_SKIP', '')
DSTOP = int(os.environ.get('DSTOP', '99'))
SSTOP = int(os.environ.get('SSTOP', '99'))
NSTEP = int(os.environ.get('NSTEP', '68'))
import numpy as np
from contextlib import ExitStack
import concourse.bass as bass
import concourse.mybir as mybir
from concourse.alu_op_type import AluOpType as ALU
from concourse.bass_utils import run_bass_kernel_spmd

AF = mybir.ActivationFunctionType
AX = mybir.AxisListType
F32 = mybir.dt.float32
BF16 = mybir.dt.bfloat16
I32 = mybir.dt.int32
U32 = mybir.dt.uint32

D = 1024
T = 4096
TC = 256
NT = T + TC
NTILE = NT // 128
DEPTH = 4
IN_COLS = 3536
DA0, RW0, ML0 = 0, 1536, 2496
EPS = 1e-6
BLOCKS = [(0, 256)] + [(256 + 512 * i, 512) for i in range(8)]
NPAD = 4356


def padcol(t):
    return t + 1 if t < 256 else t + 3


class Buf:
    def __init__(self, t, name=""):
        self.t = t
        self.name = name
        self.w = {}
        self.r = {}

    def __getitem__(self, idx):
        return self.t[idx]


class Ctx:
    NPOOL = 12

    def __init__(self, nc, stack, same_engine_sync=True):
        self.nc = nc
        self.st = stack
        self.same = same_engine_sync
        self.eng = dict(pe=nc.tensor, dve=nc.vector, act=nc.scalar, pool=nc.gpsimd, sp=nc.sync)
        self.semh = {}
        self.cnt = {}
        for e in self.eng:
            self.semh[e] = stack.enter_context(nc.semaphore("s_" + e))
            self.cnt[e] = 0
        self.seen = {e: {} for e in self.eng}
        self.dq = {}
        for q in ("sp", "pool", "act"):
            lst = []
            for i in range(self.NPOOL):
                key = "d_%s_%d" % (q, i)
                self.semh[key] = stack.enter_context(nc.semaphore(key))
                self.cnt[key] = 0
                lst.append(key)
            self.dq[q] = [lst, 0]
        self.ninstr = 0
        self.rr = {}

    def sbuf(self, name, shape, dt):
        self.uid = getattr(self, "uid", 0) + 1
        return Buf(self.st.enter_context(self.nc.sbuf_tensor("sb%d_%s" % (self.uid, name), list(shape), dt)), name)

    def psum(self, name, shape, dt=F32):
        b = Buf(self.st.enter_context(self.nc.psum_tensor("pp_" + name, list(shape), dt)), name)
        b.psum = True
        return b

    def dram(self, name, shape, dt, kind="Internal"):
        return Buf(self.nc.dram_tensor(name, list(shape), dt, kind=kind), name)

    def _wait(self, e, deps):
        eng = self.eng[e]
        seen = self.seen[e]
        for key, val in deps.items():
            if key == e and (e == "pe" or not self.same):
                continue
            if seen.get(key, 0) >= val:
                continue
            eng.wait_ge(self.semh[key], val)
            seen[key] = val

    @staticmethod
    def _merge(d, s):
        for k, v in s.items():
            if d.get(k, 0) < v:
                d[k] = v

    def _deps(self, reads, writes):
        deps = {}
        for b in reads:
            self._merge(deps, b.w)
            if getattr(b, "psum", False):
                self._merge(deps, b.r)
        for b in writes:
            self._merge(deps, b.w)
            self._merge(deps, b.r)
        return deps

    def _mark(self, key, val, reads, writes):
        for b in reads:
            if b.r.get(key, 0) < val:
                b.r[key] = val
        for b in writes:
            if b.w.get(key, 0) < val:
                b.w[key] = val

    def op(self, e, fn, reads=(), writes=()):
        self._wait(e, self._deps(reads, writes))
        ins = fn(self.eng[e])
        self.cnt[e] += 1
        ins.then_inc(self.semh[e], 1)
        self._mark(e, self.cnt[e], reads, writes)
        self.ninstr += 1
        return ins

    def dma(self, q, out, in_, reads=(), writes=(), **kw):
        lst, rr = self.dq[q]
        key = lst[rr % len(lst)]
        self.dq[q][1] = rr + 1
        deps = self._deps(reads, writes)
        deps[key] = max(deps.get(key, 0), self.cnt[key])
        self._wait(q, deps)
        ins = self.eng[q].dma_start(out=out, in_=in_, **kw)
        self.cnt[key] += 16
        ins.then_inc(self.semh[key], 16)
        self._mark(key, self.cnt[key], reads, writes)
        self.ninstr += 1
        return ins

    def finish(self, e="sp"):
        deps = {}
        for q in self.dq:
            for key in self.dq[q][0]:
                if self.cnt[key]:
                    deps[key] = self.cnt[key]
        for k in self.eng:
            if self.cnt[k]:
                deps[k] = self.cnt[k]
        self._wait(e, deps)

    def barrier(self):
        deps = {}
        for q in self.dq:
            for key in self.dq[q][0]:
                if self.cnt[key]:
                    deps[key] = self.cnt[key]
        for e in self.eng:
            if self.cnt[e]:
                deps[e] = self.cnt[e]
        for e in self.eng:
            self._wait(e, deps)

    def scope(self):
        ctx = self

        class _S:
            def __enter__(s_):
                s_.old = ctx.st
                s_.sub = ExitStack()
                ctx.st = s_.sub
                return s_

            def __exit__(s_, *a):
                if a[0] is None:
                    ctx.barrier()
                s_.sub.close()
                ctx.st = s_.old
                return False
        return _S()

    def nxt(self, name, lst):
        i = self.rr.get(name, 0)
        self.rr[name] = i + 1
        return lst[i % len(lst)]


def rope_tables():
    nf = 16
    inv = 10000.0 ** (-np.arange(nf, dtype=np.float32) / nf)
    t = np.arange(T)
    row = (t // 64).astype(np.float32)
    col = (t % 64).astype(np.float32)
    cos = np.ones((128, NT), np.float32)
    sin = np.zeros((128, NT), np.float32)
    for p in range(128):
        d = p % 64
        pos = row if d < 32 else col
        j = d % 32
        f = j % 16
        ang = pos * inv[f]
        cos[p, TC:] = np.cos(ang)
        s = np.sin(ang)
        sin[p, TC:] = -s if j < 16 else s
    return cos, sin


def rope_partner_perm():
    perm = np.zeros(64, np.int64)
    for d in range(64):
        j = d % 32
        perm[d] = d + 16 if j < 16 else d - 16
    return perm


def build(n_layers=DEPTH, stage=99, dbg=()):
    nc = bass.Bass("TRN2", target_bir_lowering=False)
    st = ExitStack()
    with st:
        k = Ctx(nc, st)
        build_body(nc, k, n_layers, stage, dbg)
        k.finish()
        print("instructions:", k.ninstr, {e: k.cnt[e] for e in k.eng})
    return nc


def build_body(nc, k, n_layers, stage, dbg):
    L = DEPTH
    ein = lambda name, shape, dt=F32: k.dram(name, shape, dt, kind="ExternalInput")
    x_in = ein("x", [T, D])
    ctx_in = ein("ctx", [TC, D])
    c2T_in = ein("c2T", [128, 8, 2])
    ada_w = ein("ada_w", [L, D, 6 * D])
    ada_bT = ein("ada_bT", [L, 128, 48])
    n1gT = ein("n1gT", [L, 128, 8])
    n2gT = ein("n2gT", [L, 128, 8])
    w_in = ein("w_in", [L, D, IN_COLS])
    w_inp = ein("w_inp", [L, D, 1024])
    w_out = ein("w_out", [L, D, D])
    cos_in = ein("cos_t", [128, NT])
    sin_in = ein("sin_t", [128, NT])
    ident_in = ein("ident", [128, 128])
    fgT = ein("fgT", [128, 8])
    out = k.dram("out", [T, D], F32, kind="ExternalOutput")
    dbg_out = {}
    for name, shape in dbg:
        dbg_out[name] = k.dram("dbg_" + name, shape, F32, kind="ExternalOutput")

    XT = k.dram("XT", [D, NT], F32)
    QT = k.dram("QT", [4, 128, NT], BF16)
    KT = k.dram("KT", [4, 128, NT], BF16)
    VA = k.dram("VA", [NT, 4 * 130], BF16)
    MIXT = k.dram("MIXT", [D, NT], BF16)

    ident = k.sbuf("ident", [128, 128], F32)
    identb = k.sbuf("identb", [128, 128], BF16)
    ones_f = k.sbuf("ones_f", [128, 128], F32)
    k.dma("sp", ident[:], ident_in[:], reads=[ident_in], writes=[ident])
    k.op("dve", lambda e: e.tensor_copy(out=identb[:], in_=ident[:]), reads=[ident], writes=[identb])
    k.op("dve", lambda e: e.memset(ones_f[:], 1.0), writes=[ones_f])

    PS = [k.psum("ps%d" % i, [128, 512], F32) for i in range(8)]

    def ps():
        return k.nxt("ps", PS)

    c2T = k.sbuf("c2T", [128, 8, 2], F32)
    sc2T = k.sbuf("sc2T", [128, 8, 2], F32)
    k.dma("sp", c2T[:], c2T_in[:], reads=[c2T_in], writes=[c2T])
    k.op("act", lambda e: e.activation(out=sc2T[:], in_=c2T[:], func=AF.Silu), reads=[c2T], writes=[sc2T])
    modT = [k.sbuf("modT%d" % l, [128, 48, 2], F32) for l in range(L)]
    adab = [k.sbuf("adab%d" % l, [128, 48], F32) for l in range(L)]
    n1g = [k.sbuf("n1g%d" % l, [128, 8], F32) for l in range(L)]
    n2g = [k.sbuf("n2g%d" % l, [128, 8], F32) for l in range(L)]
    A1 = [k.sbuf("A1_%d" % l, [128, 8, 2], F32) for l in range(L)]
    A2 = [k.sbuf("A2_%d" % l, [128, 8, 2], F32) for l in range(L)]
    fg = k.sbuf("fg", [128, 8], F32)
    k.dma("sp", fg[:], fgT[:], reads=[fgT], writes=[fg])
    with k.scope():
        adaw_sb = [k.sbuf("adaw%d" % i, [128, 8, 768], F32) for i in range(2)]
        for l in range(n_layers):
            k.dma("sp", adab[l][:], ada_bT[l], reads=[ada_bT], writes=[adab[l]])
            k.dma("sp", n1g[l][:], n1gT[l], reads=[n1gT], writes=[n1g[l]])
            k.dma("sp", n2g[l][:], n2gT[l], reads=[n2gT], writes=[n2g[l]])
            for piece in range(8):
                wsb = k.nxt("adaw", adaw_sb)
                for kc in range(8):
                    k.dma("sp" if kc % 2 == 0 else "pool", wsb[:, kc, :],
                          ada_w[l][kc * 128:(kc + 1) * 128, piece * 768:(piece + 1) * 768],
                          reads=[ada_w], writes=[wsb])
                p = ps()
                for cc in range(6):
                    for kc in range(8):
                        k.op("pe", lambda e, cc=cc, kc=kc: e.matmul(
                            p[:, cc * 2:(cc + 1) * 2], wsb[:, kc, cc * 128:(cc + 1) * 128], sc2T[:, kc, :],
                            start=(kc == 0), stop=(kc == 7)), reads=[wsb, sc2T], writes=[p])
                k.op("dve", lambda e, piece=piece: e.tensor_tensor(
                    out=modT[l][:, piece * 6:(piece + 1) * 6, :],
                    in0=p[:, 0:12].rearrange("p (c s) -> p c s", s=2),
                    in1=adab[l][:, piece * 6:(piece + 1) * 6].unsqueeze(2).to_broadcast([128, 6, 2]),
                    op=ALU.add), reads=[p, adab[l]], writes=[modT[l]])
            for (A, g, c0) in ((A1[l], n1g[l], 8), (A2[l], n2g[l], 32)):
                k.op("dve", lambda e, A=A, g=g, c0=c0: e.scalar_tensor_tensor(
                    out=A[:], in0=modT[l][:, c0:c0 + 8, :], scalar=1.0,
                    in1=g[:].unsqueeze(2).to_broadcast([128, 8, 2]), op0=ALU.add, op1=ALU.mult),
                    reads=[modT[l], g], writes=[A])
        if "modT" in dbg_out:
            k.dma("sp", dbg_out["modT"][:], modT[0][:], reads=[modT[0]], writes=[dbg_out["modT"]])

        xin_sb = [k.sbuf("xin%d" % i, [128, D], F32) for i in range(2)]
        xtr_sb = [k.sbuf("xtr%d" % i, [128, 8, 128], F32) for i in range(2)]
        XTv = XT.t.ap().rearrange("(kc p) t -> p kc t", p=128)
        for tt in range(NTILE):
            xs = k.nxt("xin", xin_sb)
            src = ctx_in[tt * 128:(tt + 1) * 128, :] if tt < 2 else x_in[(tt - 2) * 128:(tt - 1) * 128, :]
            k.dma("sp" if tt % 2 == 0 else "pool", xs[:], src, reads=[], writes=[xs])
            xo = k.nxt("xtr", xtr_sb)
            for half in range(2):
                p = ps()
                for j in range(4):
                    kc = half * 4 + j
                    k.op("pe", lambda e, kc=kc, j=j: e.transpose(p[:, j * 128:(j + 1) * 128], xs[:, kc * 128:(kc + 1) * 128], ident[:]),
                         reads=[xs, ident], writes=[p])
                k.op("act" if half == 0 else "dve",
                     (lambda e, half=half: e.copy(out=xo[:, half * 4:(half + 1) * 4, :], in_=p[:, :].rearrange("p (j t) -> p j t", t=128))) if half == 0 else
                     (lambda e, half=half: e.tensor_copy(out=xo[:, half * 4:(half + 1) * 4, :], in_=p[:, :].rearrange("p (j t) -> p j t", t=128))),
                     reads=[p], writes=[xo])
            k.dma("sp", XTv[:, :, tt * 128:(tt + 1) * 128], xo[:], reads=[xo], writes=[XT])

    da_lam_in = ein("da_lambda", [L, 256])
    sublnT_in = ein("sublnT", [128, L])
    RZT = k.dram("RZT", [960, NPAD], F32)
    MQKT = k.dram("MQKT", [512, NPAD], F32)
    MOT = k.dram("MOT", [256, NT], F32)
    MVT = k.dram("MVT", [256, NT], F32)
    MGT = k.dram("MGT", [16, NT], F32)
    HFB = [k.dram("HF", [NT, 256], F32), k.dram("HB", [NT, 256], F32)]
    zero_sb = k.sbuf("zero_sb", [128, 8], F32)
    k.op("dve", lambda e: e.memset(zero_sb[:], 0.0), writes=[zero_sb])
    for (dst, rows) in (() if 'z' in DBG_SKIP else ((RZT, 960), (MQKT, 512))):
        for r0 in range(0, rows, 128):
            nr = min(128, rows - r0)
            for c0, w in ((0, 1), (257, 2), (4355, 1)):
                k.dma("sp", dst[r0:r0 + nr, c0:c0 + w], zero_sb[0:nr, 0:w], reads=[zero_sb], writes=[dst], allow_slow_non_contiguous=True)
    sublnT = k.sbuf("sublnT", [128, L], F32)
    k.dma("sp", sublnT[:], sublnT_in[:], reads=[sublnT_in], writes=[sublnT])

    def phase_A(l):
        WI = k.sbuf("WI", [128, 8, IN_COLS], BF16)
        WIP = k.sbuf("WIP", [128, 8, 1024], BF16)
        pa_x = [k.sbuf("pa_x%d" % i, [128, 8, 512], F32) for i in range(2)]
        pa_sq = k.sbuf("pa_sq", [128, 8, 512], F32)
        pa_r = k.sbuf("pa_r", [128, 512], F32)
        pa_tmp = [k.sbuf("pa_tmp%d" % i, [128, 512], F32) for i in range(2)]
        pa_h = k.sbuf("pa_h", [128, 8, 512], BF16)
        pa_cos = k.sbuf("pa_cos", [128, 512], F32)
        pa_sin = k.sbuf("pa_sin", [128, 512], F32)
        pa_t1 = [k.sbuf("pa_t1_%d" % i, [128, 512], F32) for i in range(2)]
        pa_t2 = [k.sbuf("pa_t2_%d" % i, [128, 512], F32) for i in range(2)]
        pa_ob = [k.sbuf("pa_ob%d" % i, [128, 512], BF16) for i in range(3)]
        pa_of = [k.sbuf("pa_of%d" % i, [128, 512], F32) for i in range(3)]
        pa_va = [k.sbuf("pa_va%d" % i, [128, 4, 130], BF16) for i in range(2)]
        for b_ in ([] if 'm' in DBG_SKIP else pa_va):
            k.op("pool", lambda e, b_=b_: e.memset(b_[:], 1.0), writes=[b_])

        evac_rr = [0]

        def evac_copy(dst_ap, src_ap, reads, writes):
            evac_rr[0] += 1
            if evac_rr[0] % 2 == 0:
                k.op("act", lambda e: e.copy(out=dst_ap, in_=src_ap), reads=reads, writes=writes)
            else:
                k.op("dve", lambda e: e.tensor_copy(out=dst_ap, in_=src_ap), reads=reads, writes=writes)

        for kc in range(0 if 'w' in DBG_SKIP else 8):
            for c0 in range(0, IN_COLS, 1768):
                k.dma("pool", WI[:, kc, c0:c0 + 1768], w_in[l][kc * 128:(kc + 1) * 128, c0:c0 + 1768], reads=[w_in], writes=[WI])
            k.dma("pool", WIP[:, kc, :], w_inp[l][kc * 128:(kc + 1) * 128, :], reads=[w_inp], writes=[WIP])
        if DBG_STOP <= 1:
            return
        for (t0, n) in BLOCKS:
            s = 1 if t0 == 0 else 0
            xb = k.nxt("pa_x", pa_x)
            k.dma("sp", xb[:, :, 0:n], XTv[:, :, t0:t0 + n], reads=[XT], writes=[xb])
            k.dma("sp", pa_cos[:, 0:n], cos_in[:, t0:t0 + n], reads=[cos_in], writes=[pa_cos])
            k.dma("sp", pa_sin[:, 0:n], sin_in[:, t0:t0 + n], reads=[sin_in], writes=[pa_sin])
            k.op("act", lambda e: e.activation(out=pa_sq[:, :, 0:n], in_=xb[:, :, 0:n], func=AF.Square), reads=[xb], writes=[pa_sq])
            p = ps()
            for kc in range(8):
                k.op("pe", lambda e, kc=kc: e.matmul(p[:, 0:n], ones_f[:], pa_sq[:, kc, 0:n], start=(kc == 0), stop=(kc == 7)),
                     reads=[ones_f, pa_sq], writes=[p])
            k.op("act", lambda e: e.activation(out=pa_r[:, 0:n], in_=p[:, 0:n], func=AF.Sqrt, scale=1.0 / D, bias=EPS), reads=[p], writes=[pa_r])
            k.op("dve", lambda e: e.reciprocal(out=pa_r[:, 0:n], in_=pa_r[:, 0:n]), reads=[pa_r], writes=[pa_r])
            for kc in range(8):
                tmp = k.nxt("pa_tmp", pa_tmp)
                k.op("dve", lambda e, kc=kc: e.tensor_tensor(out=tmp[:, 0:n], in0=xb[:, kc, 0:n], in1=pa_r[:, 0:n], op=ALU.mult),
                     reads=[xb, pa_r], writes=[tmp])
                k.op("act", lambda e, kc=kc: e.activation(out=pa_h[:, kc, 0:n], in_=tmp[:, 0:n], func=AF.Identity,
                                                          scale=A1[l][:, kc, s:s + 1], bias=modT[l][:, kc, s:s + 1]),
                     reads=[tmp, A1[l], modT[l]], writes=[pa_h])

            if DBG_STOP <= 2:
                continue

            def fm(Wb, c0, ncols=128):
                p = ps()
                for kc in range(8):
                    k.op("pe", lambda e, kc=kc: e.matmul(p[0:ncols, 0:n], Wb[:, kc, c0:c0 + ncols], pa_h[:, kc, 0:n], start=(kc == 0), stop=(kc == 7)),
                         reads=[Wb, pa_h], writes=[p])
                return p

            for which, dst in ((0, QT), (1, KT)):
                for h in range(4):
                    c0 = which * 512 + h * 128
                    p1 = fm(WI, c0)
                    p2 = fm(WIP, c0)
                    t1 = k.nxt("pa_t1", pa_t1)
                    t2 = k.nxt("pa_t2", pa_t2)
                    ob = k.nxt("pa_ob", pa_ob)
                    k.op("dve", lambda e: e.tensor_tensor(out=t1[:, 0:n], in0=p1[:, 0:n], in1=pa_cos[:, 0:n], op=ALU.mult), reads=[p1, pa_cos], writes=[t1])
                    k.op("dve", lambda e: e.tensor_tensor(out=t2[:, 0:n], in0=p2[:, 0:n], in1=pa_sin[:, 0:n], op=ALU.mult), reads=[p2, pa_sin], writes=[t2])
                    k.op("pool", lambda e: e.tensor_tensor(out=ob[:, 0:n], in0=t1[:, 0:n], in1=t2[:, 0:n], op=ALU.add), reads=[t1, t2], writes=[ob])
                    k.dma("sp", dst[h][:, t0:t0 + n], ob[:, 0:n], reads=[ob], writes=[dst])
            if DBG_STOP <= 3:
                continue
            pc = padcol(t0)
            for j in range(8):
                ncols = 128 if j < 7 else 64
                p1 = fm(WI, RW0 + j * 128, ncols)
                of = k.nxt("pa_of", pa_of)
                evac_copy(of[0:ncols, 0:n], p1[0:ncols, 0:n], [p1], [of])
                k.dma("sp", RZT[j * 128:j * 128 + ncols, pc:pc + n], of[0:ncols, 0:n], reads=[of], writes=[RZT])
            for j in range(4):
                p1 = fm(WI, ML0 + j * 128)
                of = k.nxt("pa_of", pa_of)
                evac_copy(of[:, 0:n], p1[:, 0:n], [p1], [of])
                k.dma("sp", MQKT[j * 128:(j + 1) * 128, pc:pc + n], of[:, 0:n], reads=[of], writes=[MQKT])
            for j in range(2):
                p1 = fm(WI, ML0 + 768 + j * 128)
                of = k.nxt("pa_of", pa_of)
                k.op("act", lambda e: e.activation(out=of[:, 0:n], in_=p1[:, 0:n], func=AF.Sigmoid), reads=[p1], writes=[of])
                k.dma("sp", MOT[j * 128:(j + 1) * 128, t0:t0 + n], of[:, 0:n], reads=[of], writes=[MOT])
            if DBG_STOP <= 4:
                continue
            for j in range(n // 128):
                r0 = t0 + j * 128
                p1 = ps()
                for kc in range(8):
                    k.op("pe", lambda e, kc=kc: e.matmul(p1[:, 0:512], pa_h[:, kc, j * 128:(j + 1) * 128], WI[:, kc, 1024:1536], start=(kc == 0), stop=(kc == 7)),
                         reads=[WI, pa_h], writes=[p1])
                va = k.nxt("pa_va", pa_va)
                evac_copy(va[:, :, 0:128], p1[:, 0:512].rearrange("p (h e) -> p h e", e=128), [p1], [va])
                k.dma("sp", VA[r0:r0 + 128, :], va[:].rearrange("p h e -> p (h e)"), reads=[va], writes=[VA])
            for j in range(2):
                p1 = fm(WI, ML0 + 512 + j * 128)
                of = k.nxt("pa_of", pa_of)
                evac_copy(of[:, 0:n], p1[:, 0:n], [p1], [of])
                k.dma("sp", MVT[j * 128:(j + 1) * 128, t0:t0 + n], of[:, 0:n], reads=[of], writes=[MVT])
            p1 = fm(WI, ML0 + 1024, 16)
            of = k.nxt("pa_of", pa_of)
            evac_copy(of[0:16, 0:n], p1[0:16, 0:n], [p1], [of])
            k.dma("sp", MGT[:, t0:t0 + n], of[0:16, 0:n], reads=[of], writes=[MGT])

    def phase_B(l):
        at_k = k.sbuf("at_k", [128, NT], BF16)
        at_q = k.sbuf("at_q", [128, NT], BF16)
        at_v = k.sbuf("at_v", [128, NTILE, 130], BF16)
        at_p = [k.sbuf("at_p%d" % i, [128, 512], BF16) for i in range(3)]
        at_o = [k.sbuf("at_o%d" % i, [128, 4, 128], F32) for i in range(2)]
        at_a = k.sbuf("at_a", [128, 4, 128], F32)
        at_sq = k.sbuf("at_sq", [128, 4, 128], F32)
        at_ss = k.sbuf("at_ss", [128, 4], F32)
        at_rec = k.sbuf("at_rec", [128, 4], F32)
        at_ob = [k.sbuf("at_ob%d" % i, [128, 512], BF16) for i in range(2)]
        dl = k.sbuf("dl", [128, 256], F32)
        dl_t = k.sbuf("dl_t", [128, 2, 64], F32)
        dl_s = k.sbuf("dl_s", [128, 2], F32)
        neglam = k.sbuf("neglam", [128, 1], F32)
        subg = k.sbuf("subg", [128, 1], F32)
        ACC = [k.psum("acc%d" % i, [128, 512], F32) for i in range(0)]

        lam_init = 0.8 - 0.6 * math.exp(-0.3 * l)
        k.dma("sp", dl[:], da_lam_in[l].partition_broadcast(128), reads=[da_lam_in], writes=[dl])
        dl4 = dl[:, :].rearrange("p (a b d) -> p a b d", a=2, b=2)
        k.op("dve", lambda e: e.tensor_tensor(out=dl_t[:], in0=dl4[:, :, 0, :], in1=dl4[:, :, 1, :], op=ALU.mult), reads=[dl], writes=[dl_t])
        k.op("dve", lambda e: e.tensor_reduce(out=dl_s[:], in_=dl_t[:], axis=AX.X, op=ALU.add), reads=[dl_t], writes=[dl_s])
        k.op("act", lambda e: e.activation(out=dl_s[:], in_=dl_s[:], func=AF.Exp), reads=[dl_s], writes=[dl_s])
        k.op("dve", lambda e: e.scalar_tensor_tensor(out=neglam[:], in0=dl_s[:, 1:2], scalar=-lam_init, in1=dl_s[:, 0:1], op0=ALU.add, op1=ALU.subtract),
             reads=[dl_s], writes=[neglam])
        k.op("dve", lambda e: e.tensor_scalar(out=subg[:], in0=sublnT[:, l:l + 1], scalar1=(1.0 - lam_init), scalar2=None, op0=ALU.mult),
             reads=[sublnT], writes=[subg])
        qsets = [(256 + 512 * i, 512, list(range(NTILE))) for i in range(8)]
        if l < DEPTH - 1:
            qsets = [(0, 256, [0, 1])] + qsets
        for h in range(4):
            k.dma("sp", at_k[:], KT[h], reads=[KT], writes=[at_k])
            k.dma("pool", at_q[:], QT[h], reads=[QT], writes=[at_q])
            k.dma("sp", at_v[:], VA.t.ap().rearrange("(t p) (h e) -> p t h e", p=128, e=130)[:, :, h, :], reads=[VA], writes=[at_v])
            for (q0, nq, kts) in qsets:
                nj = nq // 128
                for m in range(2):
                    accs = PS[0:4]
                    osb = at_o[m]
                    for ki, kt in enumerate(kts):
                        sp_ = k.nxt("psB", PS[4:8])
                        k.op("pe", lambda e: e.matmul(sp_[:, 0:nq], at_k[m * 64:(m + 1) * 64, kt * 128:(kt + 1) * 128], at_q[m * 64:(m + 1) * 64, q0:q0 + nq],
                                                      start=True, stop=True), reads=[at_k, at_q], writes=[sp_])
                        pt = k.nxt("at_p", at_p)
                        k.op("act", lambda e: e.activation(out=pt[:, 0:nq], in_=sp_[:, 0:nq], func=AF.Exp, scale=0.125), reads=[sp_], writes=[pt])
                        for j in range(nj):
                            acc = accs[j]
                            k.op("pe", lambda e, j=j: e.matmul(acc[:, 0:129], pt[:, j * 128:(j + 1) * 128], at_v[:, kt, 0:129],
                                                               start=(ki == 0), stop=(ki == len(kts) - 1)), reads=[pt, at_v], writes=[acc])
                    for j in range(nj):
                        acc = accs[j]
                        c0 = 0
                        k.op("dve", lambda e, j=j: e.reciprocal(out=at_rec[:, j:j + 1], in_=acc[:, c0 + 128:c0 + 129]), reads=[acc], writes=[at_rec])
                        k.op("dve", lambda e, j=j: e.tensor_scalar(out=osb[:, j, :], in0=acc[:, c0:c0 + 128], scalar1=at_rec[:, j:j + 1], scalar2=None, op0=ALU.mult),
                             reads=[acc, at_rec], writes=[osb])
                k.op("dve", lambda e: e.scalar_tensor_tensor(out=at_a[:, 0:nj, :], in0=at_o[1][:, 0:nj, :], scalar=neglam[:, 0:1], in1=at_o[0][:, 0:nj, :],
                                                             op0=ALU.mult, op1=ALU.add), reads=[at_o[0], at_o[1], neglam], writes=[at_a])
                k.op("pool", lambda e: e.tensor_tensor(out=at_sq[:, 0:nj, :], in0=at_a[:, 0:nj, :], in1=at_a[:, 0:nj, :], op=ALU.mult), reads=[at_a], writes=[at_sq])
                k.op("dve", lambda e: e.tensor_reduce(out=at_ss[:, 0:nj], in_=at_sq[:, 0:nj, :], axis=AX.X, op=ALU.add), reads=[at_sq], writes=[at_ss])
                k.op("act", lambda e: e.activation(out=at_ss[:, 0:nj], in_=at_ss[:, 0:nj], func=AF.Sqrt, scale=1.0 / 128, bias=EPS), reads=[at_ss], writes=[at_ss])
                k.op("dve", lambda e: e.reciprocal(out=at_ss[:, 0:nj], in_=at_ss[:, 0:nj]), reads=[at_ss], writes=[at_ss])
                k.op("dve", lambda e: e.tensor_tensor(out=at_a[:, 0:nj, :], in0=at_a[:, 0:nj, :], in1=at_ss[:, 0:nj].unsqueeze(2).to_broadcast([128, nj, 128]), op=ALU.mult),
                     reads=[at_a, at_ss], writes=[at_a])
                pt_ = k.nxt("psB", PS[4:8])
                for j in range(nj):
                    k.op("pe", lambda e, j=j: e.transpose(pt_[:, j * 128:(j + 1) * 128], at_a[:, j, :], ident[:]), reads=[at_a, ident], writes=[pt_])
                ob = k.nxt("at_ob", at_ob)
                k.op("act", lambda e: e.activation(out=ob[:, 0:nq], in_=pt_[:, 0:nq], func=AF.Identity, scale=subg[:, 0:1]), reads=[pt_, subg], writes=[ob])
                k.dma("sp", MIXT[h * 128:(h + 1) * 128, q0:q0 + nq], ob[:, 0:nq], reads=[ob], writes=[MIXT])

    mcw_in = ein("mcwT", [L, 128, 4, 3])
    mcb_in = ein("mcbT", [L, 128, 4])
    mgb_in = ein("mgbT", [L, 16, 1])
    mng_in = ein("mngT", [L, 128, 2])
    triu_in = ein("triu", [128, 128])
    tril_in = ein("tril", [128, 128])
    triu = k.sbuf("triu", [128, 128], F32)
    tril = k.sbuf("tril", [128, 128], F32)
    k.dma("sp", triu[:], triu_in[:], reads=[triu_in], writes=[triu])
    k.dma("sp", tril[:], tril_in[:], reads=[tril_in], writes=[tril])
    ORDER = [list(range(NTILE)), [1, 0] + list(range(NTILE - 1, 1, -1))]

    def phase_D(l):
        mcw = k.sbuf("mcw", [128, 4, 3], F32)
        mcb = k.sbuf("mcb", [128, 4], F32)
        mgb = k.sbuf("mgb", [16, 1], F32)
        mng = k.sbuf("mng", [128, 2], F32)
        k.dma("sp", mcw[:], mcw_in[l], reads=[mcw_in], writes=[mcw])
        k.dma("sp", mcb[:], mcb_in[l], reads=[mcb_in], writes=[mcb])
        k.dma("sp", mgb[:], mgb_in[l], reads=[mgb_in], writes=[mgb])
        k.dma("sp", mng[:], mng_in[l], reads=[mng_in], writes=[mng])
        mq = k.sbuf("mq_all", [128, 4, NT], BF16)
        zc = [k.sbuf("md_z%d" % i, [128, 514], F32) for i in range(2)]
        ac = [k.sbuf("md_a%d" % i, [128, 512], F32) for i in range(2)]
        for c in range(4):
            for (t0, n) in BLOCKS:
                z = k.nxt("md_z", zc)
                a = k.nxt("md_a", ac)
                pc = padcol(t0)
                k.dma("sp", z[:, 0:n + 2], MQKT[c * 128:(c + 1) * 128, pc - 1:pc + n + 1], reads=[MQKT], writes=[z])
                k.op("dve", lambda e: e.tensor_scalar(out=a[:, 0:n], in0=z[:, 0:n], scalar1=mcw[:, c, 0:1], scalar2=None, op0=ALU.mult), reads=[z, mcw], writes=[a])
                k.op("dve", lambda e: e.scalar_tensor_tensor(out=a[:, 0:n], in0=z[:, 1:n + 1], scalar=mcw[:, c, 1:2], in1=a[:, 0:n], op0=ALU.mult, op1=ALU.add), reads=[z, mcw, a], writes=[a])
                k.op("dve", lambda e: e.scalar_tensor_tensor(out=a[:, 0:n], in0=z[:, 2:n + 2], scalar=mcw[:, c, 2:3], in1=a[:, 0:n], op0=ALU.mult, op1=ALU.add), reads=[z, mcw, a], writes=[a])
                if c < 2:
                    k.op("act", lambda e: e.activation(out=mq[:, c, t0:t0 + n], in_=a[:, 0:n], func=AF.Silu, bias=mcb[:, c:c + 1], scale=1.0), reads=[a, mcb], writes=[mq])
                else:
                    k.op("act", lambda e: e.activation(out=a[:, 0:n], in_=a[:, 0:n], func=AF.Silu, bias=mcb[:, c:c + 1], scale=1.0), reads=[a, mcb], writes=[a])
                    k.op("dve", lambda e: e.tensor_scalar(out=mq[:, c, t0:t0 + n], in0=a[:, 0:n], scalar1=0.125, scalar2=None, op0=ALU.mult), reads=[a], writes=[mq])
        if DSTOP <= 1:
            return
        GI = k.sbuf("md_gi", [16, NT], F32)
        GL = k.sbuf("md_gl", [16, NT], F32)
        k.dma("sp", GI[:], MGT[:], reads=[MGT], writes=[GI])
        k.op("dve", lambda e: e.tensor_scalar(out=GI[:], in0=GI[:], scalar1=mgb[:, 0:1], scalar2=None, op0=ALU.add), reads=[GI, mgb], writes=[GI])
        k.op("act", lambda e: e.activation(out=GL[:], in_=GI[:], func=AF.Exp, scale=-1.0), reads=[GI], writes=[GL])
        k.op("act", lambda e: e.activation(out=GL[:], in_=GL[:], func=AF.Ln, bias=1.0, scale=1.0), reads=[GL], writes=[GL])
        k.op("dve", lambda e: e.tensor_scalar(out=GL[:], in0=GL[:], scalar1=-1.0, scalar2=None, op0=ALU.mult), reads=[GL], writes=[GL])
        ES = k.sbuf("md_es", [128, NTILE, 8], F32)
        EB = k.sbuf("md_eb", [128, NTILE, 8], F32)
        EBL = k.sbuf("md_ebl", [128, NTILE, 8], F32)
        VAUG = k.sbuf("md_vaug", [128, NTILE, 4, 66], BF16)
        k.op("pool", lambda e: e.memset(VAUG[:], 1.0), writes=[VAUG])
        gtok = [k.sbuf("md_gtok%d" % i, [128, 32], F32) for i in range(2)]
        cs = [k.sbuf("md_cs%d" % i, [128, 16], F32) for i in range(2)]
        vt = [k.sbuf("md_vt%d" % i, [128, 2, 128], F32) for i in range(2)]
        for t in range(NTILE):
            tsl = slice(t * 128, (t + 1) * 128)
            p = ps()
            k.op("pe", lambda e: e.transpose(p[:, 0:16], GI[0:16, tsl], ident[0:16, 0:16]), reads=[GI, ident], writes=[p])
            k.op("pe", lambda e: e.transpose(p[:, 16:32], GL[0:16, tsl], ident[0:16, 0:16]), reads=[GL, ident], writes=[p])
            g = k.nxt("md_gtok", gtok)
            k.op("act", lambda e: e.copy(out=g[:], in_=p[:, 0:32]), reads=[p], writes=[g])
            p2 = ps()
            k.op("pe", lambda e: e.matmul(p2[:, 0:4], triu[:], g[:, 20:24], start=True, stop=True), reads=[triu, g], writes=[p2])
            k.op("pe", lambda e: e.matmul(p2[:, 4:8], tril[:], g[:, 28:32], start=True, stop=True), reads=[tril, g], writes=[p2])
            k.op("pe", lambda e: e.matmul(p2[:, 8:12], ones_f[:], g[:, 20:24], start=True, stop=True), reads=[ones_f, g], writes=[p2])
            k.op("pe", lambda e: e.matmul(p2[:, 12:16], ones_f[:], g[:, 28:32], start=True, stop=True), reads=[ones_f, g], writes=[p2])
            c_ = k.nxt("md_cs", cs)
            k.op("dve", lambda e: e.tensor_copy(out=c_[:], in_=p2[:, 0:16]), reads=[p2], writes=[c_])
            k.op("dve", lambda e: e.tensor_tensor(out=ES[:, t, 0:4], in0=g[:, 0:4], in1=c_[:, 0:4], op=ALU.subtract), reads=[g, c_], writes=[ES])
            k.op("dve", lambda e: e.tensor_tensor(out=ES[:, t, 4:8], in0=g[:, 8:12], in1=c_[:, 4:8], op=ALU.subtract), reads=[g, c_], writes=[ES])
            k.op("act", lambda e: e.activation(out=ES[:, t, :], in_=ES[:, t, :], func=AF.Exp), reads=[ES], writes=[ES])
            k.op("act", lambda e: e.activation(out=EB[:, t, :], in_=c_[:, 0:8], func=AF.Exp), reads=[c_], writes=[EB])
            k.op("act", lambda e: e.activation(out=EBL[:, t, :], in_=c_[:, 8:16], func=AF.Exp), reads=[c_], writes=[EBL])
            v_ = k.nxt("md_vt", vt)
            k.dma("sp", v_[:], MVT.t.ap().rearrange("(c p) t -> p c t", p=128)[:, :, tsl], reads=[MVT], writes=[v_])
            p3 = ps()
            for c in range(2):
                k.op("pe", lambda e, c=c: e.transpose(p3[:, c * 128:(c + 1) * 128], v_[:, c, :], ident[:]), reads=[v_, ident], writes=[p3])
            k.op("dve", lambda e: e.tensor_copy(out=VAUG[:, t, :, 0:64], in_=p3[:, 0:256].rearrange("p (h d) -> p h d", d=64)), reads=[p3], writes=[VAUG])
        if DSTOP <= 2:
            return
        CT = [k.sbuf("md_ct%d" % d, [128, 2, 66], F32) for d in range(2)]
        CTb = [k.sbuf("md_ctb%d" % d, [128, 2, 66], BF16) for d in range(2)]
        keP = [k.sbuf("md_kep%d" % d, [128, 4, 128], BF16) for d in range(2)]
        for d in range(2):
            k.op("pool", lambda e, d=d: e.memset(CT[d][:], 0.0), writes=[CT[d]])
            k.op("pool", lambda e, d=d: e.memset(CTb[d][:], 0.0), writes=[CTb[d]])
            k.op("pool", lambda e, d=d: e.memset(keP[d][:], 0.0), writes=[keP[d]])
        Sp = [k.sbuf("md_sp%d" % i, [128, 4, 128], BF16) for i in range(2)]
        dn = [k.sbuf("md_dn%d" % i, [128, 4], F32) for i in range(2)]
        hv = [k.sbuf("md_hv%d" % i, [128, 4, 64], F32) for i in range(2)]
        dn2 = [k.sbuf("md_dn2%d" % i, [128, 4], F32) for i in range(2)]
        tot = [k.sbuf("md_tot%d" % i, [128, 4, 66], F32) for i in range(2)]
        Ft = [k.sbuf("md_F%d" % i, [128, 2], F32) for i in range(2)]
        ctmp = [k.sbuf("md_ctmp%d" % i, [128, 2, 66], F32) for i in range(2)]
        masks = [triu, tril]
        for i in range(NTILE):
            for d in range(2):
                t = ORDER[d][i]
                tsl = slice(t * 128, (t + 1) * 128)
                pk = ps()
                for c in range(2):
                    k.op("pe", lambda e, c=c: e.matmul(pk[:, c * 128:(c + 1) * 128], mq[:, 2 + c, tsl], identb[:], start=True, stop=True), reads=[mq, identb], writes=[pk])
                for h in range(4):
                    hb = (h % 2) * 64
                    k.op("dve", lambda e, h=h, hb=hb: e.tensor_scalar(out=keP[d][:, h, hb:hb + 64], in0=pk[:, h * 64:(h + 1) * 64], scalar1=ES[:, t, d * 4 + h:d * 4 + h + 1], scalar2=None, op0=ALU.mult),
                         reads=[pk, ES], writes=[keP[d]])
                pS2 = [ps(), ps()]
                for h in range(4):
                    hb = (h % 2) * 64
                    k.op("pe", lambda e, h=h, hb=hb: e.matmul(pS2[h % 2][:, (h // 2) * 128:(h // 2 + 1) * 128], mq[hb:hb + 64, 2 + h // 2, tsl], mq[hb:hb + 64, h // 2, tsl], start=True, stop=True),
                         reads=[mq], writes=[pS2[h % 2]])
                S_ = k.nxt("md_sp", Sp)
                for h in range(4):
                    k.op("dve", lambda e, h=h: e.scalar_tensor_tensor(out=S_[:, h, :], in0=pS2[h % 2][:, (h // 2) * 128:(h // 2 + 1) * 128], scalar=ES[:, t, d * 4 + h:d * 4 + h + 1], in1=masks[d][:],
                                                                      op0=ALU.mult, op1=ALU.mult), reads=[pS2[h % 2], ES, masks[d]], writes=[S_])
                pN = ps()
                pR2 = [ps(), ps()]
                for h in range(4):
                    hb = (h % 2) * 64
                    k.op("pe", lambda e, h=h: e.matmul(pN[:, h * 66:h * 66 + 65], S_[:, h, :], VAUG[:, t, h, 0:65], start=True, stop=True), reads=[S_, VAUG], writes=[pN])
                    k.op("pe", lambda e, h=h, hb=hb: e.matmul(pR2[h % 2][:, (h // 2) * 66:(h // 2) * 66 + 65], mq[hb:hb + 64, h // 2, tsl], CTb[d][hb:hb + 64, h // 2, 0:65], start=True, stop=True),
                         reads=[mq, CTb[d]], writes=[pR2[h % 2]])
                tot_ = k.nxt("md_tot", tot)
                totv = tot_[:].rearrange("p (a b) e -> p a b e", b=2)
                for par in range(2):
                    k.op("act", lambda e, par=par: e.copy(out=totv[:, :, par, 0:65], in_=pR2[par][:, 0:132].rearrange("p (a e) -> p a e", e=66)[:, :, 0:65]), reads=[pR2[par]], writes=[tot_])
                k.op("dve", lambda e: e.tensor_tensor(out=tot_[:, :, 0:65], in0=pN[:, 0:264].rearrange("p (h e) -> p h e", e=66)[:, :, 0:65], in1=tot_[:, :, 0:65], op=ALU.add), reads=[pN, tot_], writes=[tot_])
                pNv = tot_
                dn_ = k.nxt("md_dn", dn)
                hv_ = k.nxt("md_hv", hv)
                k.op("dve", lambda e: e.tensor_tensor(out=dn_[:], in0=pNv[:, :, 64], in1=EB[:, t, d * 4:(d + 1) * 4], op=ALU.mult), reads=[tot_, EB], writes=[dn_])
                dn2_ = k.nxt("md_dn2", dn2)
                k.op("dve", lambda e: e.tensor_scalar(out=dn2_[:], in0=dn_[:], scalar1=-1.0, scalar2=None, op0=ALU.mult), reads=[dn_], writes=[dn2_])
                k.op("dve", lambda e: e.tensor_tensor(out=dn_[:], in0=dn_[:], in1=dn2_[:], op=ALU.max), reads=[dn_, dn2_], writes=[dn_])
                k.op("dve", lambda e: e.tensor_scalar(out=dn_[:], in0=dn_[:], scalar1=1.0, scalar2=None, op0=ALU.max), reads=[dn_], writes=[dn_])
                k.op("dve", lambda e: e.reciprocal(out=dn_[:], in_=dn_[:]), reads=[dn_], writes=[dn_])
                k.op("dve", lambda e: e.tensor_tensor(out=dn_[:], in0=dn_[:], in1=EB[:, t, d * 4:(d + 1) * 4], op=ALU.mult), reads=[dn_, EB], writes=[dn_])
                k.op("dve", lambda e: e.tensor_tensor(out=hv_[:], in0=pNv[:, :, 0:64], in1=dn_[:].unsqueeze(2).to_broadcast([128, 4, 64]), op=ALU.mult), reads=[tot_, dn_], writes=[hv_])
                k.dma("sp", HFB[d][tsl, :], hv_[:].rearrange("p h e -> p (h e)"), reads=[hv_], writes=[HFB[d]])
                pC = ps()
                for pp in range(2):
                    k.op("pe", lambda e, pp=pp: e.matmul(pC[:, pp * 66:pp * 66 + 65], keP[d][:, 2 * pp, :], VAUG[:, t, 2 * pp, 0:65], start=True, stop=False), reads=[keP[d], VAUG], writes=[pC])
                    k.op("pe", lambda e, pp=pp: e.matmul(pC[:, pp * 66:pp * 66 + 65], keP[d][:, 2 * pp + 1, :], VAUG[:, t, 2 * pp + 1, 0:65], start=False, stop=True), reads=[keP[d], VAUG], writes=[pC])
                F_ = k.nxt("md_F", Ft)
                ebl2 = EBL[:, t, d * 4:(d + 1) * 4].rearrange("p (a b) -> p a b", b=2)
                k.op("dve", lambda e: e.tensor_copy(out=F_[0:64, :], in_=ebl2[0:64, :, 0]), reads=[EBL], writes=[F_])
                k.op("dve", lambda e: e.tensor_copy(out=F_[64:128, :], in_=ebl2[64:128, :, 1]), reads=[EBL], writes=[F_])
                ct_ = k.nxt("md_ctmp", ctmp)
                k.op("dve", lambda e: e.tensor_tensor(out=ct_[:, :, 0:65], in0=pC[:, 0:132].rearrange("p (a e) -> p a e", e=66)[:, :, 0:65], in1=CT[d][:, :, 0:65], op=ALU.add), reads=[pC, CT[d]], writes=[ct_])
                k.op("dve", lambda e: e.tensor_tensor(out=CT[d][:, :, 0:65], in0=ct_[:, :, 0:65], in1=F_[:].unsqueeze(2).to_broadcast([128, 2, 65]), op=ALU.mult), reads=[ct_, F_], writes=[CT[d]])
                k.op("act", lambda e: e.copy(out=CTb[d][:, :, 0:65], in_=CT[d][:, :, 0:65]), reads=[CT[d]], writes=[CTb[d]])
        if DSTOP <= 3:
            return
        hf = [k.sbuf("md_hf%d" % i, [128, 4, 64], F32) for i in range(2)]
        hb_ = [k.sbuf("md_hb%d" % i, [128, 4, 64], F32) for i in range(2)]
        st4 = [k.sbuf("md_st%d" % i, [128, 4], F32) for i in range(2)]
        sq4 = [k.sbuf("md_sq%d" % i, [128, 4, 64], F32) for i in range(2)]
        mo = [k.sbuf("md_mo%d" % i, [128, 2, 128], F32) for i in range(2)]
        ob = [k.sbuf("md_ob%d" % i, [128, 2, 128], BF16) for i in range(2)]
        MOTv = MOT.t.ap().rearrange("(c p) t -> p c t", p=128)
        MIXv = MIXT.t.ap()[768:1024, :].rearrange("(c p) t -> p c t", p=128)
        for t in range(NTILE):
            tsl = slice(t * 128, (t + 1) * 128)
            a = k.nxt("md_hf", hf)
            b = k.nxt("md_hb", hb_)
            s4 = k.nxt("md_st", st4)
            q4 = k.nxt("md_sq", sq4)
            k.dma("sp", a[:].rearrange("p h e -> p (h e)"), HFB[0][tsl, :], reads=[HFB[0]], writes=[a])
            k.dma("sp", b[:].rearrange("p h e -> p (h e)"), HFB[1][tsl, :], reads=[HFB[1]], writes=[b])
            k.op("dve", lambda e: e.tensor_tensor(out=a[:], in0=a[:], in1=b[:], op=ALU.add), reads=[a, b], writes=[a])
            k.op("dve", lambda e: e.tensor_reduce(out=s4[:], in_=a[:], axis=AX.X, op=ALU.add), reads=[a], writes=[s4])
            k.op("dve", lambda e: e.tensor_scalar(out=s4[:], in0=s4[:], scalar1=-1.0 / 64, scalar2=None, op0=ALU.mult), reads=[s4], writes=[s4])
            k.op("dve", lambda e: e.tensor_tensor(out=a[:], in0=a[:], in1=s4[:].unsqueeze(2).to_broadcast([128, 4, 64]), op=ALU.add), reads=[a, s4], writes=[a])
            k.op("pool", lambda e: e.tensor_tensor(out=q4[:], in0=a[:], in1=a[:], op=ALU.mult), reads=[a], writes=[q4])
            k.op("dve", lambda e: e.tensor_reduce(out=s4[:], in_=q4[:], axis=AX.X, op=ALU.add), reads=[q4], writes=[s4])
            k.op("act", lambda e: e.activation(out=s4[:], in_=s4[:], func=AF.Sqrt, scale=1.0 / 64, bias=EPS), reads=[s4], writes=[s4])
            k.op("dve", lambda e: e.reciprocal(out=s4[:], in_=s4[:]), reads=[s4], writes=[s4])
            k.op("dve", lambda e: e.tensor_tensor(out=a[:], in0=a[:], in1=s4[:].unsqueeze(2).to_broadcast([128, 4, 64]), op=ALU.mult), reads=[a, s4], writes=[a])
            p = ps()
            av = a[:].rearrange("p h e -> p (h e)")
            for c in range(2):
                k.op("pe", lambda e, c=c: e.transpose(p[:, c * 128:(c + 1) * 128], av[:, c * 128:(c + 1) * 128], ident[:]), reads=[a, ident], writes=[p])
            m_ = k.nxt("md_mo", mo)
            o_ = k.nxt("md_ob", ob)
            k.dma("sp", m_[:], MOTv[:, :, tsl], reads=[MOT], writes=[m_])
            for c in range(2):
                k.op("dve", lambda e, c=c: e.scalar_tensor_tensor(out=o_[:, c, :], in0=p[:, c * 128:(c + 1) * 128], scalar=mng[:, c:c + 1], in1=m_[:, c, :], op0=ALU.mult, op1=ALU.mult),
                     reads=[p, mng, m_], writes=[o_])
            k.dma("sp", MIXv[:, :, tsl], o_[:], reads=[o_], writes=[MIXT])

    rp_in = ein("rw_par", [L, 64, 48])
    w2p_in = ein("rw_w2p", [L, 2, 64, 256])
    a2p_in = ein("rw_a2p", [L, 2, 64, 256])
    g2_in = ein("rw_g2", [L, 64, 256])
    lnp_in = ein("rw_lnp", [L, 128, 2, 2])
    rmask_in = ein("rw_mask", [2, 64, 5, 64])
    reset_in = ein("rw_reset", [64, 512])
    RS = k.dram("RS", [2, 4, 4, 64, NT], BF16)
    VS = k.dram("VS", [4, 64, NT], BF16)
    WLD = k.dram("WLD", [2, 4, 64, 68], F32)
    BON = k.dram("BON", [256, NT], F32)
    GGd = k.dram("GGd", [256, NT], F32)
    YS = [k.dram("YSf", [NT, 256], F32), k.dram("YSb", [NT, 256], F32)]
    CH_ORDER = [list(range(68)), [3, 2, 1, 0] + list(range(67, 3, -1))]

    def phase_C(l):
        RP = k.sbuf("rc_rp", [64, 48], F32)
        W2P = k.sbuf("rc_w2p", [64, 2, 256], F32)
        A2P = k.sbuf("rc_a2p", [64, 2, 256], F32)
        G2 = k.sbuf("rc_g2", [64, 256], F32)
        RESET = k.sbuf("rc_reset", [64, 512], F32)
        ONES = k.sbuf("rc_ones", [64, 512], F32)
        omka = k.sbuf("rc_omka", [64, 4], F32)
        k.dma("sp", RP[:], rp_in[l], reads=[rp_in], writes=[RP])
        for d in range(2):
            k.dma("sp", W2P[:, d, :], w2p_in[l][d], reads=[w2p_in], writes=[W2P])
            k.dma("sp", A2P[:, d, :], a2p_in[l][d], reads=[a2p_in], writes=[A2P])
        k.dma("sp", G2[:], g2_in[l], reads=[g2_in], writes=[G2])
        k.dma("sp", RESET[:], reset_in[:], reads=[reset_in], writes=[RESET])
        k.op("pool", lambda e: e.memset(ONES[:], 1.0), writes=[ONES])
        k.op("dve", lambda e: e.tensor_scalar(out=omka[:], in0=RP[:, 35:39], scalar1=-1.0, scalar2=1.0, op0=ALU.mult, op1=ALU.add), reads=[RP], writes=[omka])

        def mk(name, shape, dt, nb=2):
            return [k.sbuf("rc_%s%d" % (name, i), shape, dt) for i in range(nb)]
        zin = mk("zin", [64, 514], F32, 3)
        nm = mk("nm", [64, 512], F32, 2)
        wds = k.sbuf("rc_wds", [64, 512], F32)
        ads = k.sbuf("rc_ads", [64, 512], F32)
        gds = k.sbuf("rc_gds", [64, 512], F32)
        rs_ = k.sbuf("rc_rs", [64, 512], F32)
        ks_ = k.sbuf("rc_ks", [64, 512], F32)
        vs_ = k.sbuf("rc_vs", [64, 512], F32)
        kk_ = k.sbuf("rc_kk", [64, 512], F32)
        t_a = mk("ta", [64, 512], F32, 2)
        t_b = mk("tb", [64, 512], F32, 2)
        t_c = mk("tc", [64, 512], F32, 2)
        lw_ = mk("lw", [64, 512], F32, 2)
        LW = mk("LW", [64, 512], F32, 2)
        km = [k.sbuf("rc_km%d" % d, [64, 512], F32) for d in range(2)]
        aa = mk("aa", [64, 512], F32, 2)
        ob4 = mk("ob4", [64, 4, 512], BF16, 2)
        vb = mk("vb", [64, 512], BF16, 2)
        wl_sb = k.sbuf("rc_wl", [64, 8, 68], F32)

        def shift(dst, row0, mucol, pc, n):
            z = k.nxt("rc_zin", zin)
            m_ = k.nxt("rc_nm", nm)
            k.dma("sp", z[:, 0:n + 2], RZT[row0:row0 + 64, pc - 1:pc + n + 1], reads=[RZT], writes=[z])
            k.op("pool", lambda e: e.tensor_tensor(out=m_[:, 0:n], in0=z[:, 0:n], in1=z[:, 2:n + 2], op=ALU.add), reads=[z], writes=[m_])
            k.op("dve", lambda e: e.scalar_tensor_tensor(out=m_[:, 0:n], in0=m_[:, 0:n], scalar=0.5, in1=z[:, 1:n + 1], op0=ALU.mult, op1=ALU.subtract), reads=[m_, z], writes=[m_])
            k.op("dve", lambda e: e.scalar_tensor_tensor(out=dst[:, 0:n], in0=m_[:, 0:n], scalar=RP[:, mucol:mucol + 1], in1=z[:, 1:n + 1], op0=ALU.mult, op1=ALU.add), reads=[m_, z, RP], writes=[dst])

        for (t0, n) in BLOCKS:
            pc = padcol(t0)
            nch = n // 64
            c0 = t0 // 64
            shift(wds, 768, 12, pc, n)
            shift(ads, 832, 13, pc, n)
            shift(gds, 896, 14, pc, n)
            k.op("act", lambda e: e.activation(out=wds[:, 0:n], in_=wds[:, 0:n], func=AF.Tanh), reads=[wds], writes=[wds])
            k.op("act", lambda e: e.activation(out=gds[:, 0:n], in_=gds[:, 0:n], func=AF.Sigmoid), reads=[gds], writes=[gds])
            for h in range(4):
                hs = slice(h * 64, (h + 1) * 64)
                shift(rs_, h * 64, 0 + h, pc, n)
                shift(ks_, 256 + h * 64, 4 + h, pc, n)
                shift(vs_, 512 + h * 64, 8 + h, pc, n)
                p = ps()
                k.op("pe", lambda e: e.matmul(p[0:64, 0:n], G2[:, hs], gds[:, 0:n], start=True, stop=True), reads=[G2, gds], writes=[p])
                ta = k.nxt("rc_ta", t_a)
                k.op("act", lambda e: e.copy(out=ta[:, 0:n], in_=p[0:64, 0:n]), reads=[p], writes=[ta])
                k.dma("sp", GGd[hs, t0:t0 + n], ta[:, 0:n], reads=[ta], writes=[GGd])
                vb_ = k.nxt("rc_vb", vb)
                k.op("pool", lambda e: e.tensor_copy(out=vb_[:, 0:n], in_=vs_[:, 0:n]), reads=[vs_], writes=[vb_])
                k.dma("sp", VS[h][:, t0:t0 + n], vb_[:, 0:n], reads=[vb_], writes=[VS])
                tb = k.nxt("rc_tb", t_b)
                tc_ = k.nxt("rc_tc", t_c)
                k.op("dve", lambda e: e.tensor_scalar(out=tb[:, 0:n], in0=ks_[:, 0:n], scalar1=RP[:, 31 + h:32 + h], scalar2=None, op0=ALU.mult), reads=[ks_, RP], writes=[tb])
                k.op("pool", lambda e: e.tensor_tensor(out=tc_[:, 0:n], in0=tb[:, 0:n], in1=tb[:, 0:n], op=ALU.mult), reads=[tb], writes=[tc_])
                p = ps()
                k.op("pe", lambda e: e.matmul(p[0:64, 0:n], ONES[:, 0:64], tc_[:, 0:n], start=True, stop=True), reads=[ONES, tc_], writes=[p])
                k.op("act", lambda e: e.activation(out=tc_[:, 0:n], in_=p[0:64, 0:n], func=AF.Sqrt), reads=[p], writes=[tc_])
                k.op("dve", lambda e: e.tensor_scalar(out=tc_[:, 0:n], in0=tc_[:, 0:n], scalar1=1e-12, scalar2=None, op0=ALU.max), reads=[tc_], writes=[tc_])
                k.op("dve", lambda e: e.reciprocal(out=tc_[:, 0:n], in_=tc_[:, 0:n]), reads=[tc_], writes=[tc_])
                k.op("dve", lambda e: e.tensor_tensor(out=kk_[:, 0:n], in0=tb[:, 0:n], in1=tc_[:, 0:n], op=ALU.mult), reads=[tb, tc_], writes=[kk_])
                for d in range(2):
                    p = ps()
                    k.op("pe", lambda e: e.matmul(p[0:64, 0:n], W2P[:, d, hs], wds[:, 0:n], start=True, stop=True), reads=[W2P, wds], writes=[p])
                    lw = k.nxt("rc_lw", lw_)
                    k.op("act", lambda e: e.activation(out=lw[:, 0:n], in_=p[0:64, 0:n], func=AF.Sigmoid, bias=RP[:, 15 + d * 4 + h:16 + d * 4 + h], scale=1.0), reads=[p, RP], writes=[lw])
                    k.op("dve", lambda e: e.tensor_scalar(out=lw[:, 0:n], in0=lw[:, 0:n], scalar1=-0.606531, scalar2=None, op0=ALU.mult), reads=[lw], writes=[lw])
                    LWt = k.nxt("rc_LW", LW)
                    k.op("dve", lambda e: e.tensor_tensor_scan(out=LWt[:, 0:n], data0=RESET[:, 0:n], data1=lw[:, 0:n], initial=0.0, op0=ALU.mult, op1=ALU.add), reads=[RESET, lw], writes=[LWt])
                    LW3 = LWt[:, 0:n].rearrange("p (c t) -> p c t", t=64)
                    if d == 1:
                        tb2 = k.nxt("rc_tb", t_b)
                        k.op("dve", lambda e: e.tensor_tensor(out=tb2[:, 0:n].rearrange("p (c t) -> p c t", t=64), in0=LW3[:, :, 63:64].to_broadcast([64, nch, 64]), in1=LW3, op=ALU.subtract),
                             reads=[LWt], writes=[tb2])
                        k.op("dve", lambda e: e.tensor_tensor(out=LWt[:, 0:n], in0=tb2[:, 0:n], in1=lw[:, 0:n], op=ALU.add), reads=[tb2, lw], writes=[LWt])
                    tot_ap = LW3[:, :, 63] if d == 0 else LW3[:, :, 0]
                    k.op("act", lambda e: e.activation(out=wl_sb[:, d * 4 + h, c0:c0 + nch], in_=tot_ap, func=AF.Exp), reads=[LWt], writes=[wl_sb])
                    p = ps()
                    k.op("pe", lambda e: e.matmul(p[0:64, 0:n], A2P[:, d, hs], ads[:, 0:n], start=True, stop=True), reads=[A2P, ads], writes=[p])
                    a_ = k.nxt("rc_aa", aa)
                    k.op("act", lambda e: e.activation(out=a_[:, 0:n], in_=p[0:64, 0:n], func=AF.Sigmoid, bias=RP[:, 23 + d * 4 + h:24 + d * 4 + h], scale=1.0), reads=[p, RP], writes=[a_])
                    k.op("dve", lambda e: e.tensor_scalar(out=km[d][:, 0:n], in0=a_[:, 0:n], scalar1=RP[:, 35 + h:36 + h], scalar2=omka[:, h:h + 1], op0=ALU.mult, op1=ALU.add), reads=[a_, RP, omka], writes=[km[d]])
                    k.op("pool", lambda e: e.tensor_tensor(out=km[d][:, 0:n], in0=km[d][:, 0:n], in1=ks_[:, 0:n], op=ALU.mult), reads=[km[d], ks_], writes=[km[d]])
                    k.op("pool", lambda e: e.tensor_tensor(out=a_[:, 0:n], in0=a_[:, 0:n], in1=kk_[:, 0:n], op=ALU.mult), reads=[a_, kk_], writes=[a_])
                    e1 = k.nxt("rc_ta", t_a)
                    e2 = k.nxt("rc_tc", t_c)
                    k.op("act", lambda e: e.activation(out=e1[:, 0:n], in_=LWt[:, 0:n], func=AF.Exp), reads=[LWt], writes=[e1])
                    k.op("act", lambda e: e.activation(out=e2[:, 0:n], in_=LWt[:, 0:n], func=AF.Exp, scale=-1.0), reads=[LWt], writes=[e2])
                    k.op("dve", lambda e: e.tensor_tensor(out=lw[:, 0:n], in0=LWt[:, 0:n], in1=lw[:, 0:n], op=ALU.subtract), reads=[LWt, lw], writes=[lw])
                    k.op("act", lambda e: e.activation(out=lw[:, 0:n], in_=lw[:, 0:n], func=AF.Exp), reads=[lw], writes=[lw])
                    o4 = k.nxt("rc_ob4", ob4)
                    k.op("dve", lambda e: e.tensor_tensor(out=o4[:, 0, 0:n], in0=kk_[:, 0:n], in1=lw[:, 0:n], op=ALU.mult), reads=[kk_, lw], writes=[o4])
                    k.op("pool", lambda e: e.tensor_tensor(out=o4[:, 1, 0:n], in0=rs_[:, 0:n], in1=e1[:, 0:n], op=ALU.mult), reads=[rs_, e1], writes=[o4])
                    k.op("dve", lambda e: e.tensor_tensor(out=o4[:, 2, 0:n], in0=a_[:, 0:n], in1=e2[:, 0:n], op=ALU.mult), reads=[a_, e2], writes=[o4])
                    k.op("pool", lambda e: e.tensor_tensor(out=o4[:, 3, 0:n], in0=km[d][:, 0:n], in1=e2[:, 0:n], op=ALU.mult), reads=[km[d], e2], writes=[o4])
                    for q in range(4):
                        k.dma("sp", RS[d][h][q][:, t0:t0 + n], o4[:, q, 0:n], reads=[o4], writes=[RS])
                tb = k.nxt("rc_tb", t_b)
                k.op("dve", lambda e: e.tensor_tensor(out=tb[:, 0:n], in0=km[0][:, 0:n], in1=km[1][:, 0:n], op=ALU.add), reads=[km[0], km[1]], writes=[tb])
                k.op("dve", lambda e: e.scalar_tensor_tensor(out=tb[:, 0:n], in0=rs_[:, 0:n], scalar=RP[:, 43 + h:44 + h], in1=tb[:, 0:n], op0=ALU.mult, op1=ALU.mult), reads=[rs_, RP, tb], writes=[tb])
                p = ps()
                k.op("pe", lambda e: e.matmul(p[0:64, 0:n], ONES[:, 0:64], tb[:, 0:n], start=True, stop=True), reads=[ONES, tb], writes=[p])
                ta = k.nxt("rc_ta", t_a)
                k.op("dve", lambda e: e.tensor_tensor(out=ta[:, 0:n], in0=p[0:64, 0:n], in1=vs_[:, 0:n], op=ALU.mult), reads=[p, vs_], writes=[ta])
                k.dma("sp", BON[hs, t0:t0 + n], ta[:, 0:n], reads=[ta], writes=[BON])
        if DSTOP <= 1:
            return
        RM = k.sbuf("rs_mask", [64, 2, 5, 64], F32)
        for d in range(2):
            k.dma("sp", RM[:, d, :, :], rmask_in[d], reads=[rmask_in], writes=[RM])
        X = [k.sbuf("rs_x%d" % i, [64, 4, 4, 64], BF16) for i in range(4)]
        XV = [k.sbuf("rs_xv%d" % i, [64, 4, 64], BF16) for i in range(4)]
        S0 = k.sbuf("rs_s0", [64, 8, 64], F32)
        S0b = k.sbuf("rs_s0b", [64, 8, 64], BF16)
        k.op("pool", lambda e: e.memset(S0[:], 0.0), writes=[S0])
        k.op("pool", lambda e: e.memset(S0b[:], 0.0), writes=[S0b])
        PQ = [k.sbuf("rs_pq%d" % i, [64, 2, 8, 64], BF16) for i in range(2)]
        A2 = k.sbuf("rs_a2", [64, 2, 8, 64], BF16)
        A3 = k.sbuf("rs_a3", [64, 8, 64], BF16)
        TOK = k.sbuf("rs_tok", [64, 3, 8, 64], BF16)
        G = k.sbuf("rs_g", [64, 8, 64], F32)
        Gb = k.sbuf("rs_gb", [64, 8, 64], BF16)
        nUb = k.sbuf("rs_nub", [64, 8, 64], BF16)
        Ysb = [k.sbuf("rs_y%d" % i, [64, 8, 64], F32) for i in range(2)]
        stmp = k.sbuf("rs_stmp", [64, 8, 64], F32)
        RSv = [[RS[d][h] for h in range(4)] for d in range(2)]
        for i in range(NSTEP):
            xs, xvs = [], []
            for d in range(2):
                c = CH_ORDER[d][i]
                x_ = k.nxt("rs_x", X)
                xv_ = k.nxt("rs_xv", XV)
                for h in range(4):
                    k.dma("sp" if h % 2 == 0 else "pool", x_[:, h, :, :], RS[d][h].rearrange("q p t -> p q t")[:, :, c * 64:(c + 1) * 64], reads=[RS], writes=[x_])
                k.dma("sp", xv_[:], VS.t.ap().rearrange("h p t -> p h t")[:, :, c * 64:(c + 1) * 64], reads=[VS], writes=[xv_])
                xs.append(x_)
                xvs.append(xv_)
            if SSTOP <= 1:
                continue
            pT1 = [ps(), ps()]
            pT2 = [ps(), ps()]
            pT3 = ps()
            for d in range(2):
                x_ = xs[d]
                for h in range(4):
                    cs_ = slice(h * 64, (h + 1) * 64)
                    cs2 = slice(256 + h * 64, 256 + (h + 1) * 64)
                    k.op("pe", lambda e: e.matmul(pT1[d][0:64, cs_], x_[:, h, 2, :], x_[:, h, 0, :], start=True, stop=True), reads=[x_], writes=[pT1[d]])
                    k.op("pe", lambda e: e.matmul(pT1[d][0:64, cs2], x_[:, h, 0, :], x_[:, h, 2, :], start=True, stop=True), reads=[x_], writes=[pT1[d]])
                    k.op("pe", lambda e: e.matmul(pT2[d][0:64, cs_], x_[:, h, 3, :], x_[:, h, 0, :], start=True, stop=True), reads=[x_], writes=[pT2[d]])
                    k.op("pe", lambda e: e.matmul(pT2[d][0:64, cs2], x_[:, h, 2, :], x_[:, h, 1, :], start=True, stop=True), reads=[x_], writes=[pT2[d]])
                    k.op("pe", lambda e: e.matmul(pT3[0:64, d * 256 + h * 64:d * 256 + (h + 1) * 64], x_[:, h, 3, :], x_[:, h, 1, :], start=True, stop=True), reads=[x_], writes=[pT3])
            pq = k.nxt("rs_pq", PQ)
            for d in range(2):
                k.op("dve", lambda e, d=d: e.tensor_tensor(out=pq[:, :, d * 4:(d + 1) * 4, :], in0=pT1[d][0:64, :].rearrange("p (a h t) -> p a h t", a=2, h=4),
                                                           in1=RM[:, d, 0:2, :].unsqueeze(2).to_broadcast([64, 2, 4, 64]), op=ALU.mult), reads=[pT1[d], RM], writes=[pq])
                k.op("dve", lambda e, d=d: e.tensor_tensor(out=A2[:, :, d * 4:(d + 1) * 4, :], in0=pT2[d][0:64, :].rearrange("p (a h t) -> p a h t", a=2, h=4),
                                                           in1=RM[:, d, 2:4, :].unsqueeze(2).to_broadcast([64, 2, 4, 64]), op=ALU.mult), reads=[pT2[d], RM], writes=[A2])
                k.op("dve", lambda e, d=d: e.tensor_tensor(out=A3[:, d * 4:(d + 1) * 4, :], in0=pT3[0:64, d * 256:(d + 1) * 256].rearrange("p (h t) -> p h t", h=4),
                                                           in1=RM[:, d, 4:5, :].to_broadcast([64, 4, 64]), op=ALU.mult), reads=[pT3, RM], writes=[A3])
            if SSTOP <= 2:
                continue
            pK = [ps(), ps()]
            for d in range(2):
                x_ = xs[d]
                for h in range(4):
                    dh = d * 4 + h
                    k.op("pe", lambda e: e.matmul(pK[0][0:64, dh * 64:(dh + 1) * 64], x_[:, h, 2, :], identb[0:64, 0:64], start=True, stop=True), reads=[x_, identb], writes=[pK[0]])
                    k.op("pe", lambda e: e.matmul(pK[1][0:64, dh * 64:(dh + 1) * 64], x_[:, h, 3, :], identb[0:64, 0:64], start=True, stop=True), reads=[x_, identb], writes=[pK[1]])
            pV = ps()
            for d in range(2):
                for h in range(4):
                    dh = d * 4 + h
                    k.op("pe", lambda e: e.matmul(pV[0:64, dh * 64:(dh + 1) * 64], xvs[d][:, h, :], identb[0:64, 0:64], start=True, stop=True), reads=[xvs[d], identb], writes=[pV])
            k.op("act", lambda e: e.copy(out=TOK[:, 0, :, :], in_=pK[0][0:64, :].rearrange("p (a t) -> p a t", t=64)), reads=[pK[0]], writes=[TOK])
            k.op("act", lambda e: e.copy(out=TOK[:, 1, :, :], in_=pK[1][0:64, :].rearrange("p (a t) -> p a t", t=64)), reads=[pK[1]], writes=[TOK])
            k.op("dve", lambda e: e.tensor_copy(out=TOK[:, 2, :, :], in_=pV[0:64, :].rearrange("p (a t) -> p a t", t=64)), reads=[pV], writes=[TOK])
            if SSTOP <= 3:
                continue
            pG = ps()
            for d in range(2):
                for h in range(4):
                    dh = d * 4 + h
                    o_ = pG[0:64, dh * 64:(dh + 1) * 64]
                    k.op("pe", lambda e: e.matmul(o_, xs[d][:, h, 0, :], S0b[:, dh, :], start=True, stop=False), reads=[xs[d], S0b], writes=[pG])
                    k.op("pe", lambda e: e.matmul(o_, A2[:, 0, dh, :], TOK[:, 2, dh, :], start=False, stop=True), reads=[A2, TOK], writes=[pG])
            k.op("dve", lambda e: e.tensor_copy(out=G[:], in_=pG[0:64, :].rearrange("p (a t) -> p a t", t=64)), reads=[pG], writes=[G])
            k.op("act", lambda e: e.copy(out=Gb[:], in_=pG[0:64, :].rearrange("p (a t) -> p a t", t=64)), reads=[pG], writes=[Gb])
            if SSTOP <= 4:
                continue
            cur = pq
            for kk in range(6):
                pD = ps()
                for dh in range(8):
                    k.op("pe", lambda e, dh=dh: e.matmul(pD[0:64, dh * 64:(dh + 1) * 64], cur[:, 0, dh, :], Gb[:, dh, :], start=True, stop=True), reads=[cur, Gb], writes=[pD])
                if kk < 5:
                    pP = ps()
                    pQ = ps()
                    for dh in range(8):
                        k.op("pe", lambda e, dh=dh: e.matmul(pP[0:64, dh * 64:(dh + 1) * 64], cur[:, 1, dh, :], cur[:, 0, dh, :], start=True, stop=True), reads=[cur], writes=[pP])
                        k.op("pe", lambda e, dh=dh: e.matmul(pQ[0:64, dh * 64:(dh + 1) * 64], cur[:, 0, dh, :], cur[:, 1, dh, :], start=True, stop=True), reads=[cur], writes=[pQ])
                k.op("dve", lambda e: e.tensor_tensor(out=G[:], in0=pD[0:64, :].rearrange("p (a t) -> p a t", t=64), in1=G[:], op=ALU.add), reads=[pD, G], writes=[G])
                k.op("pool", lambda e: e.tensor_copy(out=Gb[:], in_=G[:]), reads=[G], writes=[Gb])
                if kk < 5:
                    nx = k.nxt("rs_pq", PQ)
                    k.op("act", lambda e: e.copy(out=nx[:, 0, :, :], in_=pP[0:64, :].rearrange("p (a t) -> p a t", t=64)), reads=[pP], writes=[nx])
                    k.op("dve", lambda e: e.tensor_copy(out=nx[:, 1, :, :], in_=pQ[0:64, :].rearrange("p (a t) -> p a t", t=64)), reads=[pQ], writes=[nx])
                    cur = nx
            k.op("act", lambda e: e.activation(out=nUb[:], in_=G[:], func=AF.Copy, scale=-1.0), reads=[G], writes=[nUb])
            if SSTOP <= 5:
                continue
            pY = ps()
            for d in range(2):
                for h in range(4):
                    dh = d * 4 + h
                    o_ = pY[0:64, dh * 64:(dh + 1) * 64]
                    k.op("pe", lambda e: e.matmul(o_, xs[d][:, h, 1, :], S0b[:, dh, :], start=True, stop=False), reads=[xs[d], S0b], writes=[pY])
                    k.op("pe", lambda e: e.matmul(o_, A2[:, 1, dh, :], Gb[:, dh, :], start=False, stop=False), reads=[A2, Gb], writes=[pY])
                    k.op("pe", lambda e: e.matmul(o_, A3[:, dh, :], TOK[:, 2, dh, :], start=False, stop=True), reads=[A3, TOK], writes=[pY])
            y_ = k.nxt("rs_y", Ysb)
            k.op("act", lambda e: e.copy(out=y_[:], in_=pY[0:64, :].rearrange("p (a t) -> p a t", t=64)), reads=[pY], writes=[y_])
            for d in range(2):
                c = CH_ORDER[d][i]
                k.dma("sp", YS[d][c * 64:(c + 1) * 64, :].rearrange("p (h t) -> p h t", t=64), y_[:, d * 4:(d + 1) * 4, :], reads=[y_], writes=[YS[d]])
            if SSTOP <= 6:
                continue
            pS = ps()
            for d in range(2):
                for h in range(4):
                    dh = d * 4 + h
                    o_ = pS[0:64, dh * 64:(dh + 1) * 64]
                    k.op("pe", lambda e: e.matmul(o_, TOK[:, 0, dh, :], nUb[:, dh, :], start=True, stop=False), reads=[TOK, nUb], writes=[pS])
                    k.op("pe", lambda e: e.matmul(o_, TOK[:, 1, dh, :], TOK[:, 2, dh, :], start=False, stop=True), reads=[TOK], writes=[pS])
            k.op("dve", lambda e: e.tensor_tensor(out=stmp[:], in0=pS[0:64, :].rearrange("p (a t) -> p a t", t=64), in1=S0[:], op=ALU.add), reads=[pS, S0], writes=[stmp])
            for d in range(2):
                c = CH_ORDER[d][i]
                k.op("dve", lambda e, d=d, c=c: e.tensor_tensor(out=S0[:, d * 4:(d + 1) * 4, :], in0=stmp[:, d * 4:(d + 1) * 4, :],
                                                                in1=wl_sb[:, d * 4:(d + 1) * 4, c:c + 1].to_broadcast([64, 4, 64]), op=ALU.mult), reads=[stmp, wl_sb], writes=[S0])
            k.op("act", lambda e: e.copy(out=S0b[:], in_=S0[:]), reads=[S0], writes=[S0b])
        if DSTOP <= 2:
            return
        LNP = k.sbuf("rf_lnp", [128, 2, 2], F32)
        k.dma("sp", LNP[:], lnp_in[l], reads=[lnp_in], writes=[LNP])
        yf = mk("yf", [128, 4, 64], F32, 2)
        yb = mk("yb", [128, 4, 64], F32, 2)
        s4_ = mk("s4", [128, 4], F32, 2)
        q4_ = mk("q4", [128, 4, 64], F32, 2)
        bg = mk("bg", [128, 2, 2, 128], F32, 2)
        o2 = mk("o2", [128, 2, 128], F32, 2)
        ob = mk("ob", [128, 2, 128], BF16, 2)
        BONv = BON.t.ap().rearrange("(c p) t -> p c t", p=128)
        GGv = GGd.t.ap().rearrange("(c p) t -> p c t", p=128)
        MIXr = MIXT.t.ap()[512:768, :].rearrange("(c p) t -> p c t", p=128)
        for t in range(NTILE):
            tsl = slice(t * 128, (t + 1) * 128)
            a = k.nxt("rc_yf", yf)
            b = k.nxt("rc_yb", yb)
            s4 = k.nxt("rc_s4", s4_)
            q4 = k.nxt("rc_q4", q4_)
            k.dma("sp", a[:].rearrange("p h e -> p (h e)"), YS[0][tsl, :], reads=[YS[0]], writes=[a])
            k.dma("sp", b[:].rearrange("p h e -> p (h e)"), YS[1][tsl, :], reads=[YS[1]], writes=[b])
            k.op("dve", lambda e: e.tensor_tensor(out=a[:], in0=a[:], in1=b[:], op=ALU.add), reads=[a, b], writes=[a])
            k.op("dve", lambda e: e.tensor_reduce(out=s4[:], in_=a[:], axis=AX.X, op=ALU.add), reads=[a], writes=[s4])
            k.op("dve", lambda e: e.tensor_scalar(out=s4[:], in0=s4[:], scalar1=-1.0 / 64, scalar2=None, op0=ALU.mult), reads=[s4], writes=[s4])
            k.op("dve", lambda e: e.tensor_tensor(out=a[:], in0=a[:], in1=s4[:].unsqueeze(2).to_broadcast([128, 4, 64]), op=ALU.add), reads=[a, s4], writes=[a])
            k.op("pool", lambda e: e.tensor_tensor(out=q4[:], in0=a[:], in1=a[:], op=ALU.mult), reads=[a], writes=[q4])
            k.op("dve", lambda e: e.tensor_reduce(out=s4[:], in_=q4[:], axis=AX.X, op=ALU.add), reads=[q4], writes=[s4])
            k.op("act", lambda e: e.activation(out=s4[:], in_=s4[:], func=AF.Sqrt, scale=1.0 / 64, bias=64e-5), reads=[s4], writes=[s4])
            k.op("dve", lambda e: e.reciprocal(out=s4[:], in_=s4[:]), reads=[s4], writes=[s4])
            k.op("dve", lambda e: e.tensor_tensor(out=a[:], in0=a[:], in1=s4[:].unsqueeze(2).to_broadcast([128, 4, 64]), op=ALU.mult), reads=[a, s4], writes=[a])
            p = ps()
            av = a[:].rearrange("p h e -> p (h e)")
            for c in range(2):
                k.op("pe", lambda e, c=c: e.transpose(p[:, c * 128:(c + 1) * 128], av[:, c * 128:(c + 1) * 128], ident[:]), reads=[a, ident], writes=[p])
            g_ = k.nxt("rc_bg", bg)
            k.dma("sp", g_[:, 0, :, :], BONv[:, :, tsl], reads=[BON], writes=[g_])
            k.dma("sp", g_[:, 1, :, :], GGv[:, :, tsl], reads=[GGd], writes=[g_])
            o_ = k.nxt("rc_o2", o2)
            ob_ = k.nxt("rc_ob", ob)
            for c in range(2):
                k.op("act", lambda e, c=c: e.activation(out=o_[:, c, :], in_=p[:, c * 128:(c + 1) * 128], func=AF.Identity, scale=LNP[:, c, 0:1], bias=LNP[:, c, 1:2]), reads=[p, LNP], writes=[o_])
            k.op("dve", lambda e: e.tensor_tensor(out=o_[:], in0=o_[:], in1=g_[:, 0, :, :], op=ALU.add), reads=[o_, g_], writes=[o_])
            k.op("dve", lambda e: e.tensor_tensor(out=ob_[:], in0=o_[:], in1=g_[:, 1, :, :], op=ALU.mult), reads=[o_, g_], writes=[ob_])
            k.dma("sp", MIXr[:, :, tsl], ob_[:], reads=[ob_], writes=[MIXT])

    def phase_E(l):
        WO = k.sbuf("WO", [128, 8, D], BF16)
        for kc in range(8):
            k.dma("pool", WO[:, kc, :], w_out[l][kc * 128:(kc + 1) * 128, :], reads=[w_out], writes=[WO])
        mx = [k.sbuf("pe_mx%d" % i, [128, 8, 512], BF16) for i in range(2)]
        xb_ = [k.sbuf("pe_x%d" % i, [128, 8, 512], F32) for i in range(2)]
        MIXv8 = MIXT.t.ap().rearrange("(kc p) t -> p kc t", p=128)
        for (t0, n) in BLOCKS:
            s_ = 1 if t0 == 0 else 0
            if s_ == 1 and l == DEPTH - 1:
                continue
            m_ = k.nxt("pe_mx", mx)
            xb = k.nxt("pe_x", xb_)
            k.dma("sp", m_[:, :, 0:n], MIXv8[:, :, t0:t0 + n], reads=[MIXT], writes=[m_])
            k.dma("sp", xb[:, :, 0:n], XTv[:, :, t0:t0 + n], reads=[XT], writes=[xb])
            for oc in range(8):
                p = ps()
                for kc in range(8):
                    k.op("pe", lambda e, kc=kc: e.matmul(p[:, 0:n], WO[:, kc, oc * 128:(oc + 1) * 128], m_[:, kc, 0:n], start=(kc == 0), stop=(kc == 7)), reads=[WO, m_], writes=[p])
                k.op("dve", lambda e: e.scalar_tensor_tensor(out=xb[:, oc, 0:n], in0=p[:, 0:n], scalar=modT[l][:, 16 + oc, s_:s_ + 1], in1=xb[:, oc, 0:n], op0=ALU.mult, op1=ALU.add),
                     reads=[p, modT[l], xb], writes=[xb])
            k.dma("sp", XTv[:, :, t0:t0 + n], xb[:, :, 0:n], reads=[xb], writes=[XT])

    rw_in = ein("routerT", [L, 128, 8, 36])
    rb_in = ein("router_b", [L, 36])
    wgu_in = ein("exp_w_gu", [L, 32, D, D])
    wdn_in = ein("exp_w_down", [L, 32, 512, D])

    def phase_F(l):
        RW = k.sbuf("pf_rw", [128, 8, 36], F32)
        RB = k.sbuf("pf_rb", [128, 36], F32)
        k.dma("sp", RW[:], rw_in[l], reads=[rw_in], writes=[RW])
        k.dma("sp", RB[:], rb_in[l].partition_broadcast(128), reads=[rb_in], writes=[RB])
        xb = k.sbuf("pf_x", [128, 8, 512], F32)
        sq = k.sbuf("pf_sq", [128, 8, 512], F32)
        rr = k.sbuf("pf_r", [128, 512], F32)
        hf = k.sbuf("pf_hf", [128, 8, 512], F32)
        hbf2 = [k.sbuf("pf_hb%d" % i, [128, 8, 512], BF16) for i in range(3)]
        WT2 = [k.sbuf("pf_wt%d" % i, [128, 4, 32], F32) for i in range(3)]
        lg = k.sbuf("pf_lg", [128, 36], F32)
        gm = k.sbuf("pf_gm", [128, 1], F32)
        gmask = k.sbuf("pf_gmask", [128, 4], F32)
        gex = k.sbuf("pf_gex", [128, 4], F32)
        gw = k.sbuf("pf_gw", [128, 1], F32)
        e84 = k.sbuf("pf_e84", [128, 8, 4], F32)
        es = k.sbuf("pf_es", [128, 8], F32)
        m1 = k.sbuf("pf_m1", [128, 1], F32)
        m2 = k.sbuf("pf_m2", [128, 1], F32)
        k1 = k.sbuf("pf_k1", [128, 8], F32)
        k2 = k.sbuf("pf_k2", [128, 8], F32)
        es2 = k.sbuf("pf_es2", [128, 8], F32)
        p2 = k.sbuf("pf_p2", [128, 1], F32)
        w1 = k.sbuf("pf_w1", [128, 1], F32)
        w2 = k.sbuf("pf_w2", [128, 1], F32)
        wj = k.sbuf("pf_wj", [128, 8], F32)
        WGU = [k.sbuf("pf_wgu%d" % i, [128, 8, D], BF16) for i in range(2)]
        WDN = [k.sbuf("pf_wdn%d" % i, [128, 4, D], BF16) for i in range(2)]
        sg = [k.sbuf("pf_sg%d" % i, [128, 512], F32) for i in range(2)]
        act_ = [k.sbuf("pf_act%d" % i, [128, 4, 512], BF16) for i in range(2)]
        yacc2 = [k.sbuf("pf_yacc%d" % i, [128, 4, D], F32) for i in range(3)]
        blks = [b_ for b_ in BLOCKS if not (b_[0] == 0 and l == DEPTH - 1)]
        GS = 3
        groups = [blks[i_:i_ + GS] for i_ in range(0, len(blks), GS)]
        for grp in groups:
            for gi, (t0, n) in enumerate(grp):
                hbf, WT, yacc = hbf2[gi], WT2[gi], yacc2[gi]
                s_ = 1 if t0 == 0 else 0
                nj = n // 128
                k.dma("sp", xb[:, :, 0:n], XTv[:, :, t0:t0 + n], reads=[XT], writes=[xb])
                k.op("act", lambda e: e.activation(out=sq[:, :, 0:n], in_=xb[:, :, 0:n], func=AF.Square), reads=[xb], writes=[sq])
                p = ps()
                for kc in range(8):
                    k.op("pe", lambda e, kc=kc: e.matmul(p[:, 0:n], ones_f[:], sq[:, kc, 0:n], start=(kc == 0), stop=(kc == 7)), reads=[ones_f, sq], writes=[p])
                k.op("act", lambda e: e.activation(out=rr[:, 0:n], in_=p[:, 0:n], func=AF.Sqrt, scale=1.0 / D, bias=EPS), reads=[p], writes=[rr])
                k.op("dve", lambda e: e.reciprocal(out=rr[:, 0:n], in_=rr[:, 0:n]), reads=[rr], writes=[rr])
                for kc in range(8):
                    k.op("dve", lambda e, kc=kc: e.tensor_tensor(out=sq[:, kc, 0:n], in0=xb[:, kc, 0:n], in1=rr[:, 0:n], op=ALU.mult), reads=[xb, rr], writes=[sq])
                    k.op("act", lambda e, kc=kc: e.activation(out=hf[:, kc, 0:n], in_=sq[:, kc, 0:n], func=AF.Identity, scale=A2[l][:, kc, s_:s_ + 1], bias=modT[l][:, 24 + kc, s_:s_ + 1]),
                         reads=[sq, A2[l], modT[l]], writes=[hf])
                k.op("pool", lambda e: e.tensor_copy(out=hbf[:, :, 0:n], in_=hf[:, :, 0:n]), reads=[hf], writes=[hbf])
                for j in range(nj):
                    jsl = slice(j * 128, (j + 1) * 128)
                    p = ps()
                    for kc in range(8):
                        k.op("pe", lambda e, kc=kc: e.matmul(p[:, 0:36], hf[:, kc, jsl], RW[:, kc, :], start=(kc == 0), stop=(kc == 7)), reads=[hf, RW], writes=[p])
                    k.op("dve", lambda e: e.tensor_tensor(out=lg[:], in0=p[:, 0:36], in1=RB[:], op=ALU.add), reads=[p, RB], writes=[lg])
                    k.op("dve", lambda e: e.tensor_reduce(out=gm[:], in_=lg[:, 0:4], axis=AX.X, op=ALU.max), reads=[lg], writes=[gm])
                    k.op("dve", lambda e: e.tensor_scalar(out=gmask[:], in0=lg[:, 0:4], scalar1=gm[:, 0:1], scalar2=None, op0=ALU.is_equal), reads=[lg, gm], writes=[gmask])
                    k.op("dve", lambda e: e.tensor_scalar(out=gex[:], in0=lg[:, 0:4], scalar1=gm[:, 0:1], scalar2=None, op0=ALU.subtract), reads=[lg, gm], writes=[gex])
                    k.op("act", lambda e: e.activation(out=gex[:], in_=gex[:], func=AF.Exp), reads=[gex], writes=[gex])
                    k.op("dve", lambda e: e.tensor_reduce(out=gw[:], in_=gex[:], axis=AX.X, op=ALU.add), reads=[gex], writes=[gw])
                    k.op("dve", lambda e: e.reciprocal(out=gw[:], in_=gw[:]), reads=[gw], writes=[gw])
                    k.op("dve", lambda e: e.tensor_tensor(out=e84[:], in0=lg[:, 4:36].rearrange("p (g j) -> p j g", j=8), in1=gmask[:].unsqueeze(1).to_broadcast([128, 8, 4]), op=ALU.mult),
                         reads=[lg, gmask], writes=[e84])
                    k.op("dve", lambda e: e.tensor_reduce(out=es[:], in_=e84[:], axis=AX.X, op=ALU.add), reads=[e84], writes=[es])
                    k.op("dve", lambda e: e.tensor_reduce(out=m1[:], in_=es[:], axis=AX.X, op=ALU.max), reads=[es], writes=[m1])
                    k.op("dve", lambda e: e.tensor_scalar(out=k1[:], in0=es[:], scalar1=m1[:, 0:1], scalar2=None, op0=ALU.is_equal), reads=[es, m1], writes=[k1])
                    k.op("dve", lambda e: e.scalar_tensor_tensor(out=es2[:], in0=k1[:], scalar=-1e30, in1=es[:], op0=ALU.mult, op1=ALU.add), reads=[k1, es], writes=[es2])
                    k.op("dve", lambda e: e.tensor_reduce(out=m2[:], in_=es2[:], axis=AX.X, op=ALU.max), reads=[es2], writes=[m2])
                    k.op("dve", lambda e: e.tensor_scalar(out=k2[:], in0=es2[:], scalar1=m2[:, 0:1], scalar2=None, op0=ALU.is_equal), reads=[es2, m2], writes=[k2])
                    k.op("dve", lambda e: e.tensor_tensor(out=p2[:], in0=m2[:], in1=m1[:], op=ALU.subtract), reads=[m1, m2], writes=[p2])
                    k.op("act", lambda e: e.activation(out=p2[:], in_=p2[:], func=AF.Exp), reads=[p2], writes=[p2])
                    k.op("dve", lambda e: e.tensor_scalar(out=w1[:], in0=p2[:], scalar1=1.0, scalar2=None, op0=ALU.add), reads=[p2], writes=[w1])
                    k.op("dve", lambda e: e.reciprocal(out=w1[:], in_=w1[:]), reads=[w1], writes=[w1])
                    k.op("dve", lambda e: e.tensor_tensor(out=w1[:], in0=w1[:], in1=gw[:], op=ALU.mult), reads=[w1, gw], writes=[w1])
                    k.op("dve", lambda e: e.tensor_tensor(out=w2[:], in0=w1[:], in1=p2[:], op=ALU.mult), reads=[w1, p2], writes=[w2])
                    k.op("dve", lambda e: e.tensor_scalar(out=wj[:], in0=k1[:], scalar1=w1[:, 0:1], scalar2=None, op0=ALU.mult), reads=[k1, w1], writes=[wj])
                    k.op("dve", lambda e: e.scalar_tensor_tensor(out=wj[:], in0=k2[:], scalar=w2[:, 0:1], in1=wj[:], op0=ALU.mult, op1=ALU.add), reads=[k2, w2, wj], writes=[wj])
                    k.op("dve", lambda e, j=j: e.tensor_tensor(out=WT[:, j, :].rearrange("p (g j) -> p g j", j=8), in0=gmask[:].unsqueeze(2).to_broadcast([128, 4, 8]),
                                                               in1=wj[:].unsqueeze(1).to_broadcast([128, 4, 8]), op=ALU.mult), reads=[gmask, wj], writes=[WT])
            for ex in range(32):
                wg_ = k.nxt("pf_wgu", WGU)
                wd_ = k.nxt("pf_wdn", WDN)
                for kc in range(8):
                    k.dma("pool", wg_[:, kc, :], wgu_in[l][ex][kc * 128:(kc + 1) * 128, :], reads=[wgu_in], writes=[wg_])
                for hk in range(4):
                    k.dma("pool", wd_[:, hk, :], wdn_in[l][ex][hk * 128:(hk + 1) * 128, :], reads=[wdn_in], writes=[wd_])
                for gi, (t0, n) in enumerate(grp):
                    hbf, WT, yacc = hbf2[gi], WT2[gi], yacc2[gi]
                    nj = n // 128
                    a_ = k.nxt("pf_act", act_)
                    for hc in range(4):
                        pg = ps()
                        pu = ps()
                        for kc in range(8):
                            k.op("pe", lambda e, kc=kc: e.matmul(pg[:, 0:n], wg_[:, kc, hc * 128:(hc + 1) * 128], hbf[:, kc, 0:n], start=(kc == 0), stop=(kc == 7)), reads=[wg_, hbf], writes=[pg])
                        for kc in range(8):
                            k.op("pe", lambda e, kc=kc: e.matmul(pu[:, 0:n], wg_[:, kc, 512 + hc * 128:512 + (hc + 1) * 128], hbf[:, kc, 0:n], start=(kc == 0), stop=(kc == 7)), reads=[wg_, hbf], writes=[pu])
                        s2 = k.nxt("pf_sg", sg)
                        k.op("act", lambda e: e.activation(out=s2[:, 0:n], in_=pg[:, 0:n], func=AF.Silu), reads=[pg], writes=[s2])
                        k.op("dve", lambda e, hc=hc: e.tensor_tensor(out=a_[:, hc, 0:n], in0=pu[:, 0:n], in1=s2[:, 0:n], op=ALU.mult), reads=[pu, s2], writes=[a_])
                    for j in range(nj):
                        jsl = slice(j * 128, (j + 1) * 128)
                        for half in range(2):
                            py = ps()
                            for hk in range(4):
                                k.op("pe", lambda e, hk=hk: e.matmul(py[:, :], a_[:, hk, jsl], wd_[:, hk, half * 512:(half + 1) * 512], start=(hk == 0), stop=(hk == 3)), reads=[a_, wd_], writes=[py])
                            if ex == 0:
                                k.op("dve", lambda e: e.tensor_scalar(out=yacc[:, j, half * 512:(half + 1) * 512], in0=py[:, :], scalar1=WT[:, j, ex:ex + 1], scalar2=None, op0=ALU.mult),
                                     reads=[py, WT], writes=[yacc])
                            else:
                                k.op("dve", lambda e: e.scalar_tensor_tensor(out=yacc[:, j, half * 512:(half + 1) * 512], in0=py[:, :], scalar=WT[:, j, ex:ex + 1], in1=yacc[:, j, half * 512:(half + 1) * 512],
                                                                             op0=ALU.mult, op1=ALU.add), reads=[py, WT, yacc], writes=[yacc])
            for gi, (t0, n) in enumerate(grp):
                hbf, WT, yacc = hbf2[gi], WT2[gi], yacc2[gi]
                s_ = 1 if t0 == 0 else 0
                nj = n // 128
                k.dma("sp", xb[:, :, 0:n], XTv[:, :, t0:t0 + n], reads=[XT], writes=[xb])
                for j in range(nj):
                    jsl = slice(j * 128, (j + 1) * 128)
                    for half in range(2):
                        p = ps()
                        for q in range(4):
                            oc = half * 4 + q
                            k.op("pe", lambda e, q=q, oc=oc: e.transpose(p[:, q * 128:(q + 1) * 128], yacc[:, j, oc * 128:(oc + 1) * 128], ident[:]), reads=[yacc, ident], writes=[p])
                        for q in range(4):
                            oc = half * 4 + q
                            k.op("dve", lambda e, q=q, oc=oc: e.scalar_tensor_tensor(out=xb[:, oc, jsl], in0=p[:, q * 128:(q + 1) * 128], scalar=modT[l][:, 40 + oc, s_:s_ + 1], in1=xb[:, oc, jsl],
                                                                                     op0=ALU.mult, op1=ALU.add), reads=[p, modT[l], xb], writes=[xb])
                k.dma("sp", XTv[:, :, t0:t0 + n], xb[:, :, 0:n], reads=[xb], writes=[XT])

    for l in range(n_layers):
        if stage >= 1:
            with k.scope():
                phase_A(l)
        if stage >= 2 and 'B' not in DBG_SKIP:
            with k.scope():
                phase_B(l)
        if stage >= 3 and 'D' not in DBG_SKIP:
            with k.scope():
                phase_D(l)
        if stage >= 4 and 'C' not in DBG_SKIP:
            with k.scope():
                phase_C(l)
        if stage >= 5:
            with k.scope():
                phase_E(l)
        if stage >= 6:
            with k.scope():
                phase_F(l)

    with k.scope():
        if dbg_out:
            tq = k.sbuf("dbgq", [128, NT], BF16)
            tf = k.sbuf("dbgf", [128, NPAD], F32)

        def dump_bf(src_ap, srcbuf, dst_ap, dstbuf):
            k.dma("sp", tq[:], src_ap, reads=[srcbuf], writes=[tq])
            k.op("dve", lambda e: e.tensor_copy(out=tf[:, 0:NT], in_=tq[:]), reads=[tq], writes=[tf])
            k.dma("sp", dst_ap, tf[:, 0:NT], reads=[tf], writes=[dstbuf])

        def dump_f(src, dst, rows, cols):
            for r0 in range(0, rows, 128):
                nr = min(128, rows - r0)
                k.dma("sp", tf[0:nr, 0:cols], src[r0:r0 + nr, :], reads=[src], writes=[tf])
                k.dma("sp", dst[r0:r0 + nr, :], tf[0:nr, 0:cols], reads=[tf], writes=[dst])
        for name in dbg_out:
            if name == "QT":
                dump_bf(QT[0], QT, dbg_out["QT"][:], dbg_out["QT"])
            if name == "KT":
                dump_bf(KT[1], KT, dbg_out["KT"][:], dbg_out["KT"])
            if name == "MIXT":
                for r0 in range(0, 1024, 128):
                    dump_bf(MIXT[r0:r0 + 128, :], MIXT, dbg_out["MIXT"][r0:r0 + 128, :], dbg_out["MIXT"])
            if name == "RZT":
                dump_f(RZT, dbg_out["RZT"], 960, NPAD)
            if name == "MQKT":
                dump_f(MQKT, dbg_out["MQKT"], 512, NPAD)
            if name == "MGT":
                dump_f(MGT, dbg_out["MGT"], 16, NT)
            if name == "XT":
                dump_f(XT, dbg_out["XT"], D, NT)

    with k.scope():
        fin_x = [k.sbuf("finx%d" % i, [128, 8, 512], F32) for i in range(2)]
        fin_sq = k.sbuf("finsq", [128, 8, 512], F32)
        fin_r = k.sbuf("finr", [128, 512], F32)
        fin_o = [k.sbuf("fino%d" % i, [128, D], F32) for i in range(2)]
        for (t0, n) in BLOCKS[1:]:
            xb = k.nxt("finx", fin_x)
            k.dma("sp", xb[:], XTv[:, :, t0:t0 + n], reads=[XT], writes=[xb])
            k.op("act", lambda e: e.activation(out=fin_sq[:], in_=xb[:], func=AF.Square), reads=[xb], writes=[fin_sq])
            p = ps()
            for kc in range(8):
                k.op("pe", lambda e, kc=kc: e.matmul(p[:, :], ones_f[:], fin_sq[:, kc, :], start=(kc == 0), stop=(kc == 7)),
                     reads=[ones_f, fin_sq], writes=[p])
            k.op("act", lambda e: e.activation(out=fin_r[:], in_=p[:, :], func=AF.Sqrt, scale=1.0 / D, bias=EPS), reads=[p], writes=[fin_r])
            k.op("dve", lambda e: e.reciprocal(out=fin_r[:], in_=fin_r[:]), reads=[fin_r], writes=[fin_r])
            for kc in range(8):
                k.op("dve", lambda e, kc=kc: e.scalar_tensor_tensor(out=xb[:, kc, :], in0=xb[:, kc, :], scalar=fg[:, kc:kc + 1], in1=fin_r[:],
                                                                    op0=ALU.mult, op1=ALU.mult), reads=[xb, fg, fin_r], writes=[xb])
            for j in range(n // 128):
                fo = k.nxt("fino", fin_o)
                for half in range(2):
                    p = ps()
                    for q in range(4):
                        kc = half * 4 + q
                        k.op("pe", lambda e, kc=kc, q=q, j=j: e.transpose(p[:, q * 128:(q + 1) * 128], xb[:, kc, j * 128:(j + 1) * 128], ident[:]),
                             reads=[xb, ident], writes=[p])
                    if half == 0:
                        k.op("act", lambda e: e.copy(out=fo[:, 0:512], in_=p[:, :]), reads=[p], writes=[fo])
                    else:
                        k.op("dve", lambda e: e.tensor_copy(out=fo[:, 512:1024], in_=p[:, :]), reads=[p], writes=[fo])
                r0 = t0 - TC + j * 128
                k.dma("sp", out[r0:r0 + 128, :], fo[:], reads=[fo], writes=[out])


def host_inputs(inputs, b):
    f = np.float32
    L = DEPTH
    c2 = np.stack([inputs["c"][b], inputs["c_ctx"]], 0).astype(f)
    c2T = np.ascontiguousarray(c2.reshape(2, 8, 128).transpose(2, 1, 0))
    perm = rope_partner_perm()
    w_in = inputs["w_in"]
    qk = w_in[:, :, 0:1024].reshape(L, D, 16, 64)
    w_inp = np.ascontiguousarray(qk[:, :, :, perm].reshape(L, D, 1024))
    cos, sin = rope_tables()
    m = {
        "x": np.ascontiguousarray(inputs["x"][b]),
        "ctx": np.ascontiguousarray(inputs["ctx"][b]),
        "c2T": c2T,
        "ada_w": inputs["ada_w"],
        "ada_bT": np.ascontiguousarray(inputs["ada_b"].reshape(L, 48, 128).transpose(0, 2, 1)),
        "n1gT": np.ascontiguousarray(inputs["norm1_g"].reshape(L, 8, 128).transpose(0, 2, 1)),
        "n2gT": np.ascontiguousarray(inputs["norm2_g"].reshape(L, 8, 128).transpose(0, 2, 1)),
        "w_in": w_in,
        "w_inp": w_inp,
        "w_out": inputs["w_out"],
        "cos_t": cos,
        "sin_t": sin,
        "ident": np.eye(128, dtype=f),
        "fgT": np.ascontiguousarray(inputs["final_g"].reshape(8, 128).T),
        "da_lambda": np.ascontiguousarray(inputs["da_lambda"].reshape(L, 256)),
        "sublnT": np.ascontiguousarray(inputs["da_subln_g"].T),
        "mcwT": np.ascontiguousarray(inputs["ml_conv_w"].reshape(L, 3, 4, 128).transpose(0, 3, 2, 1)),
        "mcbT": np.ascontiguousarray(inputs["ml_conv_b"].reshape(L, 4, 128).transpose(0, 2, 1)),
        "mgbT": np.ascontiguousarray(inputs["ml_gate_b"].reshape(L, 16, 1)),
        "mngT": np.ascontiguousarray(inputs["ml_norm_g"].reshape(L, 2, 128).transpose(0, 2, 1)),
        "triu": np.triu(np.ones((128, 128), f)),
        "tril": np.tril(np.ones((128, 128), f)),
        "routerT": np.ascontiguousarray(np.concatenate([inputs["router_wg"], inputs["router_we"]], -1).reshape(L, 8, 128, 36).transpose(0, 2, 1, 3)),
        "router_b": np.ascontiguousarray(np.concatenate([inputs["router_bg"], inputs["router_be"]], -1)),
        "rw_par": rw_par(inputs),
        "rw_w2p": rw_pad(inputs["rw_w2"]),
        "rw_a2p": rw_pad(inputs["rw_a2"]),
        "rw_g2": inputs["rw_g2"],
        "rw_lnp": np.ascontiguousarray(np.stack([inputs["rw_ln_g"].reshape(L, 2, 128), inputs["rw_ln_b"].reshape(L, 2, 128)], -1).transpose(0, 2, 1, 3)),
        "rw_mask": rw_masks(),
        "rw_reset": rw_reset(),
        "exp_w_gu": inputs["exp_w_gu"],
        "exp_w_down": inputs["exp_w_down"],
    }
    return m


def rw_par(inputs):
    L = DEPTH
    P = np.zeros((L, 64, 48), np.float32)
    mu = inputs["rw_shift_mu"]
    for h in range(4):
        P[:, :, 0 + h] = mu[:, h * 64:(h + 1) * 64]
        P[:, :, 4 + h] = mu[:, 256 + h * 64:256 + (h + 1) * 64]
        P[:, :, 8 + h] = mu[:, 512 + h * 64:512 + (h + 1) * 64]
        P[:, :, 31 + h] = inputs["rw_k_k"][:, h * 64:(h + 1) * 64]
        P[:, :, 35 + h] = inputs["rw_k_a"][:, h * 64:(h + 1) * 64]
        P[:, :, 43 + h] = inputs["rw_r_k"][:, h, :]
        for d in range(2):
            P[:, :, 15 + d * 4 + h] = inputs["rw_w0"][:, d, h * 64:(h + 1) * 64]
            P[:, :, 23 + d * 4 + h] = inputs["rw_a0"][:, d, h * 64:(h + 1) * 64]
    P[:, :, 12] = mu[:, 768:832]
    P[:, :, 13] = mu[:, 832:896]
    P[:, :, 14] = mu[:, 896:960]
    return P


def rw_pad(w):
    L = w.shape[0]
    o = np.zeros((L, 2, 64, 256), np.float32)
    o[:, 0, 0:32] = w[:, 0]
    o[:, 1, 32:64] = w[:, 1]
    return o


def rw_masks():
    f = np.float32
    su = np.triu(np.ones((64, 64), f), 1)
    iu = np.triu(np.ones((64, 64), f), 0)
    sl = np.tril(np.ones((64, 64), f), -1)
    il = np.tril(np.ones((64, 64), f), 0)
    m = np.zeros((2, 64, 5, 64), f)
    m[0, :, 0] = -su; m[0, :, 1] = -sl; m[0, :, 2] = su; m[0, :, 3] = -iu; m[0, :, 4] = iu
    m[1, :, 0] = -sl; m[1, :, 1] = -su; m[1, :, 2] = sl; m[1, :, 3] = -il; m[1, :, 4] = il
    return m


def rw_reset():
    r = np.ones((64, 512), np.float32)
    r[:, ::64] = 0.0
    return r


def kernel(**inputs):
    inputs = {k_: np.asarray(v) for k_, v in inputs.items()}
    nc = build()
    in_maps = [host_inputs(inputs, b) for b in range(8)]
    res = run_bass_kernel_spmd(nc, in_maps, core_ids=list(range(8)))
    return np.stack([r["out"] for r in res.results], 0).astype(np.float32)
```
